# Optimizing a Trainium2 kernel written in Bass

```python
import jax
import jax.numpy as jnp
from jax import lax
import numpy as np

D_MODEL = 1024
BATCH = 8
SEQ = 2048
DEPTH = 2

GRID_W = 64
CTX_LEN = 256

N_MLSTM_HEADS = 4
MLSTM_WIDTH = D_MODEL // 2
MLSTM_HEAD_DIM = MLSTM_WIDTH // N_MLSTM_HEADS
N_ROW_HEADS = N_MLSTM_HEADS // 2
MLSTM_CHUNK = 64
CONV_CH = D_MODEL // 4
CONV_K = 31
FOURIER_CH = D_MODEL // 4
N_FOURIER_GROUPS = 4
FOURIER_GROUP_CH = FOURIER_CH // N_FOURIER_GROUPS
MIX_WIDTH = MLSTM_WIDTH + CONV_CH + FOURIER_CH
N_GATES = 4
QKVG_COLS = 3 * MLSTM_WIDTH + N_GATES * N_MLSTM_HEADS
IN_COLS = QKVG_COLS + MLSTM_WIDTH + 2 * CONV_CH + FOURIER_CH
N_EXPERTS = 16
EC_CAPACITY_FACTOR = 2
EXPERT_HIDDEN = D_MODEL
N_ADA = 6
EPS = 1e-6

kernel_name = 'hybrid_mlstm_conv_fourier_ecmoe_dit'


def _rms(x, gain):
    xf = x.astype(jnp.float32)
    return xf * lax.rsqrt(jnp.mean(xf * xf, axis=-1, keepdims=True) + EPS) * gain.astype(jnp.float32)


def modulated_rmsnorm(x, gain, shift, scale):
    y = _rms(x, gain) * (1.0 + scale.astype(jnp.float32)) + shift.astype(jnp.float32)
    return y.astype(x.dtype)


def layernorm(x, gain, bias):
    xf = x.astype(jnp.float32)
    mu = jnp.mean(xf, axis=-1, keepdims=True)
    var = jnp.mean(jnp.square(xf - mu), axis=-1, keepdims=True)
    y = (xf - mu) * lax.rsqrt(var + EPS) * gain.astype(jnp.float32) + bias.astype(jnp.float32)
    return y.astype(x.dtype)


def _raster_to_colmajor(a, rows):
    b, h, t, f = a.shape
    return a.reshape(b, h, rows, GRID_W, f).swapaxes(2, 3).reshape(b, h, t, f)


def _colmajor_to_raster(a, rows):
    b, h, t, f = a.shape
    return a.reshape(b, h, GRID_W, rows, f).swapaxes(2, 3).reshape(b, h, t, f)


def _to_chunks(a):
    b, h, t = a.shape[:3]
    a = a.reshape((b, h, t // MLSTM_CHUNK, MLSTM_CHUNK) + a.shape[3:])
    return jnp.moveaxis(a, 2, 0)


def _from_chunks(a):
    a = jnp.moveaxis(a, 0, 2)
    return a.reshape((a.shape[0], a.shape[1], -1) + a.shape[4:])


def _chunk_outputs(q, k, v, i_pre, b, c_st, n_st, m_st):
    L = q.shape[-2]
    lower = jnp.tril(jnp.ones((L, L), dtype=bool))
    dlog = jnp.where(lower, b[..., :, None] - b[..., None, :] + i_pre[..., None, :], -jnp.inf)
    inter = b + m_st[..., None]
    m_t = jnp.maximum(inter, jnp.max(dlog, axis=-1))
    s = jnp.einsum('...td,...sd->...ts', q, k) * jnp.exp(dlog - m_t[..., None])
    w_inter = jnp.exp(inter - m_t)
    num = w_inter[..., None] * jnp.einsum('...ed,...td->...te', c_st, q) + jnp.einsum('...ts,...se->...te', s, v)
    den = w_inter * jnp.einsum('...td,...d->...t', q, n_st) + jnp.sum(s, axis=-1)
    return num / jnp.maximum(jnp.abs(den), jnp.exp(-m_t))[..., None]


def mlstm_direction(q, k, v, i_pre, log_f, state0, with_outputs):
    qc, kc, vc, ic = (_to_chunks(a) for a in (q, k, v, i_pre))
    bc = jnp.cumsum(_to_chunks(log_f), axis=-1)

    def step(state, inp):
        c_st, n_st, m_st = state
        kx, vx, ix, bx = inp
        b_last = bx[..., -1]
        wlog = b_last[..., None] - bx + ix
        m_new = jnp.maximum(b_last + m_st, jnp.max(wlog, axis=-1))
        decay = jnp.exp(b_last + m_st - m_new)
        w = jnp.exp(wlog - m_new[..., None])
        c_new = decay[..., None, None] * c_st + jnp.einsum('bhs,bhse,bhsd->bhed', w, vx, kx)
        n_new = decay[..., None] * n_st + jnp.einsum('bhs,bhsd->bhd', w, kx)
        return (c_new, n_new, m_new), (state if with_outputs else None)

    final, starts = lax.scan(step, state0, (kc, vc, ic, bc))
    if not with_outputs:
        return None, final
    h = _chunk_outputs(qc, kc, vc, ic, bc, *starts)
    return _from_chunks(h), final


def mlstm_bidir(q, k, v, gates, init_fwd, init_bwd, with_outputs):
    flip = lambda a: jnp.flip(a, axis=2)
    h_f, st_f = mlstm_direction(q, k, v, gates[..., 0], jax.nn.log_sigmoid(gates[..., 1]), init_fwd, with_outputs)
    h_b, st_b = mlstm_direction(flip(q), flip(k), flip(v), flip(gates[..., 2]),
                                flip(jax.nn.log_sigmoid(gates[..., 3])), init_bwd, with_outputs)
    h = h_f + flip(h_b) if with_outputs else None
    return h, st_f, st_b


def zero_state(bsz):
    return (jnp.zeros((bsz, N_MLSTM_HEADS, MLSTM_HEAD_DIM, MLSTM_HEAD_DIM), jnp.float32),
            jnp.zeros((bsz, N_MLSTM_HEADS, MLSTM_HEAD_DIM), jnp.float32),
            jnp.zeros((bsz, N_MLSTM_HEADS), jnp.float32))


def mlstm_heads(u_qkvg, b_gates, rows):
    bsz, t, _ = u_qkvg.shape
    per_head = lambda a: a.reshape(bsz, t, N_MLSTM_HEADS, -1).transpose(0, 2, 1, 3)
    q, k, v, g = jnp.split(u_qkvg.astype(jnp.float32), [MLSTM_WIDTH, 2 * MLSTM_WIDTH, 3 * MLSTM_WIDTH], axis=-1)
    z = jnp.concatenate([per_head(q), per_head(k) * MLSTM_HEAD_DIM ** -0.5, per_head(v),
                         per_head(g) + b_gates.astype(jnp.float32)[None, :, None, :]], axis=-1)
    if rows is not None:
        z = jnp.concatenate([z[:, :N_ROW_HEADS], _raster_to_colmajor(z[:, N_ROW_HEADS:], rows)], axis=1)
    return jnp.split(z, [MLSTM_HEAD_DIM, 2 * MLSTM_HEAD_DIM, 3 * MLSTM_HEAD_DIM], axis=-1)


def conformer_conv(a, g, conv_w, conv_b, ln_g, ln_b):
    y = a * jax.nn.sigmoid(g)
    y = lax.conv_general_dilated(y, conv_w[:, None, :], window_strides=(1,),
                                 padding=[(CONV_K // 2, CONV_K // 2)],
                                 dimension_numbers=('NWC', 'WIO', 'NWC'),
                                 feature_group_count=y.shape[-1]) + conv_b
    return jax.nn.silu(layernorm(y, ln_g, ln_b))


def fourier_mix(f):
    bsz, t, _ = f.shape
    z = f.astype(jnp.float32).reshape(bsz, t, N_FOURIER_GROUPS, FOURIER_GROUP_CH)
    z = jnp.fft.fft2(z, axes=(1, 3), norm='ortho').real
    return z.reshape(bsz, t, FOURIER_CH).astype(f.dtype)


def token_mixer(u, b_gates, g_hnorm, conv_w, conv_b, conv_ln_g, conv_ln_b, init_fwd, init_bwd, rows):
    bsz, t, _ = u.shape
    o0 = QKVG_COLS + MLSTM_WIDTH
    u_qkvg, o, ca, cg, fr = jnp.split(u, [QKVG_COLS, o0, o0 + CONV_CH, o0 + 2 * CONV_CH], axis=-1)
    q, k, v, g = mlstm_heads(u_qkvg, b_gates, rows)
    h, st_f, st_b = mlstm_bidir(q, k, v, g, init_fwd, init_bwd, True)
    if rows is not None:
        h = jnp.concatenate([h[:, :N_ROW_HEADS], _colmajor_to_raster(h[:, N_ROW_HEADS:], rows)], axis=1)
    h = h * lax.rsqrt(jnp.mean(h * h, axis=-1, keepdims=True) + EPS) \
        * g_hnorm.astype(jnp.float32).reshape(N_MLSTM_HEADS, 1, MLSTM_HEAD_DIM)
    h = h.transpose(0, 2, 1, 3).reshape(bsz, t, MLSTM_WIDTH).astype(u.dtype) * jax.nn.sigmoid(o)
    y = jnp.concatenate([h, conformer_conv(ca, cg, conv_w, conv_b, conv_ln_g, conv_ln_b), fourier_mix(fr)], axis=-1)
    return y, st_f, st_b


def expert_choice_ffn(xn, w_router, w_gate, w_up, w_down):
    bsz, n, _ = xn.shape
    cap = EC_CAPACITY_FACTOR * n // N_EXPERTS
    aff = jax.nn.softmax(jnp.einsum('bnd,de->bne', xn, w_router).astype(jnp.float32), axis=-1)
    gate, idx = lax.top_k(jnp.swapaxes(aff, 1, 2), cap)
    bidx = jnp.arange(bsz)[:, None, None]
    xs = xn[bidx, idx]
    hid = jax.nn.silu(jnp.einsum('becd,edf->becf', xs, w_gate)) * jnp.einsum('becd,edf->becf', xs, w_up)
    ys = jnp.einsum('becf,efd->becd', hid, w_down) * gate[..., None].astype(xn.dtype)
    return jnp.zeros_like(xn).at[bidx, idx].add(ys)


def setup_inputs(seed: int = 0) -> dict:
    key = jax.random.key(seed)
    ks = jax.random.split(key, 24)
    nrm = lambda k, shape, s: jax.random.normal(k, shape, jnp.float32) * s
    D, L, H = D_MODEL, DEPTH, N_MLSTM_HEADS
    ig = nrm(ks[8], (L, H, 2), 0.1)
    fg = jnp.linspace(3.0, 6.0, H)[None, :, None] + nrm(ks[9], (L, H, 2), 0.1)
    b_gates = jnp.stack([ig[..., 0], fg[..., 0], ig[..., 1], fg[..., 1]], axis=-1)
    return {
        'x': nrm(ks[0], (BATCH, SEQ, D), 1.0),
        'c': nrm(ks[1], (BATCH, D), 1.0),
        'ctx': nrm(ks[2], (BATCH, CTX_LEN, D), 1.0),
        'c_ctx': nrm(ks[3], (D,), 1.0),
        'w_ada': nrm(ks[4], (L, D, N_ADA * D), 0.5 * D ** -0.5),
        'b_ada': nrm(ks[5], (L, N_ADA * D), 0.02),
        'g_norm1': 1.0 + nrm(ks[6], (L, D), 0.02),
        'w_in': nrm(ks[7], (L, D, IN_COLS), D ** -0.5),
        'b_gates': b_gates,
        'g_hnorm': 1.0 + nrm(ks[10], (L, MLSTM_WIDTH), 0.02),
        'conv_w': nrm(ks[11], (L, CONV_K, CONV_CH), CONV_K ** -0.5),
        'conv_b': nrm(ks[12], (L, CONV_CH), 0.02),
        'conv_ln_g': 1.0 + nrm(ks[13], (L, CONV_CH), 0.02),
        'conv_ln_b': nrm(ks[14], (L, CONV_CH), 0.02),
        'w_out': nrm(ks[15], (L, MIX_WIDTH, D), MIX_WIDTH ** -0.5),
        'g_norm2': 1.0 + nrm(ks[16], (L, D), 0.02),
        'w_router': nrm(ks[17], (L, D, N_EXPERTS), D ** -0.5),
        'w_e_gate': nrm(ks[18], (L, N_EXPERTS, D, EXPERT_HIDDEN), D ** -0.5),
        'w_e_up': nrm(ks[19], (L, N_EXPERTS, D, EXPERT_HIDDEN), D ** -0.5),
        'w_e_down': nrm(ks[20], (L, N_EXPERTS, EXPERT_HIDDEN, D), EXPERT_HIDDEN ** -0.5),
        'g_final': 1.0 + nrm(ks[21], (D,), 0.02),
    }


def reference(x, c, ctx, c_ctx, w_ada, b_ada, g_norm1, w_in, b_gates, g_hnorm, conv_w, conv_b,
              conv_ln_g, conv_ln_b, w_out, g_norm2, w_router, w_e_gate, w_e_up, w_e_down, g_final):
    bsz, t, _ = x.shape
    rows = t // GRID_W
    h_ctx = ctx
    silu_c = jax.nn.silu(c)
    silu_cc = jax.nn.silu(c_ctx)
    for l in range(DEPTH):
        last = l == DEPTH - 1
        mod = silu_c @ w_ada[l] + b_ada[l]
        mod_c = silu_cc @ w_ada[l] + b_ada[l]
        sh1, sc1, gt1, sh2, sc2, gt2 = jnp.split(mod[:, None, :], N_ADA, axis=-1)
        csh1, csc1, cgt1, csh2, csc2, cgt2 = jnp.split(mod_c, N_ADA)
        init = zero_state(h_ctx.shape[0])
        xn_c = modulated_rmsnorm(h_ctx, g_norm1[l], csh1, csc1)
        if last:
            q, k, v, g = mlstm_heads(xn_c @ w_in[l][:, :QKVG_COLS], b_gates[l], None)
            _, st_f, st_b = mlstm_bidir(q, k, v, g, init, init, False)
        else:
            mix_c, st_f, st_b = token_mixer(xn_c @ w_in[l], b_gates[l], g_hnorm[l], conv_w[l], conv_b[l],
                                            conv_ln_g[l], conv_ln_b[l], init, init, None)
            h_ctx = h_ctx + cgt1 * (mix_c @ w_out[l])
            h_ctx = h_ctx + cgt2 * expert_choice_ffn(modulated_rmsnorm(h_ctx, g_norm2[l], csh2, csc2),
                                                     w_router[l], w_e_gate[l], w_e_up[l], w_e_down[l])
        xn = modulated_rmsnorm(x, g_norm1[l], sh1, sc1)
        mix_x, _, _ = token_mixer(xn @ w_in[l], b_gates[l], g_hnorm[l], conv_w[l], conv_b[l],
                                  conv_ln_g[l], conv_ln_b[l], st_f, st_b, rows)
        x = x + gt1 * (mix_x @ w_out[l])
        x = x + gt2 * expert_choice_ffn(modulated_rmsnorm(x, g_norm2[l], sh2, sc2),
                                        w_router[l], w_e_gate[l], w_e_up[l], w_e_down[l])
    return _rms(x, g_final).astype(x.dtype)
```

```python
from concourse.bass_utils import run_bass_kernel_spmd
import numpy as np
import concourse.bass as bass
import concourse.mybir as mybir
from contextlib import ExitStack

F32 = mybir.dt.float32
F32R = mybir.dt.float32r
I32 = mybir.dt.int32
U32 = mybir.dt.uint32
AF = mybir.ActivationFunctionType
ALU = mybir.AluOpType
AX = mybir.AxisListType

ENG_ATTR = {'pe': 'tensor', 'dve': 'vector', 'act': 'scalar', 'pool': 'gpsimd', 'sp': 'sync'}


class _Res:
    __slots__ = ('name', 'writers', 'readers', 'semi')

    def __init__(self, name):
        self.name = name
        self.writers = []
        self.readers = []
        self.semi = {}


class _Op:
    __slots__ = ('eng', 'fn', 'dma_res', 'dma_sem', 'waits_c', 'waits_s', 'signal', 'sig')

    def __init__(self, eng, fn):
        self.eng = eng
        self.fn = fn
        self.dma_res = None
        self.dma_sem = None
        self.waits_c = []
        self.waits_s = []
        self.signal = False
        self.sig = 0


class Prog:
    def __init__(self, nc):
        self.nc = nc
        self.q = {e: [] for e in ENG_ATTR}
        self.res = {}
        self.nops = 0
        self.semcnt = []
        self.semcls = []
        self.free_sems = {'hw': [], 'sw': []}

    def _r(self, key):
        r = self.res.get(key)
        if r is None:
            r = self.res[key] = _Res(key)
        return r

    def add(self, eng, fn, r=(), w=(), cw=(), dma=False):
        op = _Op(eng, fn)
        deps = []
        for key in r:
            R = self._r(key)
            deps.extend(R.writers)
            R.readers.append(op)
        for key in w:
            R = self._r(key)
            deps.extend(R.writers)
            deps.extend(R.readers)
            R.writers = [op]
            R.readers = []
        for key in cw:
            R = self._r(key)
            if R.readers:
                deps.extend(R.readers)
                R.writers = [op]
                R.readers = []
            else:
                R.writers.append(op)
        for d in deps:
            if d is op:
                continue
            if d.dma_res is not None:
                si = d.dma_sem
                op.waits_s.append((si, self.semcnt[si]))
            else:
                if d.eng == eng and eng == 'pe':
                    continue
                d.signal = True
                op.waits_c.append(d)
        if dma:
            keys = list(w) + list(cw)
            assert len(keys) == 1, keys
            R = self._r(keys[0])
            cls = 'sw' if eng == 'pool' else 'hw'
            si = R.semi.get(cls)
            if si is None:
                if self.free_sems[cls]:
                    si = self.free_sems[cls].pop()
                else:
                    self.semcnt.append(0)
                    self.semcls.append(cls)
                    si = len(self.semcnt) - 1
                R.semi[cls] = si
            self.semcnt[si] += 1
            op.dma_res = R
            op.dma_sem = si
        self.q[eng].append(op)
        self.nops += 1
        return op

    def dma(self, eng, out, in_, r=(), w=(), cw=(), **kw):
        return self.add(eng, lambda e: e.dma_start(out=out, in_=in_, **kw), r=r, w=w, cw=cw, dma=True)

    def barrier(self):
        lasts = []
        for e in ('pe', 'dve', 'act', 'pool'):
            for o in reversed(self.q[e]):
                if o.dma_res is None:
                    lasts.append(o)
                    break
        dres = [(si, self.semcnt[si]) for R in self.res.values() for si in R.semi.values()]
        for e in ENG_ATTR:
            op = _Op(e, lambda en: en.nop())
            for d in lasts:
                if d.dma_res is None:
                    d.signal = True
                    op.waits_c.append(d)
            op.waits_s = dres
            self.q[e].append(op)
        self.res = {}
        self.free_sems = {c: [i for i in range(len(self.semcnt)) if self.semcls[i] == c] for c in ('hw', 'sw')}

    def emit(self):
        nc = self.nc
        with ExitStack() as es:
            csem = {}
            for e in ('pe', 'dve', 'act', 'pool'):
                csem[e] = es.enter_context(nc.semaphore('c_' + e))
            dsem = [es.enter_context(nc.semaphore('d%d' % i)) for i in range(len(self.semcnt))]
            for e, ops in self.q.items():
                n = 0
                for op in ops:
                    if op.signal:
                        n += 1
                        op.sig = n
            q = self.q

            def run(eng_name, e):
                waited = {}
                for op in q[eng_name]:
                    for d in op.waits_c:
                        s = csem[d.eng]
                        if waited.get(d.eng, 0) < d.sig:
                            e.wait_ge(s, d.sig)
                            waited[d.eng] = d.sig
                    for si, cnt in op.waits_s:
                        v = 16 * cnt
                        s = dsem[si]
                        if waited.get(si, 0) < v:
                            e.wait_ge(s, v)
                            waited[si] = v
                    ins = op.fn(e)
                    if op.dma_sem is not None:
                        ins.then_inc(dsem[op.dma_sem], 16)
                    elif op.signal:
                        ins.then_inc(csem[eng_name], 1)

            with nc.Block() as block:
                @block.tensor
                def _(e):
                    run('pe', e)

                @block.vector
                def _(e):
                    run('dve', e)

                @block.scalar
                def _(e):
                    run('act', e)

                @block.gpsimd
                def _(e):
                    run('pool', e)

                @block.sync
                def _(e):
                    run('sp', e)
        return len(self.semcnt)


import os

L = 2
D = 1024
TL = 2048
TC = 256
T = TL + TC
NT = T // 128
INC = 2832
NE = 16
CAPL, CAPC = 256, 32
NSLOT = CAPL + CAPC
EPS = 1e-6
TCH = [(0, 512), (512, 512), (1024, 512), (1536, 512), (2048, 256)]
CHUNKS = ([(i * 128, 128) for i in range(12)] + [(1536, 16)] +
          [(1552 + i * 128, 128) for i in range(4)] + [(2064 + i * 128, 128) for i in range(2)] +
          [(2320 + i * 128, 128) for i in range(2)] + [(2576 + i * 128, 128) for i in range(2)])

C_ID, C_ONE, C_TU, C_TL, C_TS, C_BD, C_DC, C_EPS = 0, 128, 256, 384, 512, 640, 896, 1920
NCONST = 1924
NCONST2 = NCONST + 578 + 18


def make_consts():
    c = np.zeros((128, NCONST), np.float32)
    i = np.arange(128)
    c[:, C_ID:C_ID + 128] = np.eye(128)
    c[:, C_ONE:C_ONE + 128] = 1.0
    c[:, C_TU:C_TU + 128] = (i[:, None] <= i[None, :])
    c[:, C_TL:C_TL + 128] = (i[:, None] >= i[None, :])
    c[:, C_TS:C_TS + 128] = (i[:, None] < i[None, :])
    j = np.arange(64)
    ang = 2 * np.pi * np.outer(j, j) / 64.0
    bc = np.zeros((128, 128)); bs = np.zeros((128, 128))
    for g in range(2):
        bc[g * 64:(g + 1) * 64, g * 64:(g + 1) * 64] = np.cos(ang)
        bs[g * 64:(g + 1) * 64, g * 64:(g + 1) * 64] = np.sin(ang)
    c[:, C_BD:C_BD + 128] = bc
    c[:, C_BD + 128:C_BD + 256] = bs
    t = np.arange(256)
    a2 = 2 * np.pi * np.outer(t, t) / 256.0
    cc = np.cos(a2).reshape(2, 128, 256).transpose(1, 0, 2).reshape(128, 512)
    ss = np.sin(a2).reshape(2, 128, 256).transpose(1, 0, 2).reshape(128, 512)
    c[:, C_DC:C_DC + 512] = cc
    c[:, C_DC + 512:C_DC + 1024] = ss
    c[:, C_EPS] = EPS
    c[:, C_EPS + 1] = 1.0
    c[:, C_EPS + 2] = -0.5 * np.log(128.0)
    return c


def make_dft():
    t = np.arange(TL, dtype=np.float64)
    a = 2 * np.pi * ((np.outer(t, t)) % TL) / TL
    return np.cos(a).astype(np.float32), np.sin(a).astype(np.float32)


class K:
    def __init__(self, stop_after=None, taps=(), ne_decl=NE):
        self.stop_after = stop_after
        self.taps = taps
        nc = self.nc = bass.Bass("TRN2", target_bir_lowering=False)
        self.P = Prog(nc)
        dt = lambda n, s, kind="ExternalInput", d=F32: nc.dram_tensor(n, s, d, kind=kind).ap()
        self.x_in = dt("x", [TL, D]); self.ctx_in = dt("ctx", [TC, D]); self.cT = dt("cT", [128, 16])
        self.w_ada = dt("w_ada", [L, D, 6 * D]); self.b_ada = dt("b_ada", [L, 6 * D])
        self.g_norm1 = dt("g_norm1", [L, D]); self.w_in = dt("w_in", [L, D, INC])
        self.b_gates = dt("b_gates", [L, 16]); self.g_hnorm = dt("g_hnorm", [L, 512])
        self.conv_wT = dt("conv_wT", [L, 256, 31]); self.conv_b = dt("conv_b", [L, 256])
        self.conv_ln_g = dt("conv_ln_g", [L, 256]); self.conv_ln_b = dt("conv_ln_b", [L, 256])
        self.w_out = dt("w_out", [L, D, D]); self.g_norm2 = dt("g_norm2", [L, D])
        self.w_router = dt("w_router", [L, D, NE])
        self.w_e_gate = dt("w_e_gate", [L, ne_decl, D, D]); self.w_e_up = dt("w_e_up", [L, ne_decl, D, D])
        self.w_e_down = dt("w_e_down", [L, ne_decl, D, D]); self.g_final = dt("g_final", [D])
        self.consts = dt("consts", [128, NCONST2]); self.dftc = dt("dftc", [TL, TL]); self.dfts = dt("dfts", [TL, TL])
        self.out = dt("out", [TL, D], kind="ExternalOutput")
        self.modrows = dt("modrows", [L, 6, 2, D], kind="Internal")
        self.uT = dt("uT", [INC, T], kind="Internal")
        self.yT = dt("yT", [D, T], kind="Internal")
        self.xres = dt("xres", [T, D], kind="Internal")
        self.xn2x = dt("xn2x", [T, 1042], kind="Internal")
        self.xs = dt("xs", [NE * NSLOT, 1042], kind="Internal")
        self.macc = dt("macc", [T, D], kind="Internal")
        self.tapd = {}
        for name, shape in taps:
            self.tapd[name] = dt("tap_" + name, shape, kind="ExternalOutput")

    def build(self):
        nc, P = self.nc, self.P
        with ExitStack() as es:
            AW = 50600
            self.A = es.enter_context(nc.sbuf_tensor("arena", [128, AW], F32))
            self.CS = es.enter_context(nc.sbuf_tensor("cs", [128, NCONST2], F32))
            self.ps = [es.enter_context(nc.psum_tensor("ps%d" % i, [128, 512], F32)) for i in range(8)]
            P.dma('sp', self.CS[:, :], self.consts[:, :], w=['CS'])
            P.barrier()
            self.phases()
            P.barrier()
            nsem = P.emit()
            print("ops", P.nops, "dma sems", nsem)
        return nc

    def tap_yT(self):
        if 'yT' in self.tapd:
            self.P.dma('sp', self.tapd['yT'][:, :], self.yT[:, :], w=['tapy'])
            self.P.barrier()

    def tap_xres(self):
        if 'xres' in self.tapd:
            self.P.dma('sp', self.tapd['xres'][:, :], self.xres[:, :], w=['tapxr'])
            self.P.barrier()

    def bcreg(self, e):
        if getattr(self, '_bcr', None) is None:
            self._bcr = e.to_reg(NE * NSLOT - 1)
        return self._bcr

    def carve(self, off, n):
        return self.A[:, off:off + n]

    def phases(self):
        self.phase0(0)
        self.phase0(1)
        if self.stop_after == 0:
            return
        for l in range(L):
            self.phase1(l)
            if self.stop_after == 1:
                return
            for h in range(4):
                self.phase2(l, h)
            if self.stop_after == 2:
                self.tap_yT()
                return
            self.phase34(l)
            if self.stop_after == 4:
                self.tap_yT()
                return
            self.phase56(l)
            if self.stop_after == 5:
                self.tap_xres()
                return
            self.phase6b(l)
            if self.stop_after in (6, 61):
                return
            self.phase8(l)
            if l == L - 1:
                self.phase9(l)
            if self.stop_after == 9:
                self.tap_xres()
                return

    def phase0_steps(self, l):
        P, CS = self.P, self.CS
        sc = self.carve(20000, 16)
        sc2 = self.carve(20016, 16)
        wb = [self.carve(21024 + i * 4096, 4096).rearrange("p (k n) -> p k n", k=8) for i in range(2)]
        brow = [self.carve(30000 + i * 1024, 1024) for i in range(2)]
        grow = [self.carve(32048 + i * 1024, 1024) for i in range(2)]
        mrow = [self.carve(34096 + i * 1024, 1024) for i in range(2)]
        sc2v = sc2.rearrange("p (k c) -> p k c", c=2)
        steps = []

        def init():
            P.dma('sp', sc, self.cT[:, :], w=['sc'])
            P.add('act', lambda e: e.activation(out=sc2, in_=sc, func=AF.Silu), r=['sc'], w=['sc2'])
        steps.append((init, None))
        for seg in range(6):
            for half in range(2):
                def pe_step(seg=seg, half=half):
                    sb = seg % 2
                    if half == 0:
                        P.dma('sp', brow[sb][0:2, :], self.b_ada[l, seg * D:(seg + 1) * D].partition_broadcast(2), w=['brow%d' % sb])
                        if seg in (1, 4):
                            g = self.g_norm1 if seg == 1 else self.g_norm2
                            P.dma('sp', grow[sb][0:2, :], g[l, :].partition_broadcast(2), w=['grow%d' % sb])
                    n0 = seg * D + half * 512
                    b = half
                    wt = wb[b]
                    P.dma('sp', wt, self.w_ada[l, :, n0:n0 + 512].rearrange("(kc p) n -> p kc n", p=128), w=['wb%d' % b])
                    pb = self.ps[6 + b]
                    for kc in range(8):
                        P.add('pe', lambda e, kc=kc, wt=wt, pb=pb: e.matmul(pb[0:2, :], lhsT=sc2v[:, kc, :], rhs=wt[:, kc, :],
                                                                            start=(kc == 0), stop=(kc == 7)),
                              r=['sc2', 'wb%d' % b], w=['ps%d' % (6 + b)])

                def post_step(seg=seg, half=half):
                    sb = seg % 2
                    b = half
                    pb = self.ps[6 + b]
                    mr = mrow[sb]; mk = 'mrow%d' % sb
                    P.add('dve', lambda e: e.tensor_tensor(out=mr[0:2, half * 512:(half + 1) * 512], in0=pb[0:2, :],
                                                           in1=brow[sb][0:2, half * 512:(half + 1) * 512], op=ALU.add),
                          r=['brow%d' % sb], w=['ps%d' % (6 + b), mk])
                    if half == 1:
                        if seg in (1, 4):
                            P.add('dve', lambda e: e.scalar_tensor_tensor(out=mr[0:2, :], in0=mr[0:2, :], scalar=1.0, in1=grow[sb][0:2, :],
                                                                          op0=ALU.add, op1=ALU.mult), r=['grow%d' % sb], w=[mk])
                        P.dma('pool', self.modrows[l, seg, :, :], mr[0:2, :], r=[mk], cw=['modrows'])
                steps.append((pe_step, post_step))
        return steps

    def phase0(self, l):
        for pe_step, post_step in self.phase0_steps(l):
            pe_step()
            if post_step is not None:
                post_step()
        self.P.barrier()

    def phase1(self, l):
        P, CS = self.P, self.CS
        ident = CS[:, C_ID:C_ID + 128]
        xnT = self.carve(0, 8 * T).rearrange("p (k t) -> p k t", k=8)
        o = 8 * T
        modt = {}
        for nm, seg, row in (('gs_l', 1, 0), ('sh_l', 0, 0), ('gs_c', 1, 1), ('sh_c', 0, 1)):
            modt[nm] = self.carve(o, 1024); o += 1024
            P.dma('sp', modt[nm], self.modrows[l, seg, row, :].partition_broadcast(128), w=[nm])
        NB1 = 3
        xt = [self.carve(o + i * 1024, 1024) for i in range(NB1)]; o += NB1 * 1024
        xn = [self.carve(o + i * 1024, 1024) for i in range(NB1)]; o += NB1 * 1024
        sqs = [self.carve(o + i * 1024, 1024) for i in range(NB1)]; o += NB1 * 1024
        st = [self.carve(o + i * 4, 4) for i in range(NB1)]; o += 4 * NB1
        wbs = [self.carve(o + i * 1024, 1024).rearrange("p (k n) -> p k n", k=8) for i in range(3)]; o += 3072
        stg = [self.carve(o + i * 512, 512) for i in range(4)]; o += 2048
        if l > 0:
            mtl = [self.carve(o + i * 1024, 1024) for i in range(NB1)]; o += NB1 * 1024
            gt2 = {'l': self.carve(o, 1024), 'c': self.carve(o + 1024, 1024)}; o += 2048
            P.dma('sp', gt2['l'], self.modrows[l - 1, 5, 0, :].partition_broadcast(128), w=['gt2l'])
            P.dma('sp', gt2['c'], self.modrows[l - 1, 5, 1, :].partition_broadcast(128), w=['gt2c'])
        NPRE = int(os.environ.get('K_NPRE', '0'))
        nbc = [0]

        def loadw(ci):
            c0, w = CHUNKS[ci]
            wt = wbs[ci % 3]
            P.dma('sp', wt[:, :, 0:w], self.w_in[l, :, c0:c0 + w].rearrange("(kc p) n -> p kc n", p=128), w=['wbs%d' % (ci % 3)])

        def proj(ci, tci):
            c0, w = CHUNKS[ci]
            t0, n = TCH[tci]
            if l == L - 1 and c0 >= 1552 and tci == 0:
                t0, n = TC, 512 - TC
            wt = wbs[ci % 3]; wk = 'wbs%d' % (ci % 3)
            bank = 2 + nbc[0] % 6; sb = nbc[0] % 4; nbc[0] += 1
            pb = self.ps[bank]
            rk = ['xnT%d' % i for i in range(t0 // 128, (t0 + n) // 128)] + [wk]
            for kc in range(8):
                lh, rh = wt[:, kc, 0:w], xnT[:, kc, t0:t0 + n]
                P.add('pe', lambda e, pb=pb, lh=lh, rh=rh, kc=kc, w=w, n=n: e.matmul(pb[0:w, 0:n], lhsT=lh, rhs=rh, start=(kc == 0), stop=(kc == 7)),
                      r=rk, w=['ps%d' % bank])
            sg = stg[sb]
            if nbc[0] % 2 == 0:
                P.add('act', lambda e, sg=sg, pb=pb, w=w, n=n: e.activation(out=sg[0:w, 0:n], in_=pb[0:w, 0:n], func=AF.Copy),
                      w=['ps%d' % bank, 'stg%d' % sb])
            else:
                P.add('dve', lambda e, sg=sg, pb=pb, w=w, n=n: e.tensor_copy(out=sg[0:w, 0:n], in_=pb[0:w, 0:n]),
                      w=['ps%d' % bank, 'stg%d' % sb])
            P.dma('pool', self.uT[c0:c0 + w, t0:t0 + n], sg[0:w, 0:n], r=['stg%d' % sb], cw=['uT'])
        for ci in range(NPRE):
            loadw(ci)
        ready = {3: 0, 7: 1, 11: 2, 15: 3, 17: 4}
        def normN(i):
            b = i % NB1
            isc = i < 2
            sq = sqs[b]
            if l == 0:
                src = self.ctx_in[i * 128:(i + 1) * 128, :] if isc else self.x_in[(i - 2) * 128:(i - 1) * 128, :]
            else:
                src = self.xres[i * 128:(i + 1) * 128, :]
            x_, xn_, st_ = xt[b], xn[b], st[b]
            P.dma('sp', x_, src, w=['xt%d' % b])
            if l > 0:
                m_ = mtl[b]
                g2 = gt2['c' if isc else 'l']; g2k = 'gt2c' if isc else 'gt2l'
                P.dma('sp', m_, self.macc[i * 128:(i + 1) * 128, :], w=['mtl%d' % b])
                P.add('dve', lambda e, m_=m_, g2=g2: e.tensor_tensor(out=m_, in0=m_, in1=g2, op=ALU.mult), r=[g2k], w=['mtl%d' % b])
                P.add('pool', lambda e, m_=m_, x_=x_: e.tensor_tensor(out=x_, in0=m_, in1=x_, op=ALU.add), r=['mtl%d' % b], w=['xt%d' % b])
                P.dma('pool', self.xres[i * 128:(i + 1) * 128, :], x_, r=['xt%d' % b], cw=['xres'])
            P.add('act', lambda e, x_=x_, st_=st_, sq=sq: e.activation(out=sq, in_=x_, func=AF.Square, accum_out=st_[:, 0:1]),
                  r=['xt%d' % b], w=['sq%d' % b, 'st%d' % b])
            P.add('act', lambda e, st_=st_: e.activation(out=st_[:, 1:2], in_=st_[:, 0:1], func=AF.Sqrt, scale=1.0 / D,
                                                         bias=CS[:, C_EPS:C_EPS + 1]), w=['st%d' % b])
            P.add('dve', lambda e, st_=st_: e.reciprocal(out=st_[:, 2:3], in_=st_[:, 1:2]), w=['st%d' % b])
            gs = modt['gs_c' if isc else 'gs_l']; sh = modt['sh_c' if isc else 'sh_l']
            gk = 'gs_c' if isc else 'gs_l'; sk = 'sh_c' if isc else 'sh_l'
            P.add('dve', lambda e, x_=x_, xn_=xn_, st_=st_, gs=gs: e.scalar_tensor_tensor(out=xn_, in0=x_, scalar=st_[:, 2:3], in1=gs,
                                                                                           op0=ALU.mult, op1=ALU.mult),
                  r=['xt%d' % b, 'st%d' % b, gk], w=['xn%d' % b])
            P.add('pool', lambda e, xn_=xn_, sh=sh: e.tensor_tensor(out=xn_, in0=xn_, in1=sh, op=ALU.add), r=[sk], w=['xn%d' % b])

        def transX(i):
            b = i % NB1
            xn_ = xn[b]
            for hb in range(2):
                pb = self.ps[hb]
                for k4 in range(4):
                    kc = hb * 4 + k4
                    P.add('pe', lambda e, pb=pb, k4=k4, kc=kc, xn_=xn_: e.transpose(pb[:, k4 * 128:(k4 + 1) * 128], xn_[:, kc * 128:(kc + 1) * 128], ident),
                          r=['xn%d' % b, 'CS'], w=['ps%d' % hb])
                dst = xnT[:, hb * 4:hb * 4 + 4, i * 128:(i + 1) * 128]
                srcp = pb[:, :].rearrange("p (k t) -> p k t", k=4)
                if hb == 0:
                    P.add('act', lambda e, dst=dst, srcp=srcp: e.activation(out=dst, in_=srcp, func=AF.Copy), w=['ps0'], cw=['xnT%d' % i])
                else:
                    P.add('dve', lambda e, dst=dst, srcp=srcp: e.tensor_copy(out=dst, in_=srcp), w=['ps1'], cw=['xnT%d' % i])
            if i in ready:
                for ci in range(NPRE):
                    proj(ci, ready[i])
        normN(0)
        for i in range(NT):
            if i + 1 < NT:
                normN(i + 1)
            transX(i)
        if 'xnT' in self.tapd and l == 0:
            P.dma('sp', self.tapd['xnT'].rearrange("(k p) t -> p k t", p=128), xnT, r=['xnT%d' % i for i in range(NT)], w=['tapx'])
        for ci in range(NPRE, len(CHUNKS)):
            loadw(ci)
            for tci in range(len(TCH)):
                proj(ci, tci)
        P.barrier()
        if 'uT' in self.tapd and l == 0:
            P.dma('sp', self.tapd['uT'][:, :], self.uT[:, :], w=['tapu'])
            P.barrier()


def host_inputs(inp, b):
    cT = np.stack([np.asarray(inp['c'][b]).reshape(8, 128).T, np.asarray(inp['c_ctx']).reshape(8, 128).T], axis=-1)
    m = {
        'x': np.ascontiguousarray(inp['x'][b]), 'ctx': np.ascontiguousarray(inp['ctx'][b]),
        'cT': np.ascontiguousarray(cT.reshape(128, 16)).astype(np.float32),
        'b_gates': np.asarray(inp['b_gates']).reshape(L, 16),
    }
    m['conv_wT'] = np.ascontiguousarray(np.asarray(inp['conv_w']).transpose(0, 2, 1))
    for k in ('w_ada', 'b_ada', 'g_norm1', 'w_in', 'g_hnorm', 'conv_b', 'conv_ln_g', 'conv_ln_b', 'w_out', 'g_norm2',
              'w_router', 'w_e_gate', 'w_e_up', 'w_e_down', 'g_final'):
        m[k] = np.asarray(inp[k])
    return m


def _phase2(self, l, h):
    P, CS = self.P, self.CS
    ident = CS[:, C_ID:C_ID + 128]; ones = CS[:, C_ONE:C_ONE + 128]
    triU = CS[:, C_TU:C_TU + 128]; triL = CS[:, C_TL:C_TL + 128]
    o = [0]

    def cv(n):
        a = self.carve(o[0], n); o[0] += n
        return a
    rawbuf = [[cv(T) for _ in range(3)] for _ in range(2)]
    raw = rawbuf[h % 2]
    rk = ['raw%d_%d' % (h % 2, j) for j in range(3)]
    colmaj = h >= 2
    scanbuf = [cv(T) for _ in range(3)]
    scan = scanbuf if colmaj else raw
    Q, Kt, Vt = scan
    g4 = cv(T); g4s_ = cv(T); g4s = g4s_ if colmaj else g4
    ktm = cv(NT * 128).rearrange("p (c d) -> p c d", c=NT)
    vp = [cv(NT * 130).rearrange("p (c d) -> p c d", c=NT) for _ in range(2)]
    H = [cv(NT * 128).rearrange("p (c d) -> p c d", c=NT) for _ in range(2)]
    oT = cv(T)
    YT = cv(T)
    bg = cv(4); ghn = cv(1)
    G = [cv(NT) for _ in range(4)]
    nlf = [cv(NT) for _ in range(2)]
    totS = [cv(NT) for _ in range(2)]
    dd = [cv(NT) for _ in range(2)]
    flo = [cv(NT) for _ in range(2)]
    wk = [cv(NT) for _ in range(2)]
    dec = [cv(NT) for _ in range(2)]
    Cst = [cv(130) for _ in range(2)]
    Cd2 = [[cv(130) for _ in range(2)] for _ in range(2)]
    STm2 = [[cv(128) for _ in range(2)] for _ in range(2)]
    dn2 = [[cv(2) for _ in range(2)] for _ in range(2)]
    ssq = cv(NT); rst = cv(NT); tmp = cv(NT * 128)
    for j, base in enumerate((0, 512, 1024)):
        P.dma('sp', raw[j], self.uT[base + h * 128: base + (h + 1) * 128, :], w=[rk[j]])
    P.dma('sp', g4[0:4, :], self.uT[1536 + 4 * h:1536 + 4 * h + 4, :], w=['g4'])
    P.dma('sp', oT, self.uT[1552 + h * 128:1552 + (h + 1) * 128, :], w=['oT'])
    P.dma('sp', bg, self.b_gates[l, 4 * h:4 * h + 4].partition_broadcast(128), w=['bg'])
    P.dma('sp', ghn, self.g_hnorm[l, h * 128:(h + 1) * 128].rearrange("(p o) -> p o", o=1), w=['ghn'])
    if colmaj:
        for j in range(3):
            eng = ('pool', 'dve', 'act')[j]
            s_, d_ = raw[j], scan[j]
            if eng == 'act':
                P.add(eng, lambda e, s_=s_, d_=d_: e.activation(out=d_[:, 0:TC], in_=s_[:, 0:TC], func=AF.Copy), r=[rk[j]], w=['scanA%d' % j])
                P.add(eng, lambda e, s_=s_, d_=d_: e.activation(out=d_[:, TC:].rearrange("p (c r) -> p c r", r=32),
                                                                in_=s_[:, TC:].rearrange("p (r c) -> p c r", c=64), func=AF.Copy),
                      r=[rk[j]], w=['scan%d' % j])
            else:
                P.add(eng, lambda e, s_=s_, d_=d_: e.tensor_copy(out=d_[:, 0:TC], in_=s_[:, 0:TC]), r=[rk[j]], w=['scanA%d' % j])
                P.add(eng, lambda e, s_=s_, d_=d_: e.tensor_copy(out=d_[:, TC:].rearrange("p (c r) -> p c r", r=32),
                                                                 in_=s_[:, TC:].rearrange("p (r c) -> p c r", c=64)),
                      r=[rk[j]], w=['scan%d' % j])
        P.add('pool', lambda e: e.tensor_copy(out=g4s[0:4, 0:TC], in_=g4[0:4, 0:TC]), r=['g4'], w=['g4sA'])
        P.add('pool', lambda e: e.tensor_copy(out=g4s[0:4, TC:].rearrange("p (c r) -> p c r", r=32),
                                              in_=g4[0:4, TC:].rearrange("p (r c) -> p c r", c=64)), r=['g4'], w=['g4s'])
        sk = [['scan%d' % j, 'scanA%d' % j] for j in range(3)]
        gk = ['g4s', 'g4sA']
    else:
        sk = [[rk[j]] for j in range(3)]
        gk = ['g4']
    pb = self.ps[0]
    for c in range(NT):
        P.add('pe', lambda e, c=c: e.transpose(pb[:, c * 4:(c + 1) * 4], g4s[0:4, c * 128:(c + 1) * 128], ident[0:4, 0:4]), r=gk + ['CS'], w=['ps0'])
    pv = pb[:, 0:NT * 4].rearrange("p (c g) -> p c g", g=4)
    for g in range(4):
        P.add('dve', lambda e, g=g: e.tensor_scalar(out=G[g], in0=pv[:, :, g], scalar1=bg[:, g:g + 1], scalar2=None, op0=ALU.add),
              r=['bg'], w=['ps0', 'G%d' % g])
    p1 = self.ps[1]
    for d in range(2):
        Fg = G[1 + 2 * d]; Ig = G[2 * d]
        P.add('act', lambda e, d=d, Fg=Fg: e.activation(out=nlf[d], in_=Fg, func=AF.Exp, scale=-1.0), r=['G%d' % (1 + 2 * d)], w=['nlf%d' % d])
        P.add('act', lambda e, d=d: e.activation(out=nlf[d], in_=nlf[d], func=AF.Ln, bias=CS[:, C_EPS + 1:C_EPS + 2]), w=['nlf%d' % d])
        tri = triU if d == 0 else triL
        P.add('pe', lambda e, d=d, tri=tri: e.matmul(p1[:, d * 64:d * 64 + NT], lhsT=tri, rhs=nlf[d], start=True, stop=True), r=['nlf%d' % d, 'CS'], w=['ps1'])
        P.add('pe', lambda e, d=d: e.matmul(p1[:, d * 64 + 32:d * 64 + 32 + NT], lhsT=ones, rhs=nlf[d], start=True, stop=True), r=['nlf%d' % d, 'CS'], w=['ps1'])
        P.add('act', lambda e, d=d: e.activation(out=totS[d], in_=p1[:, d * 64 + 32:d * 64 + 32 + NT], func=AF.Copy), w=['ps1', 'tot%d' % d])
        P.add('dve', lambda e, d=d: e.tensor_tensor(out=dd[d], in0=p1[:, d * 64:d * 64 + NT], in1=totS[d], op=ALU.subtract), r=['tot%d' % d], w=['ps1', 'dd%d' % d])
        P.add('act', lambda e, d=d: e.activation(out=flo[d], in_=dd[d], func=AF.Exp), r=['dd%d' % d], w=['flo%d' % d])
        P.add('dve', lambda e, d=d, Ig=Ig: e.tensor_tensor(out=wk[d], in0=dd[d], in1=Ig, op=ALU.add), r=['dd%d' % d, 'G%d' % (2 * d)], w=['wk%d' % d])
        P.add('act', lambda e, d=d: e.activation(out=wk[d], in_=wk[d], func=AF.Exp, bias=CS[:, C_EPS + 2:C_EPS + 3]), w=['wk%d' % d])
        P.add('act', lambda e, d=d: e.activation(out=dec[d], in_=totS[d], func=AF.Exp, scale=-1.0), r=['tot%d' % d], w=['dec%d' % d])
        P.add('pool', lambda e, d=d: e.memset(vp[d][:, :, 128:130], 0.0), w=['vpx%d' % d])
        P.add('pool', lambda e, d=d: e.memset(Cst[d], 0.0), w=['C%d' % d])
    for d in range(2):
        P.add('dve', lambda e, d=d: e.tensor_copy(out=vp[d][:, :, 128], in_=wk[d]), r=['wk%d' % d], w=['vpx%d' % d])
    nb = 0
    for c in range(NT):
        bank = 2 + nb % 6; nb += 1
        pk = self.ps[bank]
        P.add('pe', lambda e, c=c, pk=pk: e.transpose(pk[:, 0:128], Kt[:, c * 128:(c + 1) * 128], ident), r=sk[1] + ['CS'], w=['ps%d' % bank])
        P.add('pe', lambda e, c=c, pk=pk: e.transpose(pk[:, 128:256], Vt[:, c * 128:(c + 1) * 128], ident), r=sk[2] + ['CS'], w=['ps%d' % bank])
        P.add('act', lambda e, c=c, pk=pk: e.activation(out=ktm[:, c, :], in_=pk[:, 0:128], func=AF.Copy), w=['ps%d' % bank], cw=['ktm'])
        P.add('dve', lambda e, c=c, pk=pk: e.tensor_scalar(out=vp[0][:, c, 0:128], in0=pk[:, 128:256], scalar1=wk[0][:, c:c + 1], scalar2=None, op0=ALU.mult),
              r=['wk0'], w=['ps%d' % bank], cw=['vp0'])
        P.add('act', lambda e, c=c, pk=pk: e.activation(out=vp[1][:, c, 0:128], in_=pk[:, 128:256], func=AF.Copy, scale=wk[1][:, c:c + 1]),
              r=['wk1'], w=['ps%d' % bank], cw=['vp1'])
    order = [list(range(NT)), [1, 0] + list(range(NT - 1, 1, -1))]

    def front(step, d):
        c = order[d][step]
        cs = slice(c * 128, (c + 1) * 128)
        bST, bO, bC = self.ps[4 * d], self.ps[4 * d + 1 + step % 2], self.ps[4 * d + 3]
        kST, kO, kC = 'ps%d' % (4 * d), 'ps%d' % (4 * d + 1 + step % 2), 'ps%d' % (4 * d + 3)
        sp_ = step % 2
        stm = STm2[d][sp_]; stk = 'STm%d_%d' % (d, sp_)
        msk = triU if d == 0 else triL
        cdt = Cd2[d][sp_]; cdk = 'Cd%d_%d' % (d, sp_)
        P.add('pe', lambda e: e.matmul(bST[:, 0:128], lhsT=Kt[:, cs], rhs=Q[:, cs], start=True, stop=True), r=sk[0] + sk[1], w=[kST])
        P.add('pe', lambda e: e.matmul(bC[:, 0:130], lhsT=ktm[:, c, :], rhs=vp[d][:, c, :], start=True, stop=True),
              r=['ktm', 'vp%d' % d, 'vpx%d' % d], w=[kC])
        P.add('dve', lambda e: e.tensor_scalar(out=cdt, in0=Cst[d], scalar1=dec[d][:, c:c + 1], scalar2=None, op0=ALU.mult),
              r=['C%d' % d, 'dec%d' % d], w=[cdk])
        P.add('dve', lambda e: e.tensor_tensor(out=stm, in0=bST[:, 0:128], in1=msk, op=ALU.mult), r=['CS'], w=[kST, stk])
        P.add('dve', lambda e: e.tensor_tensor(out=Cst[d], in0=bC[:, 0:130], in1=cdt, op=ALU.add), r=[cdk], w=[kC, 'C%d' % d])
        P.add('pe', lambda e: e.matmul(bO[:, 0:130], lhsT=stm, rhs=vp[d][:, c, :], start=True, stop=False),
              r=[stk, 'vp%d' % d, 'vpx%d' % d], w=[kO])
        P.add('pe', lambda e: e.matmul(bO[:, 0:130], lhsT=Q[:, cs], rhs=cdt, start=False, stop=True), r=sk[0] + [cdk], w=[kO])

    def back(step, d):
        c = order[d][step]
        bO = self.ps[4 * d + 1 + step % 2]; kO = 'ps%d' % (4 * d + 1 + step % 2)
        sp_ = step % 2
        dn_ = dn2[d][sp_]; dnk = 'dn%d_%d' % (d, sp_)
        P.add('act', lambda e: e.activation(out=dn_[:, 0:1], in_=bO[:, 128:129], func=AF.Abs), w=[kO, dnk])
        P.add('dve', lambda e: e.tensor_tensor(out=dn_[:, 0:1], in0=dn_[:, 0:1], in1=flo[d][:, c:c + 1], op=ALU.max), r=['flo%d' % d], w=[dnk])
        P.add('dve', lambda e: e.reciprocal(out=dn_[:, 1:2], in_=dn_[:, 0:1]), w=[dnk])
        P.add('act', lambda e: e.activation(out=H[d][:, c, :], in_=bO[:, 0:128], func=AF.Copy, scale=dn_[:, 1:2]), r=[dnk], w=[kO], cw=['H%d' % d])

    for step in range(NT):
        for d in range(2):
            front(step, d)
        if step > 0:
            for d in range(2):
                back(step - 1, d)
    for d in range(2):
        back(NT - 1, d)
    Hf = H[0][:, :, :].rearrange("p c d -> p (c d)"); Hb = H[1][:, :, :].rearrange("p c d -> p (c d)")
    P.add('pool', lambda e: e.tensor_tensor(out=Hf, in0=Hf, in1=Hb, op=ALU.add), r=['H1'], w=['H0'])
    P.add('dve', lambda e: e.tensor_tensor(out=tmp, in0=Hf, in1=Hf, op=ALU.mult), r=['H0'], w=['tmp'])
    P.add('dve', lambda e: e.tensor_reduce(out=ssq, in_=tmp.rearrange("p (c d) -> p c d", c=NT), axis=AX.X, op=ALU.add), r=['tmp'], w=['ssq'])
    P.add('act', lambda e: e.activation(out=rst, in_=ssq, func=AF.Sqrt, scale=1.0 / 128, bias=CS[:, C_EPS:C_EPS + 1]), r=['ssq'], w=['rst'])
    P.add('dve', lambda e: e.reciprocal(out=rst, in_=rst), w=['rst'])
    P.add('act', lambda e: e.activation(out=oT, in_=oT, func=AF.Sigmoid), w=['oT'])
    for c in range(NT):
        P.add('act', lambda e, c=c: e.activation(out=H[1][:, c, :], in_=H[0][:, c, :], func=AF.Copy, scale=rst[:, c:c + 1]), r=['H0', 'rst'], cw=['H1'])
    for c in range(NT):
        bank = c % 8
        pk = self.ps[bank]
        P.add('pe', lambda e, c=c, pk=pk: e.transpose(pk[:, 0:128], H[1][:, c, :], ident), r=['H1', 'CS'], w=['ps%d' % bank])
        if colmaj and c >= 2:
            cl = c - 2
            dst = YT[:, TC:].rearrange("p (r c) -> p c r", c=64)[:, 4 * cl:4 * cl + 4, :]
            srcp = pk[:, 0:128].rearrange("p (c r) -> p c r", r=32)
        else:
            dst = YT[:, c * 128:(c + 1) * 128]
            srcp = pk[:, 0:128]
        P.add('dve', lambda e, dst=dst, srcp=srcp: e.tensor_scalar(out=dst, in0=srcp, scalar1=ghn[:, 0:1], scalar2=None, op0=ALU.mult),
              r=['ghn'], w=['ps%d' % bank], cw=['YT'])
    P.add('pool', lambda e: e.tensor_tensor(out=YT, in0=YT, in1=oT, op=ALU.add if False else ALU.mult), r=['oT'], w=['YT'])
    P.dma('sp', self.yT[h * 128:(h + 1) * 128, :], YT, r=['YT'], cw=['yTd'])
    if h == 3:
        P.barrier()


K.phase2 = _phase2


def _phase34(self, l):
    P, CS = self.P, self.CS
    ones = CS[:, C_ONE:C_ONE + 128]
    o = [0]

    def cv(n):
        a = self.carve(o[0], n); o[0] += n
        return a
    WA = 2334
    cacg = [cv(2 * T) for _ in range(2)]
    ca = [cacg[j][:, 0:T] for j in range(2)]; cg = [cacg[j][:, T:2 * T] for j in range(2)]
    ypad = [cv(2364) for _ in range(2)]; acc = [cv(WA) for _ in range(2)]
    sqt = [cacg[j][:, 0:WA] for j in range(2)]
    ptmp = cv(WA)
    cw = [cv(31) for _ in range(2)]; cb = [cv(1) for _ in range(2)]; lg = [cv(1) for _ in range(2)]; lb = [cv(1) for _ in range(2)]
    mt = [cv(512) for _ in range(2)]; vt = [cv(512) for _ in range(2)]
    col = lambda v, j: v[l, j * 128:(j + 1) * 128].rearrange("(p o) -> p o", o=1)
    for j in range(2):
        P.dma('sp', ca[j], self.uT[2064 + j * 128:2064 + (j + 1) * 128, :], w=['ca%d' % j])
        P.dma('sp', cg[j], self.uT[2320 + j * 128:2320 + (j + 1) * 128, :], w=['cg%d' % j])
        P.dma('sp', cw[j], self.conv_wT[l, j * 128:(j + 1) * 128, :], w=['cw%d' % j])
        P.dma('sp', cb[j], col(self.conv_b, j), w=['cb%d' % j])
        P.dma('sp', lg[j], col(self.conv_ln_g, j), w=['lg%d' % j])
        P.dma('sp', lb[j], col(self.conv_ln_b, j), w=['lb%d' % j])
        P.add('pool', lambda e, j=j: e.memset(ypad[j], 0.0), w=['yp%d' % j])
        P.add('act', lambda e, j=j: e.activation(out=cg[j], in_=cg[j], func=AF.Sigmoid), w=['cg%d' % j])
        P.add('dve', lambda e, j=j: e.tensor_tensor(out=ypad[j][:, 15:15 + TC], in0=ca[j][:, 0:TC], in1=cg[j][:, 0:TC], op=ALU.mult),
              r=['ca%d' % j, 'cg%d' % j], w=['yp%d' % j])
        P.add('dve', lambda e, j=j: e.tensor_tensor(out=ypad[j][:, 301:301 + TL], in0=ca[j][:, TC:], in1=cg[j][:, TC:], op=ALU.mult),
              r=['ca%d' % j, 'cg%d' % j], w=['yp%d' % j])
    BD = CS[:, C_BD:C_BD + 256]
    DCc = CS[:, C_DC:C_DC + 512].rearrange("p (k n) -> p k n", k=2)
    DSc = CS[:, C_DC + 512:C_DC + 1024].rearrange("p (k n) -> p k n", k=2)
    fr = [cv(T) for _ in range(2)]
    Z = [cv(NT * 256).rearrange("p (c n) -> p c n", c=NT) for _ in range(2)]
    YF = fr
    DW = 128
    dcb = [cv(16 * DW).rearrange("p (k n) -> p k n", k=16) for _ in range(2)]
    dsb = [cv(16 * DW).rearrange("p (k n) -> p k n", k=16) for _ in range(2)]
    sc_c = 1.0 / 128.0
    sc_l = float(1.0 / np.sqrt(TL * 64.0))
    nb = 0
    for j in range(2):
        P.dma('sp', fr[j], self.uT[2576 + j * 128:2576 + (j + 1) * 128, :], w=['fr%d' % j])
        for i in range(NT):
            bank = 2 + nb % 6; nb += 1
            pb = self.ps[bank]
            s = sc_c if i < 2 else sc_l
            P.add('pe', lambda e, j=j, i=i, pb=pb: e.matmul(pb[:, 0:256], lhsT=fr[j][:, i * 128:(i + 1) * 128], rhs=BD, start=True, stop=True),
                  r=['fr%d' % j, 'CS'], w=['ps%d' % bank])
            P.add('act', lambda e, j=j, i=i, pb=pb, s=s: e.activation(out=Z[j][:, i, 0:128], in_=pb[:, 0:128], func=AF.Copy, scale=s),
                  w=['ps%d' % bank], cw=['Z%d' % j])
            P.add('act', lambda e, j=j, i=i, pb=pb, s=s: e.activation(out=Z[j][:, i, 128:256], in_=pb[:, 128:256], func=AF.Copy, scale=-s),
                  w=['ps%d' % bank], cw=['Z%d' % j])
    for j in range(2):
        bank = 2 + nb % 6; nb += 1
        pb = self.ps[bank]
        n = 0
        for i in range(2):
            for part, M in ((0, DCc), (1, DSc)):
                P.add('pe', lambda e, j=j, i=i, part=part, M=M, pb=pb, n=n: e.matmul(pb[:, 0:256], lhsT=Z[j][:, i, part * 128:(part + 1) * 128], rhs=M[:, i, :],
                                                                                 start=(n == 0), stop=(n == 3)), r=['Z%d' % j, 'CS'], w=['ps%d' % bank])
                n += 1
        P.add('act', lambda e, j=j, pb=pb: e.activation(out=YF[j][:, 0:TC], in_=pb[:, 0:256], func=AF.Copy), w=['ps%d' % bank], cw=['fr%d' % j])
    for tc in range(TL // DW):
        b = tc % 2
        cols = slice(tc * DW, (tc + 1) * DW)
        P.dma('sp', dcb[b], self.dftc[:, cols].rearrange("(k p) n -> p k n", p=128), w=['dcb%d' % b])
        P.dma('sp', dsb[b], self.dfts[:, cols].rearrange("(k p) n -> p k n", p=128), w=['dsb%d' % b])
        for j in range(2):
            bank = 2 + nb % 6; nb += 1
            pb = self.ps[bank]
            n = 0
            for i in range(16):
                for part, M, mk in ((0, dcb[b], 'dcb%d' % b), (1, dsb[b], 'dsb%d' % b)):
                    P.add('pe', lambda e, j=j, i=i, part=part, M=M, pb=pb, n=n: e.matmul(pb[:, 0:DW], lhsT=Z[j][:, i + 2, part * 128:(part + 1) * 128], rhs=M[:, i, :],
                                                                                     start=(n == 0), stop=(n == 31)), r=['Z%d' % j, mk], w=['ps%d' % bank])
                    n += 1
            P.add('act', lambda e, j=j, pb=pb, tc=tc: e.activation(out=YF[j][:, TC + tc * DW:TC + (tc + 1) * DW], in_=pb[:, 0:DW], func=AF.Copy),
                  w=['ps%d' % bank], cw=['fr%d' % j])
    for j in range(2):
        P.dma('sp', self.yT[768 + j * 128:768 + (j + 1) * 128, :], YF[j], r=['fr%d' % j], cw=['yTd'])


    for k in range(31):
        j = 0
        if k == 0:
            P.add('dve', lambda e, j=j: e.tensor_scalar(out=acc[j], in0=ypad[j][:, 0:WA], scalar1=cw[j][:, 0:1], scalar2=cb[j][:, 0:1], op0=ALU.mult, op1=ALU.add),
                  r=['yp0', 'cw0', 'cb0'], w=['acc0'])
        else:
            P.add('dve', lambda e, j=j, k=k: e.scalar_tensor_tensor(out=acc[j], in0=ypad[j][:, k:k + WA], scalar=cw[j][:, k:k + 1], in1=acc[j],
                                                                    op0=ALU.mult, op1=ALU.add), r=['yp0', 'cw0'], w=['acc0'])
        j = 1
        if k == 0:
            P.add('pool', lambda e, j=j: e.tensor_scalar(out=acc[j], in0=ypad[j][:, 0:WA], scalar1=cw[j][:, 0:1], scalar2=cb[j][:, 0:1], op0=ALU.mult, op1=ALU.add),
                  r=['yp1', 'cw1', 'cb1'], w=['acc1'])
        else:
            P.add('pool', lambda e, j=j, k=k: e.tensor_scalar(out=ptmp, in0=ypad[j][:, k:k + WA], scalar1=cw[j][:, k:k + 1], scalar2=0.0, op0=ALU.mult, op1=ALU.add),
                  r=['yp1', 'cw1'], w=['ptmp'])
            P.add('pool', lambda e, j=j: e.tensor_tensor(out=acc[j], in0=acc[j], in1=ptmp, op=ALU.add), w=['acc1', 'ptmp'])
    for j in range(2):
        P.add('act', lambda e, j=j: e.activation(out=sqt[j], in_=acc[j], func=AF.Square), r=['acc%d' % j], w=['sq%d' % j, 'ca%d' % j, 'cg%d' % j])
    chunks = [(0, 512), (512, 512), (1024, 512), (1536, 512), (2048, 286)]
    for ci, (a0, n) in enumerate(chunks):
        b1, b2 = self.ps[0], self.ps[1]
        k1, k2 = 'ps0', 'ps1'
        m_, v_ = mt[ci % 2], vt[ci % 2]
        mk, vk = 'mt%d' % (ci % 2), 'vt%d' % (ci % 2)
        for j in range(2):
            P.add('pe', lambda e, j=j, b1=b1, a0=a0, n=n: e.matmul(b1[:, 0:n], lhsT=ones, rhs=acc[j][:, a0:a0 + n], start=(j == 0), stop=(j == 1)),
                  r=['acc%d' % j, 'CS'], w=[k1])
        for j in range(2):
            P.add('pe', lambda e, j=j, b2=b2, a0=a0, n=n: e.matmul(b2[:, 0:n], lhsT=ones, rhs=sqt[j][:, a0:a0 + n], start=(j == 0), stop=(j == 1)),
                  r=['sq%d' % j, 'CS'], w=[k2])
        P.add('act', lambda e, b1=b1, m_=m_, n=n: e.activation(out=m_[:, 0:n], in_=b1[:, 0:n], func=AF.Copy, scale=1.0 / 256), w=[k1, mk])
        P.add('dve', lambda e, m_=m_, v_=v_, n=n: e.tensor_tensor(out=v_[:, 0:n], in0=m_[:, 0:n], in1=m_[:, 0:n], op=ALU.mult), r=[mk], w=[vk])
        P.add('dve', lambda e, b2=b2, v_=v_, n=n: e.scalar_tensor_tensor(out=v_[:, 0:n], in0=b2[:, 0:n], scalar=1.0 / 256, in1=v_[:, 0:n],
                                                                         op0=ALU.mult, op1=ALU.subtract), w=[k2, vk])
        P.add('act', lambda e, v_=v_, n=n: e.activation(out=v_[:, 0:n], in_=v_[:, 0:n], func=AF.Sqrt, bias=CS[:, C_EPS:C_EPS + 1]), w=[vk])
        P.add('dve', lambda e, v_=v_, n=n: e.reciprocal(out=v_[:, 0:n], in_=v_[:, 0:n]), w=[vk])
        for j in range(2):
            eng = 'dve' if j == 0 else 'pool'
            P.add(eng, lambda e, j=j, m_=m_, a0=a0, n=n: e.tensor_tensor(out=acc[j][:, a0:a0 + n], in0=acc[j][:, a0:a0 + n], in1=m_[:, 0:n], op=ALU.subtract),
                  r=[mk], w=['acc%d' % j])
            P.add(eng, lambda e, j=j, v_=v_, a0=a0, n=n: e.tensor_tensor(out=acc[j][:, a0:a0 + n], in0=acc[j][:, a0:a0 + n], in1=v_[:, 0:n], op=ALU.mult),
                  r=[vk], w=['acc%d' % j])
            P.add('act', lambda e, j=j, a0=a0, n=n: e.activation(out=acc[j][:, a0:a0 + n], in_=acc[j][:, a0:a0 + n], func=AF.Silu,
                                                                 scale=lg[j][:, 0:1], bias=lb[j][:, 0:1]), r=['lg%d' % j, 'lb%d' % j], w=['acc%d' % j])
    for j in range(2):
        P.dma('sp', self.yT[512 + j * 128:512 + (j + 1) * 128, 0:TC], acc[j][:, 0:TC], r=['acc%d' % j], cw=['yTd'])
        P.dma('sp', self.yT[512 + j * 128:512 + (j + 1) * 128, TC:T], acc[j][:, 286:286 + TL], r=['acc%d' % j], cw=['yTd'])
    P.barrier()


def _phase5(self, l):
    P, CS = self.P, self.CS
    o = [0]

    def cv(n):
        a = self.carve(o[0], n); o[0] += n
        return a
    YA = cv(8 * T).rearrange("p (k t) -> p k t", k=8)
    wo = [cv(4096).rearrange("p (k n) -> p k n", k=8) for _ in range(2)]
    gt = {'l': cv(1024), 'c': cv(1024)}
    xt = [cv(1024) for _ in range(2)]
    tm = [cv(1024) for _ in range(2)]
    for kc in range(8):
        P.dma('sp' if kc % 2 == 0 else 'pool', YA[:, kc, :], self.yT[kc * 128:(kc + 1) * 128, :], w=['YA%d' % kc])
    for hf in range(2):
        P.dma('sp', wo[hf], self.w_out[l, :, hf * 512:(hf + 1) * 512].rearrange("(kc p) n -> p kc n", p=128), w=['wo%d' % hf])
    P.dma('sp', gt['l'], self.modrows[l, 2, 0, :].partition_broadcast(128), w=['gtl'])
    P.dma('sp', gt['c'], self.modrows[l, 2, 1, :].partition_broadcast(128), w=['gtc'])
    nb = 0
    for i in range(NT):
        b = i % 2
        isc = i < 2
        if l == 0:
            src = self.ctx_in[i * 128:(i + 1) * 128, :] if isc else self.x_in[(i - 2) * 128:(i - 1) * 128, :]
        else:
            src = self.xres[i * 128:(i + 1) * 128, :]
        P.dma('sp', xt[b], src, w=['xt%d' % b])
        g_ = gt['c' if isc else 'l']; gk = 'gtc' if isc else 'gtl'
        for hf in range(2):
            bank = nb % 8; nb += 1
            pb = self.ps[bank]
            for kc in range(8):
                P.add('pe', lambda e, i=i, kc=kc, hf=hf, pb=pb: e.matmul(pb[:, 0:512], lhsT=YA[:, kc, i * 128:(i + 1) * 128], rhs=wo[hf][:, kc, :],
                                                                       start=(kc == 0), stop=(kc == 7)), r=['YA%d' % kc, 'wo%d' % hf], w=['ps%d' % bank])
            P.add('dve', lambda e, b=b, hf=hf, pb=pb, g_=g_: e.tensor_tensor(out=tm[b][:, hf * 512:(hf + 1) * 512], in0=pb[:, 0:512],
                                                                            in1=g_[:, hf * 512:(hf + 1) * 512], op=ALU.mult), r=[gk], w=['ps%d' % bank, 'tm%d' % b])
        P.add('pool', lambda e, b=b: e.tensor_tensor(out=tm[b], in0=tm[b], in1=xt[b], op=ALU.add), r=['xt%d' % b], w=['tm%d' % b])
        P.dma('pool', self.xres[i * 128:(i + 1) * 128, :], tm[b], r=['tm%d' % b], cw=['xres'])
    P.barrier()


K.phase34 = _phase34
K.phase5 = _phase5


BIG = 8192.0
AWTOP = 50600 - 288
C_EOFF, C_CAPT, C_KCAP, C_TOK = NCONST, NCONST + 288, NCONST + 576, NCONST + 578
NCONST2 = NCONST + 578 + 18


def make_consts2():
    c = np.zeros((128, NCONST2), np.float32)
    c[:, :NCONST] = make_consts()
    eoff = np.zeros((18, 16), np.float32); capt = np.zeros((18, 16), np.float32)
    for i in range(18):
        for e in range(16):
            eoff[i, e] = e * NSLOT + (0 if i < 2 else CAPC)
            capt[i, e] = CAPC if i < 2 else CAPL
    c[:, C_EOFF:C_EOFF + 288] = eoff.reshape(1, 288)
    c[:, C_CAPT:C_CAPT + 288] = capt.reshape(1, 288)
    c[:, C_KCAP] = CAPC; c[:, C_KCAP + 1] = CAPL
    tok = (np.arange(18)[None, :] * 128 + np.arange(128)[:, None]).astype(np.int32)
    c[:, C_TOK:C_TOK + 18] = tok.view(np.float32)
    return c


def _phase56(self, l):
    P, CS = self.P, self.CS
    ident = CS[:, C_ID:C_ID + 128]
    o = [0]

    def cv(n):
        a = self.carve(o[0], n); o[0] += n
        return a
    aff = cv(288).rearrange("p (c e) -> p c e", c=NT)
    YA = cv(8 * T).rearrange("p (k t) -> p k t", k=8)
    wo = [cv(4096).rearrange("p (k n) -> p k n", k=8) for _ in range(2)]
    gt = {'l': cv(1024), 'c': cv(1024)}
    NB5 = 3
    xt = [cv(1024) for _ in range(NB5)]
    tm = [cv(1024) for _ in range(NB5)]
    modt = {}
    for nm, seg, row in (('gs_l', 4, 0), ('sh_l', 3, 0), ('gs_c', 4, 1), ('sh_c', 3, 1)):
        modt[nm] = cv(1024)
        P.dma('sp', modt[nm], self.modrows[l, seg, row, :].partition_broadcast(128), w=[nm])
    wr = cv(128).rearrange("p (k n) -> p k n", k=8)
    P.dma('sp', wr, self.w_router[l, :, :].rearrange("(kc p) n -> p kc n", p=128), w=['wr'])
    zt = cv(1024)
    P.add('pool', lambda e: e.memset(zt, 0.0), w=['zt'])
    for i in range(NT):
        P.dma('pool', self.macc[i * 128:(i + 1) * 128, :], zt, r=['zt'], cw=['macc'])
    xr = [cv(1042) for _ in range(2)]
    xT = [cv(1024).rearrange("p (k t) -> p k t", k=8) for _ in range(2)]
    sq = cv(1024)
    st = [cv(8) for _ in range(2)]
    ex = cv(16)
    for kc in range(8):
        P.dma('sp' if kc % 2 == 0 else 'pool', YA[:, kc, :], self.yT[kc * 128:(kc + 1) * 128, :], w=['YA%d' % kc])
    for hf in range(2):
        P.dma('sp', wo[hf], self.w_out[l, :, hf * 512:(hf + 1) * 512].rearrange("(kc p) n -> p kc n", p=128), w=['wo%d' % hf])
    P.dma('sp', gt['l'], self.modrows[l, 2, 0, :].partition_broadcast(128), w=['gtl'])
    P.dma('sp', gt['c'], self.modrows[l, 2, 1, :].partition_broadcast(128), w=['gtc'])
    nbc = [0]

    abanks = {}

    def stageA_pe(i):
        b = i % NB5
        isc = i < 2
        if l == 0:
            src = self.ctx_in[i * 128:(i + 1) * 128, :] if isc else self.x_in[(i - 2) * 128:(i - 1) * 128, :]
        else:
            src = self.xres[i * 128:(i + 1) * 128, :]
        P.dma('sp', xt[b], src, w=['xt%d' % b])
        abanks[i] = []
        for hf in range(2):
            bank = 4 + nbc[0] % 4; nbc[0] += 1
            abanks[i].append(bank)
            pb = self.ps[bank]
            for kc in range(8):
                P.add('pe', lambda e, i=i, kc=kc, hf=hf, pb=pb: e.matmul(pb[:, 0:512], lhsT=YA[:, kc, i * 128:(i + 1) * 128], rhs=wo[hf][:, kc, :],
                                                                       start=(kc == 0), stop=(kc == 7)), r=['YA%d' % kc, 'wo%d' % hf], w=['ps%d' % bank])

    def stageA_post(i):
        b = i % NB5
        isc = i < 2
        g_ = gt['c' if isc else 'l']; gk = 'gtc' if isc else 'gtl'
        for hf in range(2):
            bank = abanks[i][hf]
            pb = self.ps[bank]
            P.add('dve', lambda e, b=b, hf=hf, pb=pb, g_=g_: e.tensor_tensor(out=tm[b][:, hf * 512:(hf + 1) * 512], in0=pb[:, 0:512],
                                                                            in1=g_[:, hf * 512:(hf + 1) * 512], op=ALU.mult), r=[gk], w=['ps%d' % bank, 'tm%d' % b])
        P.add('pool', lambda e, b=b: e.tensor_tensor(out=tm[b], in0=tm[b], in1=xt[b], op=ALU.add), r=['xt%d' % b], w=['tm%d' % b])
        P.dma('pool', self.xres[i * 128:(i + 1) * 128, :], tm[b], r=['tm%d' % b], cw=['xres'])

    def stageB1(i):
        b = i % 2
        bt = i % NB5
        isc = i < 2
        x_, xr_, st_, xT_ = tm[bt], xr[b], st[b], xT[b]
        xk = 'tm%d' % bt
        P.dma('pool', xr_[:, 1040:1041], self.consts[:, C_TOK + i:C_TOK + i + 1], cw=['xrk%d' % b], allow_slow_non_contiguous=True)
        P.add('act', lambda e, x_=x_, st_=st_: e.activation(out=sq, in_=x_, func=AF.Square, accum_out=st_[:, 0:1]), r=[xk], w=['sq', 'st%d' % b])
        P.add('act', lambda e, st_=st_: e.activation(out=st_[:, 1:2], in_=st_[:, 0:1], func=AF.Sqrt, scale=1.0 / D, bias=CS[:, C_EPS:C_EPS + 1]), w=['st%d' % b])
        P.add('dve', lambda e, st_=st_: e.reciprocal(out=st_[:, 2:3], in_=st_[:, 1:2]), w=['st%d' % b])
        gs = modt['gs_c' if isc else 'gs_l']; sh = modt['sh_c' if isc else 'sh_l']
        gk2 = 'gs_c' if isc else 'gs_l'; sk2 = 'sh_c' if isc else 'sh_l'
        P.add('dve', lambda e, x_=x_, xr_=xr_, st_=st_, gs=gs: e.scalar_tensor_tensor(out=xr_[:, 0:1024], in0=x_, scalar=st_[:, 2:3], in1=gs, op0=ALU.mult, op1=ALU.mult),
              r=[xk, 'st%d' % b, gk2], w=['xr%d' % b])
        P.add('pool', lambda e, xr_=xr_, sh=sh: e.tensor_tensor(out=xr_[:, 0:1024], in0=xr_[:, 0:1024], in1=sh, op=ALU.add), r=[sk2], w=['xr%d' % b])
        for hb in range(2):
            pb = self.ps[hb]
            for k4 in range(4):
                kc = hb * 4 + k4
                P.add('pe', lambda e, pb=pb, k4=k4, kc=kc, xr_=xr_: e.transpose(pb[:, k4 * 128:(k4 + 1) * 128], xr_[:, kc * 128:(kc + 1) * 128], ident),
                      r=['xr%d' % b, 'CS'], w=['ps%d' % hb])
            dst = xT_[:, hb * 4:hb * 4 + 4, :]
            srcp = pb[:, :].rearrange("p (k t) -> p k t", k=4)
            if hb == 0:
                P.add('act', lambda e, dst=dst, srcp=srcp: e.activation(out=dst, in_=srcp, func=AF.Copy), w=['ps0'], cw=['xT%d' % b])
            else:
                P.add('dve', lambda e, dst=dst, srcp=srcp: e.tensor_copy(out=dst, in_=srcp), w=['ps1'], cw=['xT%d' % b])
        p2 = self.ps[2 + b]
        for kc in range(8):
            P.add('pe', lambda e, kc=kc, p2=p2, xT_=xT_: e.matmul(p2[:, 0:16], lhsT=xT_[:, kc, :], rhs=wr[:, kc, :], start=(kc == 0), stop=(kc == 7)),
                  r=['xT%d' % b, 'wr'], w=['ps%d' % (2 + b)])
    def stageB2(i):
        b = i % 2
        xr_, st_ = xr[b], st[b]
        p2 = self.ps[2 + b]
        P.add('dve', lambda e, p2=p2, st_=st_: e.tensor_reduce(out=st_[:, 3:4], in_=p2[:, 0:16], axis=AX.X, op=ALU.max), w=['ps%d' % (2 + b), 'st%d' % b])
        P.add('dve', lambda e, st_=st_: e.tensor_scalar(out=st_[:, 4:5], in0=st_[:, 3:4], scalar1=-1.0, scalar2=None, op0=ALU.mult), w=['st%d' % b])
        P.add('act', lambda e, p2=p2, st_=st_: e.activation(out=ex, in_=p2[:, 0:16], func=AF.Exp, bias=st_[:, 4:5], accum_out=st_[:, 5:6]),
              w=['ps%d' % (2 + b), 'st%d' % b, 'ex'])
        P.add('dve', lambda e, st_=st_: e.reciprocal(out=st_[:, 6:7], in_=st_[:, 5:6]), w=['st%d' % b])
        P.add('dve', lambda e, i=i, st_=st_: e.tensor_scalar(out=aff[:, i, :], in0=ex, scalar1=st_[:, 6:7], scalar2=None, op0=ALU.mult),
              r=['st%d' % b], w=['ex'], cw=['aff'])
        P.add('pool', lambda e, i=i, xr_=xr_: e.tensor_copy(out=xr_[:, 1024:1040], in_=aff[:, i, :]), r=['aff'], cw=['xrk%d' % b])
        P.dma('pool', self.xn2x[i * 128:(i + 1) * 128, :], xr_, r=['xr%d' % b, 'xrk%d' % b], cw=['xn2x'])
    blocks = list(range(NT))
    if l == L - 1:
        blocks = list(range(2, NT))
        P.add('dve', lambda e: e.memset(aff[:, 0:2, :], 0.0), cw=['aff'])
    stageA_pe(blocks[0])
    stageA_post(blocks[0])
    for j, i in enumerate(blocks):
        nxt = blocks[j + 1] if j + 1 < len(blocks) else None
        if nxt is not None:
            stageA_pe(nxt)
        stageB1(i)
        if nxt is not None:
            stageA_post(nxt)
        stageB2(i)
    P.barrier()


def _phase6b(self, l):
    P, CS = self.P, self.CS
    ident = CS[:, C_ID:C_ID + 128]; ones = CS[:, C_ONE:C_ONE + 128]; triS = CS[:, C_TS:C_TS + 128]
    o = [0]

    def cv(n):
        a = self.carve(o[0], n); o[0] += n
        return a
    aff = cv(288).rearrange("p (c e) -> p c e", c=NT)
    affT = cv(T)
    for i in range(NT):
        bank = 4 + (i // 4) % 4
        pb = self.ps[bank]
        P.add('pe', lambda e, i=i, pb=pb: e.transpose(pb[0:16, (i % 4) * 128:(i % 4 + 1) * 128], aff[:, i, :], ident), r=['aff', 'CS'], w=['ps%d' % bank])
        if i % 4 == 3 or i == NT - 1:
            i0 = (i // 4) * 4
            n = (i - i0 + 1) * 128
            P.add('act', lambda e, pb=pb, i0=i0, n=n: e.activation(out=affT[0:16, i0 * 128:i0 * 128 + n], in_=pb[0:16, 0:n], func=AF.Copy),
                  w=['ps%d' % bank], cw=['affT'])
    lo = cv(2); mid = cv(2); cnt = cv(2); ge = cv(2); junk = cv(TL)
    kcap = CS[0:16, C_KCAP:C_KCAP + 2]
    P.add('dve', lambda e: e.memset(lo[0:16, :], 0.0), w=['lo'])
    segs = [(0, TC), (TC, TL)]
    bg_steps = []
    LAG = 3
    for it in range(1, 31):
        wv = float(2.0 ** (-it))
        k_ = it - 1
        if k_ < len(bg_steps):
            bg_steps[k_][0]()
        if 0 <= k_ - LAG < len(bg_steps) and bg_steps[k_ - LAG][1] is not None:
            bg_steps[k_ - LAG][1]()
        P.add('dve', lambda e, wv=wv: e.tensor_scalar(out=mid[0:16, :], in0=lo[0:16, :], scalar1=wv, scalar2=None, op0=ALU.add), r=['lo'], w=['mid'])
        for s, (a0, n) in enumerate(segs):
            P.add('dve', lambda e, s=s, a0=a0, n=n: e.tensor_scalar(out=junk[0:16, 0:n], in0=affT[0:16, a0:a0 + n], scalar1=mid[0:16, s:s + 1], scalar2=None,
                                                                    op0=ALU.is_ge, op1=ALU.add, accum_out=cnt[0:16, s:s + 1]),
                  r=['affT', 'mid'], w=['junk', 'cnt'])
        P.add('dve', lambda e: e.tensor_tensor(out=ge[0:16, :], in0=cnt[0:16, :], in1=kcap, op=ALU.is_ge), r=['cnt', 'CS'], w=['ge'])
        P.add('dve', lambda e, wv=wv: e.scalar_tensor_tensor(out=lo[0:16, :], in0=ge[0:16, :], scalar=wv, in1=lo[0:16, :], op0=ALU.mult, op1=ALU.add),
              r=['ge'], w=['lo'])
    for k_ in range(30 - LAG, len(bg_steps)):
        if k_ >= 0 and bg_steps[k_][1] is not None:
            bg_steps[k_][1]()
    Dg = cv(32); thb = cv(32)
    for s in range(2):
        P.add('dve', lambda e, s=s: e.tensor_scalar(out=Dg[0:16, s * 16:(s + 1) * 16], in0=ident[0:16, 0:16], scalar1=lo[0:16, s:s + 1], scalar2=None, op0=ALU.mult),
              r=['lo', 'CS'], w=['Dg'])
    p0 = self.ps[0]
    P.add('pe', lambda e: e.matmul(p0[:, 0:32], lhsT=ones[0:16, :], rhs=Dg[0:16, :], start=True, stop=True), r=['Dg', 'CS'], w=['ps0'])
    P.add('act', lambda e: e.activation(out=thb, in_=p0[:, 0:32], func=AF.Copy), w=['ps0', 'thb'])
    mask = cv(288).rearrange("p (c e) -> p c e", c=NT)
    for i in range(NT):
        s = 0 if i < 2 else 1
        if i < 2 and l == L - 1:
            P.add('dve', lambda e, i=i: e.memset(mask[:, i, :], 0.0), cw=['mask'])
            continue
        P.add('dve', lambda e, i=i, s=s: e.tensor_tensor(out=mask[:, i, :], in0=aff[:, i, :], in1=thb[:, s * 16:(s + 1) * 16], op=ALU.is_ge),
              r=['aff', 'thb'], cw=['mask'])
    maskf = mask[:, :, :].rearrange("p c e -> p (c e)")
    p1 = self.ps[1]; p2 = self.ps[2]
    P.add('pe', lambda e: e.matmul(p1[:, 0:288], lhsT=triS, rhs=maskf, start=True, stop=True), r=['mask', 'CS'], w=['ps1'])
    P.add('pe', lambda e: e.matmul(p2[:, 0:288], lhsT=ones, rhs=maskf, start=True, stop=True), r=['mask', 'CS'], w=['ps2'])
    tot = cv(288).rearrange("p (c e) -> p c e", c=NT)
    base = cv(288).rearrange("p (c e) -> p c e", c=NT)
    P.add('act', lambda e: e.activation(out=tot[:, :, :].rearrange("p c e -> p (c e)"), in_=p2[:, 0:288], func=AF.Copy), w=['ps2', 'tot'])
    P.add('dve', lambda e: e.memset(base[:, :, :].rearrange("p c e -> p (c e)"), 0.0), w=['base'])
    P.add('dve', lambda e: e.tensor_copy(out=base[:, 1, :], in_=tot[:, 0, :]), r=['tot'], w=['base'])
    for i in range(3, NT):
        P.add('dve', lambda e, i=i: e.tensor_tensor(out=base[:, i, :], in0=base[:, i - 1, :], in1=tot[:, i - 1, :], op=ALU.add), r=['tot'], w=['base'])
    rank = cv(288); val = cv(288)
    idx = self.carve(AWTOP, 288)
    basef = base[:, :, :].rearrange("p c e -> p (c e)")
    P.add('dve', lambda e: e.tensor_tensor(out=rank, in0=p1[:, 0:288], in1=basef, op=ALU.add), r=['base'], w=['ps1', 'rank'])
    P.add('dve', lambda e: e.tensor_tensor(out=val, in0=rank, in1=CS[:, C_CAPT:C_CAPT + 288], op=ALU.is_lt), r=['rank', 'CS'], w=['val'])
    P.add('dve', lambda e: e.tensor_tensor(out=val, in0=val, in1=maskf, op=ALU.mult), r=['mask'], w=['val'])
    P.add('dve', lambda e: e.tensor_tensor(out=rank, in0=rank, in1=CS[:, C_EOFF:C_EOFF + 288], op=ALU.add), r=['CS'], w=['rank'])
    P.add('dve', lambda e: e.tensor_scalar(out=rank, in0=rank, scalar1=-BIG, scalar2=None, op0=ALU.add), w=['rank'])
    P.add('dve', lambda e: e.tensor_tensor(out=rank, in0=rank, in1=val, op=ALU.mult), r=['val'], w=['rank'])
    P.add('dve', lambda e: e.tensor_scalar(out=rank, in0=rank, scalar1=BIG, scalar2=None, op0=ALU.add), w=['rank'])
    idxi = idx.bitcast(I32)
    P.add('dve', lambda e: e.tensor_copy(out=idxi, in_=rank), r=['rank'], w=['idx'])
    if 'idx' in self.tapd and l == 0:
        P.dma('sp', self.tapd['idx'][:, :], rank, r=['rank'], w=['tapi'])
    P.barrier()


K.phase56 = _phase56
K.phase6b = _phase6b


def _phase8(self, l):
    P, CS = self.P, self.CS
    ident = CS[:, C_ID:C_ID + 128]
    o = [0]

    def cv(n):
        a = self.carve(o[0], n); o[0] += n
        return a
    W = 256
    lastl = (l == L - 1)
    S0 = CAPC if lastl else 0
    NS = NSLOT - S0
    T0 = 2 if lastl else 0
    NWB = 6
    wbuf = [cv(8 * W).rearrange("p (k n) -> p k n", k=8) for _ in range(NWB)]
    xgf = [cv(3 * 1042) for _ in range(2)]
    xsT = cv(8 * NSLOT).rearrange("p (k t) -> p k t", k=8)
    hidT = cv(8 * NSLOT).rearrange("p (k t) -> p k t", k=8)
    ysb = [cv(3 * 1024) for _ in range(2)]
    tmpg = cv(NSLOT)
    xrr = [cv(1042) for _ in range(NT)]
    assert o[0] <= AWTOP, o[0]
    idxi = self.carve(AWTOP, 288).bitcast(I32)
    for i in range(NT):
        P.dma('sp' if i % 2 == 0 else 'act', xrr[i], self.xn2x[i * 128:(i + 1) * 128, :], w=['xrr%d' % i])

    def scatter(ex_):
        for i in range(T0, NT):
            P.add('pool', lambda e, i=i, ex_=ex_: e.indirect_dma_start(
                out=self.xs[:, :], out_offset=bass.IndirectOffsetOnAxis(ap=idxi[:, i * 16 + ex_:i * 16 + ex_ + 1], axis=0),
                in_=xrr[i][:, :], in_offset=None, bounds_check=self.bcreg(e), oob_is_err=False),
                r=['xrr%d' % i], cw=['xs%d' % ex_], dma=True)
    AHEAD = 4
    for ex_ in range(AHEAD):
        scatter(ex_)
    CH = [(32, 128), (160, 128)] if lastl else [(0, 128), (128, 128), (256, 32)]
    cnt = {'nw': 0, 'nd': 0, 'nbk': 0}

    def load_xg(ex_):
        b = ex_ % 2
        xg = xgf[b]
        for ch, (r0, rows) in enumerate(CH):
            P.dma('sp', xg[0:rows, ch * 1042:(ch + 1) * 1042], self.xs[ex_ * NSLOT + r0:ex_ * NSLOT + r0 + rows, :], r=['xs%d' % ex_], cw=['xg%d' % b])

    def transp(ex_):
        b = ex_ % 2
        xg = xgf[b]
        for ch, (r0, rows) in enumerate(CH):
            for hb in range(2):
                bank = cnt['nbk'] % 2; cnt['nbk'] += 1
                pb = self.ps[bank]
                for k4 in range(4):
                    kc = hb * 4 + k4
                    P.add('pe', lambda e, pb=pb, k4=k4, kc=kc, xg=xg, ch=ch, rows=rows: e.transpose(
                        pb[:, k4 * 128:k4 * 128 + rows], xg[0:rows, ch * 1042 + kc * 128:ch * 1042 + (kc + 1) * 128], ident[0:rows, 0:rows]),
                        r=['xg%d' % b, 'CS'], w=['ps%d' % bank])
                dst = xsT[:, hb * 4:hb * 4 + 4, r0 - S0:r0 - S0 + rows]
                srcp = pb[:, :].rearrange("p (k t) -> p k t", k=4)[:, :, 0:rows]
                if bank == 0:
                    P.add('act', lambda e, dst=dst, srcp=srcp: e.activation(out=dst, in_=srcp, func=AF.Copy), w=['ps%d' % bank], cw=['xsT'])
                else:
                    P.add('dve', lambda e, dst=dst, srcp=srcp: e.tensor_copy(out=dst, in_=srcp), w=['ps%d' % bank], cw=['xsT'])

    def gate_up(ex_):
        for fq in range(D // W):
            ig = cnt['nw'] % NWB; cnt['nw'] += 1
            iu = cnt['nw'] % NWB; cnt['nw'] += 1
            wgt, wut = wbuf[ig], wbuf[iu]
            gk_, uk_ = 'wbuf%d' % ig, 'wbuf%d' % iu
            P.dma('sp', wgt, self.w_e_gate[l, ex_, :, fq * W:(fq + 1) * W].rearrange("(kc p) n -> p kc n", p=128), w=[gk_])
            P.dma('sp', wut, self.w_e_up[l, ex_, :, fq * W:(fq + 1) * W].rearrange("(kc p) n -> p kc n", p=128), w=[uk_])
            if fq == 1 and ex_ + 1 < NE:
                load_xg(ex_ + 1)
            for fc in range(W // 128):
                fglob = fq * (W // 128) + fc
                bg_, bu_ = 2 + fglob % 2, 4 + fglob % 2
                pg, pu = self.ps[bg_], self.ps[bu_]
                for kc in range(8):
                    P.add('pe', lambda e, pg=pg, kc=kc, fc=fc, wgt=wgt: e.matmul(pg[:, 0:NS], lhsT=wgt[:, kc, fc * 128:(fc + 1) * 128], rhs=xsT[:, kc, 0:NS],
                                                                            start=(kc == 0), stop=(kc == 7)), r=[gk_, 'xsT'], w=['ps%d' % bg_])
                for kc in range(8):
                    P.add('pe', lambda e, pu=pu, kc=kc, fc=fc, wut=wut: e.matmul(pu[:, 0:NS], lhsT=wut[:, kc, fc * 128:(fc + 1) * 128], rhs=xsT[:, kc, 0:NS],
                                                                            start=(kc == 0), stop=(kc == 7)), r=[uk_, 'xsT'], w=['ps%d' % bu_])
                P.add('act', lambda e, pg=pg: e.activation(out=tmpg[:, 0:NS], in_=pg[:, 0:NS], func=AF.Silu), w=['ps%d' % bg_, 'tmpg'])
                P.add('dve', lambda e, pu=pu, fglob=fglob: e.tensor_tensor(out=hidT[:, fglob, 0:NS], in0=pu[:, 0:NS], in1=tmpg[:, 0:NS], op=ALU.mult),
                      r=['tmpg'], w=['ps%d' % bu_], cw=['hidT'])

    def down(ex_):
        b = ex_ % 2
        xg = xgf[b]
        for dq in range(D // W):
            idn = cnt['nw'] % NWB; cnt['nw'] += 1
            wdt = wbuf[idn]; dk_ = 'wbuf%d' % idn
            P.dma('sp', wdt, self.w_e_down[l, ex_, :, dq * W:(dq + 1) * W].rearrange("(kc p) n -> p kc n", p=128), w=[dk_])
            for ch, (r0, rows) in enumerate(CH):
                bank = 6 + (ch + dq) % 2
                pd = self.ps[bank]
                for fc in range(8):
                    P.add('pe', lambda e, pd=pd, fc=fc, wdt=wdt, r0=r0, rows=rows: e.matmul(pd[0:rows, 0:W], lhsT=hidT[:, fc, r0 - S0:r0 - S0 + rows], rhs=wdt[:, fc, :],
                                                                                      start=(fc == 0), stop=(fc == 7)), r=['hidT', dk_], w=['ps%d' % bank])
                gcol = xg[0:rows, ch * 1042 + 1024 + ex_:ch * 1042 + 1025 + ex_]
                dst = ysb[b][0:rows, ch * 1024 + dq * W:ch * 1024 + (dq + 1) * W]
                if (ch + dq) % 2 == 0:
                    P.add('act', lambda e, pd=pd, dst=dst, gcol=gcol, rows=rows: e.activation(out=dst, in_=pd[0:rows, 0:W], func=AF.Copy, scale=gcol),
                          r=['xg%d' % b], w=['ps%d' % bank], cw=['ysb%d' % b])
                else:
                    P.add('dve', lambda e, pd=pd, dst=dst, gcol=gcol, rows=rows: e.tensor_scalar(out=dst, in0=pd[0:rows, 0:W], scalar1=gcol, scalar2=None, op0=ALU.mult),
                          r=['xg%d' % b], w=['ps%d' % bank], cw=['ysb%d' % b])

    def scatter_add(ex_):
        b = ex_ % 2
        xg = xgf[b]
        for ch, (r0, rows) in enumerate(CH):
            icol = xg[0:rows, ch * 1042 + 1040:ch * 1042 + 1041].bitcast(I32)
            src = ysb[b][0:rows, ch * 1024:(ch + 1) * 1024]
            P.add('pool', lambda e, icol=icol, src=src: e.indirect_dma_start(
                out=self.macc[:, :], out_offset=bass.IndirectOffsetOnAxis(ap=icol, axis=0), in_=src, in_offset=None, compute_op=ALU.add),
                r=['ysb%d' % b, 'xg%d' % b], w=['macc'], dma=True)

    load_xg(0)
    transp(0)
    for ex_ in range(NE):
        gate_up(ex_)
        if ex_ + 1 < NE:
            transp(ex_ + 1)
        down(ex_)
        if ex_ + AHEAD < NE:
            scatter(ex_ + AHEAD)
        scatter_add(ex_)
    P.barrier()


def _phase9(self, l):
    P, CS = self.P, self.CS
    last = (l == L - 1)
    o = [0]

    def cv(n):
        a = self.carve(o[0], n); o[0] += n
        return a
    gt = {'l': cv(1024), 'c': cv(1024)}
    gf = cv(1024)
    P.dma('sp', gt['l'], self.modrows[l, 5, 0, :].partition_broadcast(128), w=['gtl'])
    P.dma('sp', gt['c'], self.modrows[l, 5, 1, :].partition_broadcast(128), w=['gtc'])
    P.dma('sp', gf, self.g_final[:].partition_broadcast(128), w=['gf'])
    NB9 = 4
    xt = [cv(1024) for _ in range(NB9)]; mt = [cv(1024) for _ in range(NB9)]; sqs = [cv(1024) for _ in range(NB9)]
    st = [cv(4) for _ in range(NB9)]
    def s1(i):
        b = i % NB9
        isc = i < 2
        sq = sqs[b]
        g_ = gt['c' if isc else 'l']; gk = 'gtc' if isc else 'gtl'
        x_, m_, st_ = xt[b], mt[b], st[b]
        P.dma('sp', x_, self.xres[i * 128:(i + 1) * 128, :], w=['xt%d' % b])
        P.dma('pool', m_, self.macc[i * 128:(i + 1) * 128, :], w=['mt%d' % b])
        P.add('dve', lambda e: e.tensor_tensor(out=m_, in0=m_, in1=g_, op=ALU.mult), r=[gk], w=['mt%d' % b])
        P.add('pool', lambda e: e.tensor_tensor(out=m_, in0=m_, in1=x_, op=ALU.add), r=['xt%d' % b], w=['mt%d' % b])
        if last:
            P.add('act', lambda e: e.activation(out=sq, in_=m_, func=AF.Square, accum_out=st_[:, 0:1]), r=['mt%d' % b], w=['sq%d' % b, 'st%d' % b])
            P.add('act', lambda e: e.activation(out=st_[:, 1:2], in_=st_[:, 0:1], func=AF.Sqrt, scale=1.0 / D, bias=CS[:, C_EPS:C_EPS + 1]), w=['st%d' % b])

    def s2(i):
        b = i % NB9
        m_, st_ = mt[b], st[b]
        if not last:
            P.dma('act', self.xres[i * 128:(i + 1) * 128, :], m_, r=['mt%d' % b], cw=['xres'])
        else:
            P.add('dve', lambda e: e.reciprocal(out=st_[:, 2:3], in_=st_[:, 1:2]), w=['st%d' % b])
            P.add('dve', lambda e: e.scalar_tensor_tensor(out=m_, in0=m_, scalar=st_[:, 2:3], in1=gf, op0=ALU.mult, op1=ALU.mult),
                  r=['st%d' % b, 'gf'], w=['mt%d' % b])
            P.dma('act', self.out[(i - 2) * 128:(i - 1) * 128, :], m_, r=['mt%d' % b], cw=['out'])
    blocks = [i for i in range(NT) if not (last and i < 2)]
    DEP = 2
    for j in range(min(DEP, len(blocks))):
        s1(blocks[j])
    for j, i in enumerate(blocks):
        if j + DEP < len(blocks):
            s1(blocks[j + DEP])
        s2(i)
    P.barrier()


K.phase8 = _phase8
K.phase9 = _phase9


def kernel(**inputs):
    kk = K(stop_after=None)
    nc = kk.build()
    consts = make_consts2()
    dc, ds = make_dft()
    in_maps = []
    for b in range(8):
        m = host_inputs(inputs, b)
        m['consts'] = consts
        m['dftc'] = dc
        m['dfts'] = ds
        in_maps.append(m)
    res = run_bass_kernel_spmd(nc, in_maps, core_ids=list(range(8)))
    return np.stack([np.asarray(res.results[b]['out']) for b in range(8)], 0).astype(np.float32)
```

```python
from concourse.bass_utils import run_bass_kernel_spmd
import numpy as np
import concourse.bass as bass
import concourse.mybir as mybir
from contextlib import ExitStack

F32 = mybir.dt.float32
F32R = mybir.dt.float32r
I32 = mybir.dt.int32
U32 = mybir.dt.uint32
AF = mybir.ActivationFunctionType
ALU = mybir.AluOpType
AX = mybir.AxisListType

ENG_ATTR = {'pe': 'tensor', 'dve': 'vector', 'act': 'scalar', 'pool': 'gpsimd', 'sp': 'sync'}


class _Res:
    __slots__ = ('name', 'writers', 'readers', 'semi')

    def __init__(self, name):
        self.name = name
        self.writers = []
        self.readers = []
        self.semi = {}


class _Op:
    __slots__ = ('eng', 'fn', 'dma_res', 'dma_sem', 'waits_c', 'waits_s', 'signal', 'sig')

    def __init__(self, eng, fn):
        self.eng = eng
        self.fn = fn
        self.dma_res = None
        self.dma_sem = None
        self.waits_c = []
        self.waits_s = []
        self.signal = False
        self.sig = 0


class Prog:
    def __init__(self, nc):
        self.nc = nc
        self.q = {e: [] for e in ENG_ATTR}
        self.res = {}
        self.nops = 0
        self.capturing = None
        self.semcnt = []
        self.semcls = []
        self.free_sems = {'hw': [], 'sw': []}

    def _r(self, key):
        r = self.res.get(key)
        if r is None:
            r = self.res[key] = _Res(key)
        return r

    def mark(self, name):
        if self.capturing is not None:
            self.capturing.append(('mark', name))

    def replay(self, ops):
        for o in ops:
            self.add(*o[0], **o[1])

    def replay_interleaved(self, a, b):
        ia = ib = 0
        na, nb = len(a), len(b)
        while ia < na or ib < nb:
            if ib >= nb or (ia < na and ia * nb <= ib * na):
                self.add(*a[ia][0], **a[ia][1]); ia += 1
            else:
                self.add(*b[ib][0], **b[ib][1]); ib += 1

    def add(self, eng, fn, r=(), w=(), cw=(), dma=False):
        if self.capturing is not None:
            self.capturing.append(((eng, fn), dict(r=r, w=w, cw=cw, dma=dma)))
            return None
        op = _Op(eng, fn)
        deps = []
        for key in r:
            R = self._r(key)
            deps.extend(R.writers)
            R.readers.append(op)
        for key in w:
            R = self._r(key)
            deps.extend(R.writers)
            deps.extend(R.readers)
            R.writers = [op]
            R.readers = []
        for key in cw:
            R = self._r(key)
            if R.readers:
                deps.extend(R.readers)
                R.writers = [op]
                R.readers = []
            else:
                R.writers.append(op)
        for d in deps:
            if d is op:
                continue
            if d.dma_res is not None:
                si = d.dma_sem
                op.waits_s.append((si, self.semcnt[si]))
            else:
                if d.eng == eng and eng == 'pe':
                    continue
                d.signal = True
                op.waits_c.append(d)
        if dma:
            keys = list(w) + list(cw)
            assert len(keys) == 1, keys
            R = self._r(keys[0])
            cls = 'sw' if eng == 'pool' else 'hw'
            si = R.semi.get(cls)
            if si is None:
                if self.free_sems[cls]:
                    si = self.free_sems[cls].pop()
                else:
                    self.semcnt.append(0)
                    self.semcls.append(cls)
                    si = len(self.semcnt) - 1
                R.semi[cls] = si
            self.semcnt[si] += 1
            op.dma_res = R
            op.dma_sem = si
        self.q[eng].append(op)
        self.nops += 1
        return op

    def dma(self, eng, out, in_, r=(), w=(), cw=(), **kw):
        return self.add(eng, lambda e: e.dma_start(out=out, in_=in_, **kw), r=r, w=w, cw=cw, dma=True)

    def barrier(self):
        lasts = []
        for e in ('pe', 'dve', 'act', 'pool'):
            for o in reversed(self.q[e]):
                if o.dma_res is None:
                    lasts.append(o)
                    break
        dres = [(si, self.semcnt[si]) for R in self.res.values() for si in R.semi.values()]
        for e in ENG_ATTR:
            op = _Op(e, lambda en: en.nop())
            for d in lasts:
                if d.dma_res is None:
                    d.signal = True
                    op.waits_c.append(d)
            op.waits_s = dres
            self.q[e].append(op)
        self.res = {}
        self.free_sems = {c: [i for i in range(len(self.semcnt)) if self.semcls[i] == c] for c in ('hw', 'sw')}

    def emit(self):
        nc = self.nc
        with ExitStack() as es:
            csem = {}
            for e in ('pe', 'dve', 'act', 'pool'):
                csem[e] = es.enter_context(nc.semaphore('c_' + e))
            dsem = [es.enter_context(nc.semaphore('d%d' % i)) for i in range(len(self.semcnt))]
            for e, ops in self.q.items():
                n = 0
                for op in ops:
                    if op.signal:
                        n += 1
                        op.sig = n
            q = self.q

            def run(eng_name, e):
                waited = {}
                for op in q[eng_name]:
                    for d in op.waits_c:
                        s = csem[d.eng]
                        if waited.get(d.eng, 0) < d.sig:
                            e.wait_ge(s, d.sig)
                            waited[d.eng] = d.sig
                    for si, cnt in op.waits_s:
                        v = 16 * cnt
                        s = dsem[si]
                        if waited.get(si, 0) < v:
                            e.wait_ge(s, v)
                            waited[si] = v
                    ins = op.fn(e)
                    if op.dma_sem is not None:
                        ins.then_inc(dsem[op.dma_sem], 16)
                    elif op.signal:
                        ins.then_inc(csem[eng_name], 1)

            with nc.Block() as block:
                @block.tensor
                def _(e):
                    run('pe', e)

                @block.vector
                def _(e):
                    run('dve', e)

                @block.scalar
                def _(e):
                    run('act', e)

                @block.gpsimd
                def _(e):
                    run('pool', e)

                @block.sync
                def _(e):
                    run('sp', e)
        return len(self.semcnt)


import os

L = 2
D = 1024
TL = 2048
TC = 256
T = TL + TC
NT = T // 128
INC = 2832
NE = 16
CAPL, CAPC = 256, 32
NSLOT = CAPL + CAPC
EPS = 1e-6
TCH = [(0, 512), (512, 512), (1024, 512), (1536, 512), (2048, 256)]
CHUNKS = ([(i * 128, 128) for i in range(12)] + [(1536, 16)] +
          [(1552 + i * 128, 128) for i in range(4)] + [(2064 + i * 128, 128) for i in range(2)] +
          [(2320 + i * 128, 128) for i in range(2)] + [(2576 + i * 128, 128) for i in range(2)])

C_ID, C_ONE, C_TU, C_TL, C_TS, C_BD, C_DC, C_EPS = 0, 128, 256, 384, 512, 640, 896, 1920
NCONST = 1924
NCONST2 = NCONST + 578 + 18


def make_consts():
    c = np.zeros((128, NCONST), np.float32)
    i = np.arange(128)
    c[:, C_ID:C_ID + 128] = np.eye(128)
    c[:, C_ONE:C_ONE + 128] = 1.0
    c[:, C_TU:C_TU + 128] = (i[:, None] <= i[None, :])
    c[:, C_TL:C_TL + 128] = (i[:, None] >= i[None, :])
    c[:, C_TS:C_TS + 128] = (i[:, None] < i[None, :])
    j = np.arange(64)
    ang = 2 * np.pi * np.outer(j, j) / 64.0
    bc = np.zeros((128, 128)); bs = np.zeros((128, 128))
    for g in range(2):
        bc[g * 64:(g + 1) * 64, g * 64:(g + 1) * 64] = np.cos(ang)
        bs[g * 64:(g + 1) * 64, g * 64:(g + 1) * 64] = np.sin(ang)
    c[:, C_BD:C_BD + 128] = bc
    c[:, C_BD + 128:C_BD + 256] = bs
    t = np.arange(256)
    a2 = 2 * np.pi * np.outer(t, t) / 256.0
    cc = np.cos(a2).reshape(2, 128, 256).transpose(1, 0, 2).reshape(128, 512)
    ss = np.sin(a2).reshape(2, 128, 256).transpose(1, 0, 2).reshape(128, 512)
    c[:, C_DC:C_DC + 512] = cc
    c[:, C_DC + 512:C_DC + 1024] = ss
    c[:, C_EPS] = EPS
    c[:, C_EPS + 1] = 1.0
    c[:, C_EPS + 2] = -0.5 * np.log(128.0)
    return c


def make_dft():
    t = np.arange(TL, dtype=np.float64)
    a = 2 * np.pi * ((np.outer(t, t)) % TL) / TL
    return np.cos(a).astype(np.float32), np.sin(a).astype(np.float32)


class K:
    def __init__(self, stop_after=None, taps=(), ne_decl=NE):
        self.stop_after = stop_after
        self.taps = taps
        nc = self.nc = bass.Bass("TRN2", target_bir_lowering=False)
        self.P = Prog(nc)
        dt = lambda n, s, kind="ExternalInput", d=F32: nc.dram_tensor(n, s, d, kind=kind).ap()
        self.x_in = dt("x", [TL, D]); self.ctx_in = dt("ctx", [TC, D]); self.cT = dt("cT", [128, 16])
        self.w_ada = dt("w_ada", [L, D, 6 * D]); self.b_ada = dt("b_ada", [L, 6 * D])
        self.g_norm1 = dt("g_norm1", [L, D]); self.w_in = dt("w_in", [L, D, INC])
        self.b_gates = dt("b_gates", [L, 16]); self.g_hnorm = dt("g_hnorm", [L, 512])
        self.conv_wT = dt("conv_wT", [L, 256, 31]); self.conv_b = dt("conv_b", [L, 256])
        self.conv_ln_g = dt("conv_ln_g", [L, 256]); self.conv_ln_b = dt("conv_ln_b", [L, 256])
        self.w_out = dt("w_out", [L, D, D]); self.g_norm2 = dt("g_norm2", [L, D])
        self.w_router = dt("w_router", [L, D, NE])
        self.w_e_gate = dt("w_e_gate", [L, ne_decl, D, D]); self.w_e_up = dt("w_e_up", [L, ne_decl, D, D])
        self.w_e_down = dt("w_e_down", [L, ne_decl, D, D]); self.g_final = dt("g_final", [D])
        self.consts = dt("consts", [128, NCONST2]); self.dftc = dt("dftc", [TL, TL]); self.dfts = dt("dfts", [TL, TL])
        self.out = dt("out", [TL, D], kind="ExternalOutput")
        self.modrows = dt("modrows", [L, 6, 2, D], kind="Internal")
        self.uT = dt("uT", [INC, T], kind="Internal")
        self.yT = dt("yT", [D, T], kind="Internal")
        self.xres = dt("xres", [T, D], kind="Internal")
        self.xn2x = dt("xn2x", [T, 1042], kind="Internal")
        self.xs = dt("xs", [NE * NSLOT, 1042], kind="Internal")
        self.macc = dt("macc", [T, D], kind="Internal")
        self.tapd = {}
        for name, shape in taps:
            self.tapd[name] = dt("tap_" + name, shape, kind="ExternalOutput")

    def build(self):
        nc, P = self.nc, self.P
        with ExitStack() as es:
            AW = 50600
            self.A = es.enter_context(nc.sbuf_tensor("arena", [128, AW], F32))
            self.CS = es.enter_context(nc.sbuf_tensor("cs", [128, NCONST2], F32))
            self.ps = [es.enter_context(nc.psum_tensor("ps%d" % i, [128, 512], F32)) for i in range(8)]
            P.dma('sp', self.CS[:, :], self.consts[:, :], w=['CS'])
            P.barrier()
            self.phases()
            P.barrier()
            nsem = P.emit()
            print("ops", P.nops, "dma sems", nsem)
        return nc

    def tap_yT(self):
        if 'yT' in self.tapd:
            self.P.dma('sp', self.tapd['yT'][:, :], self.yT[:, :], w=['tapy'])
            self.P.barrier()

    def tap_xres(self):
        if 'xres' in self.tapd:
            self.P.dma('sp', self.tapd['xres'][:, :], self.xres[:, :], w=['tapxr'])
            self.P.barrier()

    def bcreg(self, e):
        if getattr(self, '_bcr', None) is None:
            self._bcr = e.to_reg(NE * NSLOT - 1)
        return self._bcr

    def carve(self, off, n):
        return self.A[:, off:off + n]

    def phases(self):
        self.phase0(0)
        self.phase0(1)
        if self.stop_after == 0:
            return
        for l in range(L):
            self.phase1(l)
            if self.stop_after == 1:
                return
            caps = []
            for h in range(4):
                self.P.capturing = []
                self.phase2(l, h)
                ops = self.P.capturing
                self.P.capturing = None
                parts = {'pro': [], 'chain': [], 'epi': []}
                cur = 'pro'
                for o_ in ops:
                    if o_[0] == 'mark':
                        cur = o_[1]
                    else:
                        parts[cur].append(o_)
                caps.append(parts)
            self.P.replay(caps[0]['pro'])
            for h in range(4):
                self.P.replay(caps[h]['chain'])
                if h < 3:
                    self.P.replay_interleaved(caps[h]['epi'], caps[h + 1]['pro'])
                else:
                    self.P.replay(caps[h]['epi'])
            self.P.barrier()
            if self.stop_after == 2:
                self.tap_yT()
                return
            self.phase34(l)
            if self.stop_after == 4:
                self.tap_yT()
                return
            self.phase56(l)
            if self.stop_after == 5:
                self.tap_xres()
                return
            self.phase6b(l)
            if self.stop_after in (6, 61):
                return
            self.phase8(l)
            if l == L - 1:
                self.phase9(l)
            if self.stop_after == 9:
                self.tap_xres()
                return

    def phase0_steps(self, l):
        P, CS = self.P, self.CS
        sc = self.carve(20000, 16)
        sc2 = self.carve(20016, 16)
        wb = [self.carve(21024 + i * 4096, 4096).rearrange("p (k n) -> p k n", k=8) for i in range(2)]
        brow = [self.carve(30000 + i * 1024, 1024) for i in range(2)]
        grow = [self.carve(32048 + i * 1024, 1024) for i in range(2)]
        mrow = [self.carve(34096 + i * 1024, 1024) for i in range(2)]
        sc2v = sc2.rearrange("p (k c) -> p k c", c=2)
        steps = []

        def init():
            P.dma('sp', sc, self.cT[:, :], w=['sc'])
            P.add('act', lambda e: e.activation(out=sc2, in_=sc, func=AF.Silu), r=['sc'], w=['sc2'])
        steps.append((init, None))
        for seg in range(6):
            for half in range(2):
                def pe_step(seg=seg, half=half):
                    sb = seg % 2
                    if half == 0:
                        P.dma('sp', brow[sb][0:2, :], self.b_ada[l, seg * D:(seg + 1) * D].partition_broadcast(2), w=['brow%d' % sb])
                        if seg in (1, 4):
                            g = self.g_norm1 if seg == 1 else self.g_norm2
                            P.dma('sp', grow[sb][0:2, :], g[l, :].partition_broadcast(2), w=['grow%d' % sb])
                    n0 = seg * D + half * 512
                    b = half
                    wt = wb[b]
                    P.dma('sp', wt, self.w_ada[l, :, n0:n0 + 512].rearrange("(kc p) n -> p kc n", p=128), w=['wb%d' % b])
                    pb = self.ps[6 + b]
                    for kc in range(8):
                        P.add('pe', lambda e, kc=kc, wt=wt, pb=pb: e.matmul(pb[0:2, :], lhsT=sc2v[:, kc, :], rhs=wt[:, kc, :],
                                                                            start=(kc == 0), stop=(kc == 7)),
                              r=['sc2', 'wb%d' % b], w=['ps%d' % (6 + b)])

                def post_step(seg=seg, half=half):
                    sb = seg % 2
                    b = half
                    pb = self.ps[6 + b]
                    mr = mrow[sb]; mk = 'mrow%d' % sb
                    P.add('dve', lambda e: e.tensor_tensor(out=mr[0:2, half * 512:(half + 1) * 512], in0=pb[0:2, :],
                                                           in1=brow[sb][0:2, half * 512:(half + 1) * 512], op=ALU.add),
                          r=['brow%d' % sb], w=['ps%d' % (6 + b), mk])
                    if half == 1:
                        if seg in (1, 4):
                            P.add('dve', lambda e: e.scalar_tensor_tensor(out=mr[0:2, :], in0=mr[0:2, :], scalar=1.0, in1=grow[sb][0:2, :],
                                                                          op0=ALU.add, op1=ALU.mult), r=['grow%d' % sb], w=[mk])
                        P.dma('pool', self.modrows[l, seg, :, :], mr[0:2, :], r=[mk], cw=['modrows'])
                steps.append((pe_step, post_step))
        return steps

    def phase0(self, l):
        for pe_step, post_step in self.phase0_steps(l):
            pe_step()
            if post_step is not None:
                post_step()
        self.P.barrier()

    def phase1(self, l):
        P, CS = self.P, self.CS
        ident = CS[:, C_ID:C_ID + 128]
        xnT = self.carve(0, 8 * T).rearrange("p (k t) -> p k t", k=8)
        o = 8 * T
        modt = {}
        for nm, seg, row in (('gs_l', 1, 0), ('sh_l', 0, 0), ('gs_c', 1, 1), ('sh_c', 0, 1)):
            modt[nm] = self.carve(o, 1024); o += 1024
            P.dma('sp', modt[nm], self.modrows[l, seg, row, :].partition_broadcast(128), w=[nm])
        NB1 = 3
        xt = [self.carve(o + i * 1024, 1024) for i in range(NB1)]; o += NB1 * 1024
        xn = [self.carve(o + i * 1024, 1024) for i in range(NB1)]; o += NB1 * 1024
        sqs = [self.carve(o + i * 1024, 1024) for i in range(NB1)]; o += NB1 * 1024
        st = [self.carve(o + i * 4, 4) for i in range(NB1)]; o += 4 * NB1
        wbs = [self.carve(o + i * 1024, 1024).rearrange("p (k n) -> p k n", k=8) for i in range(3)]; o += 3072
        stg = [self.carve(o + i * 512, 512) for i in range(4)]; o += 2048
        if l > 0:
            mtl = [self.carve(o + i * 1024, 1024) for i in range(NB1)]; o += NB1 * 1024
            gt2 = {'l': self.carve(o, 1024), 'c': self.carve(o + 1024, 1024)}; o += 2048
            P.dma('sp', gt2['l'], self.modrows[l - 1, 5, 0, :].partition_broadcast(128), w=['gt2l'])
            P.dma('sp', gt2['c'], self.modrows[l - 1, 5, 1, :].partition_broadcast(128), w=['gt2c'])
        NPRE = int(os.environ.get('K_NPRE', '0'))
        nbc = [0]

        def loadw(ci):
            c0, w = CHUNKS[ci]
            wt = wbs[ci % 3]
            P.dma('sp', wt[:, :, 0:w], self.w_in[l, :, c0:c0 + w].rearrange("(kc p) n -> p kc n", p=128), w=['wbs%d' % (ci % 3)])

        def proj(ci, tci):
            c0, w = CHUNKS[ci]
            t0, n = TCH[tci]
            if l == L - 1 and c0 >= 1552 and tci == 0:
                t0, n = TC, 512 - TC
            wt = wbs[ci % 3]; wk = 'wbs%d' % (ci % 3)
            bank = 2 + nbc[0] % 6; sb = nbc[0] % 4; nbc[0] += 1
            pb = self.ps[bank]
            rk = ['xnT%d' % i for i in range(t0 // 128, (t0 + n) // 128)] + [wk]
            for kc in range(8):
                lh, rh = wt[:, kc, 0:w], xnT[:, kc, t0:t0 + n]
                P.add('pe', lambda e, pb=pb, lh=lh, rh=rh, kc=kc, w=w, n=n: e.matmul(pb[0:w, 0:n], lhsT=lh, rhs=rh, start=(kc == 0), stop=(kc == 7)),
                      r=rk, w=['ps%d' % bank])
            sg = stg[sb]
            if nbc[0] % 2 == 0:
                P.add('act', lambda e, sg=sg, pb=pb, w=w, n=n: e.activation(out=sg[0:w, 0:n], in_=pb[0:w, 0:n], func=AF.Copy),
                      w=['ps%d' % bank, 'stg%d' % sb])
            else:
                P.add('dve', lambda e, sg=sg, pb=pb, w=w, n=n: e.tensor_copy(out=sg[0:w, 0:n], in_=pb[0:w, 0:n]),
                      w=['ps%d' % bank, 'stg%d' % sb])
            P.dma('pool', self.uT[c0:c0 + w, t0:t0 + n], sg[0:w, 0:n], r=['stg%d' % sb], cw=['uT'])
        for ci in range(NPRE):
            loadw(ci)
        ready = {3: 0, 7: 1, 11: 2, 15: 3, 17: 4}
        def normN(i):
            b = i % NB1
            isc = i < 2
            sq = sqs[b]
            if l == 0:
                src = self.ctx_in[i * 128:(i + 1) * 128, :] if isc else self.x_in[(i - 2) * 128:(i - 1) * 128, :]
            else:
                src = self.xres[i * 128:(i + 1) * 128, :]
            x_, xn_, st_ = xt[b], xn[b], st[b]
            P.dma('sp', x_, src, w=['xt%d' % b])
            if l > 0:
                m_ = mtl[b]
                g2 = gt2['c' if isc else 'l']; g2k = 'gt2c' if isc else 'gt2l'
                P.dma('sp', m_, self.macc[i * 128:(i + 1) * 128, :], w=['mtl%d' % b])
                P.add('dve', lambda e, m_=m_, g2=g2: e.tensor_tensor(out=m_, in0=m_, in1=g2, op=ALU.mult), r=[g2k], w=['mtl%d' % b])
                P.add('pool', lambda e, m_=m_, x_=x_: e.tensor_tensor(out=x_, in0=m_, in1=x_, op=ALU.add), r=['mtl%d' % b], w=['xt%d' % b])
                P.dma('pool', self.xres[i * 128:(i + 1) * 128, :], x_, r=['xt%d' % b], cw=['xres'])
            P.add('act', lambda e, x_=x_, st_=st_, sq=sq: e.activation(out=sq, in_=x_, func=AF.Square, accum_out=st_[:, 0:1]),
                  r=['xt%d' % b], w=['sq%d' % b, 'st%d' % b])
            P.add('act', lambda e, st_=st_: e.activation(out=st_[:, 1:2], in_=st_[:, 0:1], func=AF.Sqrt, scale=1.0 / D,
                                                         bias=CS[:, C_EPS:C_EPS + 1]), w=['st%d' % b])
            P.add('dve', lambda e, st_=st_: e.reciprocal(out=st_[:, 2:3], in_=st_[:, 1:2]), w=['st%d' % b])
            gs = modt['gs_c' if isc else 'gs_l']; sh = modt['sh_c' if isc else 'sh_l']
            gk = 'gs_c' if isc else 'gs_l'; sk = 'sh_c' if isc else 'sh_l'
            P.add('dve', lambda e, x_=x_, xn_=xn_, st_=st_, gs=gs: e.scalar_tensor_tensor(out=xn_, in0=x_, scalar=st_[:, 2:3], in1=gs,
                                                                                           op0=ALU.mult, op1=ALU.mult),
                  r=['xt%d' % b, 'st%d' % b, gk], w=['xn%d' % b])
            P.add('pool', lambda e, xn_=xn_, sh=sh: e.tensor_tensor(out=xn_, in0=xn_, in1=sh, op=ALU.add), r=[sk], w=['xn%d' % b])

        def transX(i):
            b = i % NB1
            xn_ = xn[b]
            for hb in range(2):
                pb = self.ps[hb]
                for k4 in range(4):
                    kc = hb * 4 + k4
                    P.add('pe', lambda e, pb=pb, k4=k4, kc=kc, xn_=xn_: e.transpose(pb[:, k4 * 128:(k4 + 1) * 128], xn_[:, kc * 128:(kc + 1) * 128], ident),
                          r=['xn%d' % b, 'CS'], w=['ps%d' % hb])
                dst = xnT[:, hb * 4:hb * 4 + 4, i * 128:(i + 1) * 128]
                srcp = pb[:, :].rearrange("p (k t) -> p k t", k=4)
                if hb == 0:
                    P.add('act', lambda e, dst=dst, srcp=srcp: e.activation(out=dst, in_=srcp, func=AF.Copy), w=['ps0'], cw=['xnT%d' % i])
                else:
                    P.add('dve', lambda e, dst=dst, srcp=srcp: e.tensor_copy(out=dst, in_=srcp), w=['ps1'], cw=['xnT%d' % i])
            if i in ready:
                for ci in range(NPRE):
                    proj(ci, ready[i])
        normN(0)
        for i in range(NT):
            if i + 1 < NT:
                normN(i + 1)
            transX(i)
        if 'xnT' in self.tapd and l == 0:
            P.dma('sp', self.tapd['xnT'].rearrange("(k p) t -> p k t", p=128), xnT, r=['xnT%d' % i for i in range(NT)], w=['tapx'])
        for ci in range(NPRE, len(CHUNKS)):
            loadw(ci)
            for tci in range(len(TCH)):
                proj(ci, tci)
        P.barrier()
        if 'uT' in self.tapd and l == 0:
            P.dma('sp', self.tapd['uT'][:, :], self.uT[:, :], w=['tapu'])
            P.barrier()


def host_inputs(inp, b):
    cT = np.stack([np.asarray(inp['c'][b]).reshape(8, 128).T, np.asarray(inp['c_ctx']).reshape(8, 128).T], axis=-1)
    m = {
        'x': np.ascontiguousarray(inp['x'][b]), 'ctx': np.ascontiguousarray(inp['ctx'][b]),
        'cT': np.ascontiguousarray(cT.reshape(128, 16)).astype(np.float32),
        'b_gates': np.asarray(inp['b_gates']).reshape(L, 16),
    }
    m['conv_wT'] = np.ascontiguousarray(np.asarray(inp['conv_w']).transpose(0, 2, 1))
    for k in ('w_ada', 'b_ada', 'g_norm1', 'w_in', 'g_hnorm', 'conv_b', 'conv_ln_g', 'conv_ln_b', 'w_out', 'g_norm2',
              'w_router', 'w_e_gate', 'w_e_up', 'w_e_down', 'g_final'):
        m[k] = np.asarray(inp[k])
    return m


def _phase2(self, l, h):
    P, CS = self.P, self.CS
    ident = CS[:, C_ID:C_ID + 128]; ones = CS[:, C_ONE:C_ONE + 128]
    triU = CS[:, C_TU:C_TU + 128]; triL = CS[:, C_TL:C_TL + 128]
    o = [0]

    def cv(n):
        a = self.carve(o[0], n); o[0] += n
        return a
    rawbuf = [[cv(T) for _ in range(3)] for _ in range(2)]
    raw = rawbuf[h % 2]
    rk = ['raw%d_%d' % (h % 2, j) for j in range(3)]
    colmaj = h >= 2
    scanbuf = [cv(T) for _ in range(3)]
    scan = scanbuf if colmaj else raw
    Q, Kt, Vt = scan
    g4 = cv(T); g4s_ = cv(T); g4s = g4s_ if colmaj else g4
    ktm = cv(NT * 128).rearrange("p (c d) -> p c d", c=NT)
    vp = [cv(NT * 130).rearrange("p (c d) -> p c d", c=NT) for _ in range(2)]
    H = [cv(NT * 128).rearrange("p (c d) -> p c d", c=NT) for _ in range(2)]
    oTb = [cv(T) for _ in range(2)]; oT = oTb[h % 2]; oTk = 'oT%d' % (h % 2)
    YT = cv(T)
    bg = cv(4); ghnb = [cv(1) for _ in range(2)]; ghn = ghnb[h % 2]; ghk = 'ghn%d' % (h % 2)
    G = [cv(NT) for _ in range(4)]
    nlf = [cv(NT) for _ in range(2)]
    totS = [cv(NT) for _ in range(2)]
    dd = [cv(NT) for _ in range(2)]
    flo = [cv(NT) for _ in range(2)]
    wk = [cv(NT) for _ in range(2)]
    dec = [cv(NT) for _ in range(2)]
    Cst = [cv(130) for _ in range(2)]
    Cd2 = [[cv(130) for _ in range(2)] for _ in range(2)]
    STm2 = [[cv(128) for _ in range(2)] for _ in range(2)]
    dn2 = [[cv(2) for _ in range(2)] for _ in range(2)]
    ssq = cv(NT); rst = cv(NT); tmp = cv(NT * 128)
    for j, base in enumerate((0, 512, 1024)):
        P.dma('sp', raw[j], self.uT[base + h * 128: base + (h + 1) * 128, :], w=[rk[j]])
    P.dma('sp', g4[0:4, :], self.uT[1536 + 4 * h:1536 + 4 * h + 4, :], w=['g4'])
    P.dma('sp', oT, self.uT[1552 + h * 128:1552 + (h + 1) * 128, :], w=[oTk])
    P.dma('sp', bg, self.b_gates[l, 4 * h:4 * h + 4].partition_broadcast(128), w=['bg'])
    P.dma('sp', ghn, self.g_hnorm[l, h * 128:(h + 1) * 128].rearrange("(p o) -> p o", o=1), w=[ghk])
    if colmaj:
        for j in range(3):
            eng = ('pool', 'dve', 'act')[j]
            s_, d_ = raw[j], scan[j]
            if eng == 'act':
                P.add(eng, lambda e, s_=s_, d_=d_: e.activation(out=d_[:, 0:TC], in_=s_[:, 0:TC], func=AF.Copy), r=[rk[j]], w=['scanA%d' % j])
                P.add(eng, lambda e, s_=s_, d_=d_: e.activation(out=d_[:, TC:].rearrange("p (c r) -> p c r", r=32),
                                                                in_=s_[:, TC:].rearrange("p (r c) -> p c r", c=64), func=AF.Copy),
                      r=[rk[j]], w=['scan%d' % j])
            else:
                P.add(eng, lambda e, s_=s_, d_=d_: e.tensor_copy(out=d_[:, 0:TC], in_=s_[:, 0:TC]), r=[rk[j]], w=['scanA%d' % j])
                P.add(eng, lambda e, s_=s_, d_=d_: e.tensor_copy(out=d_[:, TC:].rearrange("p (c r) -> p c r", r=32),
                                                                 in_=s_[:, TC:].rearrange("p (r c) -> p c r", c=64)),
                      r=[rk[j]], w=['scan%d' % j])
        P.add('pool', lambda e: e.tensor_copy(out=g4s[0:4, 0:TC], in_=g4[0:4, 0:TC]), r=['g4'], w=['g4sA'])
        P.add('pool', lambda e: e.tensor_copy(out=g4s[0:4, TC:].rearrange("p (c r) -> p c r", r=32),
                                              in_=g4[0:4, TC:].rearrange("p (r c) -> p c r", c=64)), r=['g4'], w=['g4s'])
        sk = [['scan%d' % j, 'scanA%d' % j] for j in range(3)]
        gk = ['g4s', 'g4sA']
    else:
        sk = [[rk[j]] for j in range(3)]
        gk = ['g4']
    pb = self.ps[0]
    for c in range(NT):
        P.add('pe', lambda e, c=c: e.transpose(pb[:, c * 4:(c + 1) * 4], g4s[0:4, c * 128:(c + 1) * 128], ident[0:4, 0:4]), r=gk + ['CS'], w=['ps0'])
    pv = pb[:, 0:NT * 4].rearrange("p (c g) -> p c g", g=4)
    for g in range(4):
        P.add('dve', lambda e, g=g: e.tensor_scalar(out=G[g], in0=pv[:, :, g], scalar1=bg[:, g:g + 1], scalar2=None, op0=ALU.add),
              r=['bg'], w=['ps0', 'G%d' % g])
    p1 = self.ps[1]
    for d in range(2):
        Fg = G[1 + 2 * d]; Ig = G[2 * d]
        P.add('act', lambda e, d=d, Fg=Fg: e.activation(out=nlf[d], in_=Fg, func=AF.Exp, scale=-1.0), r=['G%d' % (1 + 2 * d)], w=['nlf%d' % d])
        P.add('act', lambda e, d=d: e.activation(out=nlf[d], in_=nlf[d], func=AF.Ln, bias=CS[:, C_EPS + 1:C_EPS + 2]), w=['nlf%d' % d])
        tri = triU if d == 0 else triL
        P.add('pe', lambda e, d=d, tri=tri: e.matmul(p1[:, d * 64:d * 64 + NT], lhsT=tri, rhs=nlf[d], start=True, stop=True), r=['nlf%d' % d, 'CS'], w=['ps1'])
        P.add('pe', lambda e, d=d: e.matmul(p1[:, d * 64 + 32:d * 64 + 32 + NT], lhsT=ones, rhs=nlf[d], start=True, stop=True), r=['nlf%d' % d, 'CS'], w=['ps1'])
        P.add('act', lambda e, d=d: e.activation(out=totS[d], in_=p1[:, d * 64 + 32:d * 64 + 32 + NT], func=AF.Copy), w=['ps1', 'tot%d' % d])
        P.add('dve', lambda e, d=d: e.tensor_tensor(out=dd[d], in0=p1[:, d * 64:d * 64 + NT], in1=totS[d], op=ALU.subtract), r=['tot%d' % d], w=['ps1', 'dd%d' % d])
        P.add('act', lambda e, d=d: e.activation(out=flo[d], in_=dd[d], func=AF.Exp), r=['dd%d' % d], w=['flo%d' % d])
        P.add('dve', lambda e, d=d, Ig=Ig: e.tensor_tensor(out=wk[d], in0=dd[d], in1=Ig, op=ALU.add), r=['dd%d' % d, 'G%d' % (2 * d)], w=['wk%d' % d])
        P.add('act', lambda e, d=d: e.activation(out=wk[d], in_=wk[d], func=AF.Exp, bias=CS[:, C_EPS + 2:C_EPS + 3]), w=['wk%d' % d])
        P.add('act', lambda e, d=d: e.activation(out=dec[d], in_=totS[d], func=AF.Exp, scale=-1.0), r=['tot%d' % d], w=['dec%d' % d])
        P.add('pool', lambda e, d=d: e.memset(vp[d][:, :, 128:130], 0.0), w=['vpx%d' % d])
        P.add('pool', lambda e, d=d: e.memset(Cst[d], 0.0), w=['C%d' % d])
    for d in range(2):
        P.add('dve', lambda e, d=d: e.tensor_copy(out=vp[d][:, :, 128], in_=wk[d]), r=['wk%d' % d], w=['vpx%d' % d])
    nb = 0
    for c in range(NT):
        bank = 2 + nb % 6; nb += 1
        pk = self.ps[bank]
        P.add('pe', lambda e, c=c, pk=pk: e.transpose(pk[:, 0:128], Kt[:, c * 128:(c + 1) * 128], ident), r=sk[1] + ['CS'], w=['ps%d' % bank])
        P.add('pe', lambda e, c=c, pk=pk: e.transpose(pk[:, 128:256], Vt[:, c * 128:(c + 1) * 128], ident), r=sk[2] + ['CS'], w=['ps%d' % bank])
        P.add('act', lambda e, c=c, pk=pk: e.activation(out=ktm[:, c, :], in_=pk[:, 0:128], func=AF.Copy), w=['ps%d' % bank], cw=['ktm'])
        P.add('dve', lambda e, c=c, pk=pk: e.tensor_scalar(out=vp[0][:, c, 0:128], in0=pk[:, 128:256], scalar1=wk[0][:, c:c + 1], scalar2=None, op0=ALU.mult),
              r=['wk0'], w=['ps%d' % bank], cw=['vp0'])
        P.add('act', lambda e, c=c, pk=pk: e.activation(out=vp[1][:, c, 0:128], in_=pk[:, 128:256], func=AF.Copy, scale=wk[1][:, c:c + 1]),
              r=['wk1'], w=['ps%d' % bank], cw=['vp1'])
    P.mark('chain')
    order = [list(range(NT)), [1, 0] + list(range(NT - 1, 1, -1))]

    def front(step, d):
        c = order[d][step]
        cs = slice(c * 128, (c + 1) * 128)
        bST, bO, bC = self.ps[4 * d], self.ps[4 * d + 1 + step % 2], self.ps[4 * d + 3]
        kST, kO, kC = 'ps%d' % (4 * d), 'ps%d' % (4 * d + 1 + step % 2), 'ps%d' % (4 * d + 3)
        sp_ = step % 2
        stm = STm2[d][sp_]; stk = 'STm%d_%d' % (d, sp_)
        msk = triU if d == 0 else triL
        cdt = Cd2[d][sp_]; cdk = 'Cd%d_%d' % (d, sp_)
        P.add('pe', lambda e: e.matmul(bST[:, 0:128], lhsT=Kt[:, cs], rhs=Q[:, cs], start=True, stop=True), r=sk[0] + sk[1], w=[kST])
        P.add('pe', lambda e: e.matmul(bC[:, 0:130], lhsT=ktm[:, c, :], rhs=vp[d][:, c, :], start=True, stop=True),
              r=['ktm', 'vp%d' % d, 'vpx%d' % d], w=[kC])
        P.add('dve', lambda e: e.tensor_scalar(out=cdt, in0=Cst[d], scalar1=dec[d][:, c:c + 1], scalar2=None, op0=ALU.mult),
              r=['C%d' % d, 'dec%d' % d], w=[cdk])
        P.add('dve', lambda e: e.tensor_tensor(out=stm, in0=bST[:, 0:128], in1=msk, op=ALU.mult), r=['CS'], w=[kST, stk])
        P.add('dve', lambda e: e.tensor_tensor(out=Cst[d], in0=bC[:, 0:130], in1=cdt, op=ALU.add), r=[cdk], w=[kC, 'C%d' % d])
        P.add('pe', lambda e: e.matmul(bO[:, 0:130], lhsT=stm, rhs=vp[d][:, c, :], start=True, stop=False),
              r=[stk, 'vp%d' % d, 'vpx%d' % d], w=[kO])
        P.add('pe', lambda e: e.matmul(bO[:, 0:130], lhsT=Q[:, cs], rhs=cdt, start=False, stop=True), r=sk[0] + [cdk], w=[kO])

    def back(step, d):
        c = order[d][step]
        bO = self.ps[4 * d + 1 + step % 2]; kO = 'ps%d' % (4 * d + 1 + step % 2)
        sp_ = step % 2
        dn_ = dn2[d][sp_]; dnk = 'dn%d_%d' % (d, sp_)
        P.add('act', lambda e: e.activation(out=dn_[:, 0:1], in_=bO[:, 128:129], func=AF.Abs), w=[kO, dnk])
        P.add('dve', lambda e: e.tensor_tensor(out=dn_[:, 0:1], in0=dn_[:, 0:1], in1=flo[d][:, c:c + 1], op=ALU.max), r=['flo%d' % d], w=[dnk])
        P.add('dve', lambda e: e.reciprocal(out=dn_[:, 1:2], in_=dn_[:, 0:1]), w=[dnk])
        P.add('act', lambda e: e.activation(out=H[d][:, c, :], in_=bO[:, 0:128], func=AF.Copy, scale=dn_[:, 1:2]), r=[dnk], w=[kO], cw=['H%d' % d])

    for step in range(NT):
        for d in range(2):
            front(step, d)
        if step > 0:
            for d in range(2):
                back(step - 1, d)
    for d in range(2):
        back(NT - 1, d)
    P.mark('epi')
    Hf = H[0][:, :, :].rearrange("p c d -> p (c d)"); Hb = H[1][:, :, :].rearrange("p c d -> p (c d)")
    P.add('pool', lambda e: e.tensor_tensor(out=Hf, in0=Hf, in1=Hb, op=ALU.add), r=['H1'], w=['H0'])
    P.add('dve', lambda e: e.tensor_tensor(out=tmp, in0=Hf, in1=Hf, op=ALU.mult), r=['H0'], w=['tmp'])
    P.add('dve', lambda e: e.tensor_reduce(out=ssq, in_=tmp.rearrange("p (c d) -> p c d", c=NT), axis=AX.X, op=ALU.add), r=['tmp'], w=['ssq'])
    P.add('act', lambda e: e.activation(out=rst, in_=ssq, func=AF.Sqrt, scale=1.0 / 128, bias=CS[:, C_EPS:C_EPS + 1]), r=['ssq'], w=['rst'])
    P.add('dve', lambda e: e.reciprocal(out=rst, in_=rst), w=['rst'])
    P.add('act', lambda e: e.activation(out=oT, in_=oT, func=AF.Sigmoid), w=[oTk])
    for c in range(NT):
        P.add('act', lambda e, c=c: e.activation(out=H[1][:, c, :], in_=H[0][:, c, :], func=AF.Copy, scale=rst[:, c:c + 1]), r=['H0', 'rst'], cw=['H1'])
    for c in range(NT):
        bank = c % 8
        pk = self.ps[bank]
        P.add('pe', lambda e, c=c, pk=pk: e.transpose(pk[:, 0:128], H[1][:, c, :], ident), r=['H1', 'CS'], w=['ps%d' % bank])
        if colmaj and c >= 2:
            cl = c - 2
            dst = YT[:, TC:].rearrange("p (r c) -> p c r", c=64)[:, 4 * cl:4 * cl + 4, :]
            srcp = pk[:, 0:128].rearrange("p (c r) -> p c r", r=32)
        else:
            dst = YT[:, c * 128:(c + 1) * 128]
            srcp = pk[:, 0:128]
        P.add('dve', lambda e, dst=dst, srcp=srcp: e.tensor_scalar(out=dst, in0=srcp, scalar1=ghn[:, 0:1], scalar2=None, op0=ALU.mult),
              r=[ghk], w=['ps%d' % bank], cw=['YT'])
    P.add('pool', lambda e: e.tensor_tensor(out=YT, in0=YT, in1=oT, op=ALU.add if False else ALU.mult), r=[oTk], w=['YT'])
    P.dma('sp', self.yT[h * 128:(h + 1) * 128, :], YT, r=['YT'], cw=['yTd'])


K.phase2 = _phase2


def _phase34(self, l):
    P, CS = self.P, self.CS
    ones = CS[:, C_ONE:C_ONE + 128]
    o = [0]

    def cv(n):
        a = self.carve(o[0], n); o[0] += n
        return a
    WA = 2334
    cacg = [cv(2 * T) for _ in range(2)]
    ca = [cacg[j][:, 0:T] for j in range(2)]; cg = [cacg[j][:, T:2 * T] for j in range(2)]
    ypad = [cv(2364) for _ in range(2)]; acc = [cv(WA) for _ in range(2)]
    sqt = [cacg[j][:, 0:WA] for j in range(2)]
    ptmp = cv(WA)
    cw = [cv(31) for _ in range(2)]; cb = [cv(1) for _ in range(2)]; lg = [cv(1) for _ in range(2)]; lb = [cv(1) for _ in range(2)]
    mt = [cv(512) for _ in range(2)]; vt = [cv(512) for _ in range(2)]
    col = lambda v, j: v[l, j * 128:(j + 1) * 128].rearrange("(p o) -> p o", o=1)
    for j in range(2):
        P.dma('sp', ca[j], self.uT[2064 + j * 128:2064 + (j + 1) * 128, :], w=['ca%d' % j])
        P.dma('sp', cg[j], self.uT[2320 + j * 128:2320 + (j + 1) * 128, :], w=['cg%d' % j])
        P.dma('sp', cw[j], self.conv_wT[l, j * 128:(j + 1) * 128, :], w=['cw%d' % j])
        P.dma('sp', cb[j], col(self.conv_b, j), w=['cb%d' % j])
        P.dma('sp', lg[j], col(self.conv_ln_g, j), w=['lg%d' % j])
        P.dma('sp', lb[j], col(self.conv_ln_b, j), w=['lb%d' % j])
        P.add('pool', lambda e, j=j: e.memset(ypad[j], 0.0), w=['yp%d' % j])
        P.add('act', lambda e, j=j: e.activation(out=cg[j], in_=cg[j], func=AF.Sigmoid), w=['cg%d' % j])
        P.add('dve', lambda e, j=j: e.tensor_tensor(out=ypad[j][:, 15:15 + TC], in0=ca[j][:, 0:TC], in1=cg[j][:, 0:TC], op=ALU.mult),
              r=['ca%d' % j, 'cg%d' % j], w=['yp%d' % j])
        P.add('dve', lambda e, j=j: e.tensor_tensor(out=ypad[j][:, 301:301 + TL], in0=ca[j][:, TC:], in1=cg[j][:, TC:], op=ALU.mult),
              r=['ca%d' % j, 'cg%d' % j], w=['yp%d' % j])
    BD = CS[:, C_BD:C_BD + 256]
    DCc = CS[:, C_DC:C_DC + 512].rearrange("p (k n) -> p k n", k=2)
    DSc = CS[:, C_DC + 512:C_DC + 1024].rearrange("p (k n) -> p k n", k=2)
    fr = [cv(T) for _ in range(2)]
    Z = [cv(NT * 256).rearrange("p (c n) -> p c n", c=NT) for _ in range(2)]
    YF = fr
    DW = 128
    dcb = [cv(16 * DW).rearrange("p (k n) -> p k n", k=16) for _ in range(2)]
    dsb = [cv(16 * DW).rearrange("p (k n) -> p k n", k=16) for _ in range(2)]
    sc_c = 1.0 / 128.0
    sc_l = float(1.0 / np.sqrt(TL * 64.0))
    nb = 0
    for j in range(2):
        P.dma('sp', fr[j], self.uT[2576 + j * 128:2576 + (j + 1) * 128, :], w=['fr%d' % j])
        for i in range(NT):
            bank = 2 + nb % 6; nb += 1
            pb = self.ps[bank]
            s = sc_c if i < 2 else sc_l
            P.add('pe', lambda e, j=j, i=i, pb=pb: e.matmul(pb[:, 0:256], lhsT=fr[j][:, i * 128:(i + 1) * 128], rhs=BD, start=True, stop=True),
                  r=['fr%d' % j, 'CS'], w=['ps%d' % bank])
            P.add('act', lambda e, j=j, i=i, pb=pb, s=s: e.activation(out=Z[j][:, i, 0:128], in_=pb[:, 0:128], func=AF.Copy, scale=s),
                  w=['ps%d' % bank], cw=['Z%d' % j])
            P.add('act', lambda e, j=j, i=i, pb=pb, s=s: e.activation(out=Z[j][:, i, 128:256], in_=pb[:, 128:256], func=AF.Copy, scale=-s),
                  w=['ps%d' % bank], cw=['Z%d' % j])
    for j in range(2):
        bank = 2 + nb % 6; nb += 1
        pb = self.ps[bank]
        n = 0
        for i in range(2):
            for part, M in ((0, DCc), (1, DSc)):
                P.add('pe', lambda e, j=j, i=i, part=part, M=M, pb=pb, n=n: e.matmul(pb[:, 0:256], lhsT=Z[j][:, i, part * 128:(part + 1) * 128], rhs=M[:, i, :],
                                                                                 start=(n == 0), stop=(n == 3)), r=['Z%d' % j, 'CS'], w=['ps%d' % bank])
                n += 1
        P.add('act', lambda e, j=j, pb=pb: e.activation(out=YF[j][:, 0:TC], in_=pb[:, 0:256], func=AF.Copy), w=['ps%d' % bank], cw=['fr%d' % j])
    for tc in range(TL // DW):
        b = tc % 2
        cols = slice(tc * DW, (tc + 1) * DW)
        P.dma('sp', dcb[b], self.dftc[:, cols].rearrange("(k p) n -> p k n", p=128), w=['dcb%d' % b])
        P.dma('sp', dsb[b], self.dfts[:, cols].rearrange("(k p) n -> p k n", p=128), w=['dsb%d' % b])
        for j in range(2):
            bank = 2 + nb % 6; nb += 1
            pb = self.ps[bank]
            n = 0
            for i in range(16):
                for part, M, mk in ((0, dcb[b], 'dcb%d' % b), (1, dsb[b], 'dsb%d' % b)):
                    P.add('pe', lambda e, j=j, i=i, part=part, M=M, pb=pb, n=n: e.matmul(pb[:, 0:DW], lhsT=Z[j][:, i + 2, part * 128:(part + 1) * 128], rhs=M[:, i, :],
                                                                                     start=(n == 0), stop=(n == 31)), r=['Z%d' % j, mk], w=['ps%d' % bank])
                    n += 1
            P.add('act', lambda e, j=j, pb=pb, tc=tc: e.activation(out=YF[j][:, TC + tc * DW:TC + (tc + 1) * DW], in_=pb[:, 0:DW], func=AF.Copy),
                  w=['ps%d' % bank], cw=['fr%d' % j])
    for j in range(2):
        P.dma('sp', self.yT[768 + j * 128:768 + (j + 1) * 128, :], YF[j], r=['fr%d' % j], cw=['yTd'])


    for k in range(31):
        j = 0
        if k == 0:
            P.add('dve', lambda e, j=j: e.tensor_scalar(out=acc[j], in0=ypad[j][:, 0:WA], scalar1=cw[j][:, 0:1], scalar2=cb[j][:, 0:1], op0=ALU.mult, op1=ALU.add),
                  r=['yp0', 'cw0', 'cb0'], w=['acc0'])
        else:
            P.add('dve', lambda e, j=j, k=k: e.scalar_tensor_tensor(out=acc[j], in0=ypad[j][:, k:k + WA], scalar=cw[j][:, k:k + 1], in1=acc[j],
                                                                    op0=ALU.mult, op1=ALU.add), r=['yp0', 'cw0'], w=['acc0'])
        j = 1
        if k == 0:
            P.add('pool', lambda e, j=j: e.tensor_scalar(out=acc[j], in0=ypad[j][:, 0:WA], scalar1=cw[j][:, 0:1], scalar2=cb[j][:, 0:1], op0=ALU.mult, op1=ALU.add),
                  r=['yp1', 'cw1', 'cb1'], w=['acc1'])
        else:
            P.add('pool', lambda e, j=j, k=k: e.tensor_scalar(out=ptmp, in0=ypad[j][:, k:k + WA], scalar1=cw[j][:, k:k + 1], scalar2=0.0, op0=ALU.mult, op1=ALU.add),
                  r=['yp1', 'cw1'], w=['ptmp'])
            P.add('pool', lambda e, j=j: e.tensor_tensor(out=acc[j], in0=acc[j], in1=ptmp, op=ALU.add), w=['acc1', 'ptmp'])
    for j in range(2):
        P.add('act', lambda e, j=j: e.activation(out=sqt[j], in_=acc[j], func=AF.Square), r=['acc%d' % j], w=['sq%d' % j, 'ca%d' % j, 'cg%d' % j])
    chunks = [(0, 512), (512, 512), (1024, 512), (1536, 512), (2048, 286)]
    for ci, (a0, n) in enumerate(chunks):
        b1, b2 = self.ps[0], self.ps[1]
        k1, k2 = 'ps0', 'ps1'
        m_, v_ = mt[ci % 2], vt[ci % 2]
        mk, vk = 'mt%d' % (ci % 2), 'vt%d' % (ci % 2)
        for j in range(2):
            P.add('pe', lambda e, j=j, b1=b1, a0=a0, n=n: e.matmul(b1[:, 0:n], lhsT=ones, rhs=acc[j][:, a0:a0 + n], start=(j == 0), stop=(j == 1)),
                  r=['acc%d' % j, 'CS'], w=[k1])
        for j in range(2):
            P.add('pe', lambda e, j=j, b2=b2, a0=a0, n=n: e.matmul(b2[:, 0:n], lhsT=ones, rhs=sqt[j][:, a0:a0 + n], start=(j == 0), stop=(j == 1)),
                  r=['sq%d' % j, 'CS'], w=[k2])
        P.add('act', lambda e, b1=b1, m_=m_, n=n: e.activation(out=m_[:, 0:n], in_=b1[:, 0:n], func=AF.Copy, scale=1.0 / 256), w=[k1, mk])
        P.add('dve', lambda e, m_=m_, v_=v_, n=n: e.tensor_tensor(out=v_[:, 0:n], in0=m_[:, 0:n], in1=m_[:, 0:n], op=ALU.mult), r=[mk], w=[vk])
        P.add('dve', lambda e, b2=b2, v_=v_, n=n: e.scalar_tensor_tensor(out=v_[:, 0:n], in0=b2[:, 0:n], scalar=1.0 / 256, in1=v_[:, 0:n],
                                                                         op0=ALU.mult, op1=ALU.subtract), w=[k2, vk])
        P.add('act', lambda e, v_=v_, n=n: e.activation(out=v_[:, 0:n], in_=v_[:, 0:n], func=AF.Sqrt, bias=CS[:, C_EPS:C_EPS + 1]), w=[vk])
        P.add('dve', lambda e, v_=v_, n=n: e.reciprocal(out=v_[:, 0:n], in_=v_[:, 0:n]), w=[vk])
        for j in range(2):
            eng = 'dve' if j == 0 else 'pool'
            P.add(eng, lambda e, j=j, m_=m_, a0=a0, n=n: e.tensor_tensor(out=acc[j][:, a0:a0 + n], in0=acc[j][:, a0:a0 + n], in1=m_[:, 0:n], op=ALU.subtract),
                  r=[mk], w=['acc%d' % j])
            P.add(eng, lambda e, j=j, v_=v_, a0=a0, n=n: e.tensor_tensor(out=acc[j][:, a0:a0 + n], in0=acc[j][:, a0:a0 + n], in1=v_[:, 0:n], op=ALU.mult),
                  r=[vk], w=['acc%d' % j])
            P.add('act', lambda e, j=j, a0=a0, n=n: e.activation(out=acc[j][:, a0:a0 + n], in_=acc[j][:, a0:a0 + n], func=AF.Silu,
                                                                 scale=lg[j][:, 0:1], bias=lb[j][:, 0:1]), r=['lg%d' % j, 'lb%d' % j], w=['acc%d' % j])
    for j in range(2):
        P.dma('sp', self.yT[512 + j * 128:512 + (j + 1) * 128, 0:TC], acc[j][:, 0:TC], r=['acc%d' % j], cw=['yTd'])
        P.dma('sp', self.yT[512 + j * 128:512 + (j + 1) * 128, TC:T], acc[j][:, 286:286 + TL], r=['acc%d' % j], cw=['yTd'])
    P.barrier()


def _phase5(self, l):
    P, CS = self.P, self.CS
    o = [0]

    def cv(n):
        a = self.carve(o[0], n); o[0] += n
        return a
    YA = cv(8 * T).rearrange("p (k t) -> p k t", k=8)
    wo = [cv(4096).rearrange("p (k n) -> p k n", k=8) for _ in range(2)]
    gt = {'l': cv(1024), 'c': cv(1024)}
    xt = [cv(1024) for _ in range(2)]
    tm = [cv(1024) for _ in range(2)]
    for kc in range(8):
        P.dma('sp' if kc % 2 == 0 else 'pool', YA[:, kc, :], self.yT[kc * 128:(kc + 1) * 128, :], w=['YA%d' % kc])
    for hf in range(2):
        P.dma('sp', wo[hf], self.w_out[l, :, hf * 512:(hf + 1) * 512].rearrange("(kc p) n -> p kc n", p=128), w=['wo%d' % hf])
    P.dma('sp', gt['l'], self.modrows[l, 2, 0, :].partition_broadcast(128), w=['gtl'])
    P.dma('sp', gt['c'], self.modrows[l, 2, 1, :].partition_broadcast(128), w=['gtc'])
    nb = 0
    for i in range(NT):
        b = i % 2
        isc = i < 2
        if l == 0:
            src = self.ctx_in[i * 128:(i + 1) * 128, :] if isc else self.x_in[(i - 2) * 128:(i - 1) * 128, :]
        else:
            src = self.xres[i * 128:(i + 1) * 128, :]
        P.dma('sp', xt[b], src, w=['xt%d' % b])
        g_ = gt['c' if isc else 'l']; gk = 'gtc' if isc else 'gtl'
        for hf in range(2):
            bank = nb % 8; nb += 1
            pb = self.ps[bank]
            for kc in range(8):
                P.add('pe', lambda e, i=i, kc=kc, hf=hf, pb=pb: e.matmul(pb[:, 0:512], lhsT=YA[:, kc, i * 128:(i + 1) * 128], rhs=wo[hf][:, kc, :],
                                                                       start=(kc == 0), stop=(kc == 7)), r=['YA%d' % kc, 'wo%d' % hf], w=['ps%d' % bank])
            P.add('dve', lambda e, b=b, hf=hf, pb=pb, g_=g_: e.tensor_tensor(out=tm[b][:, hf * 512:(hf + 1) * 512], in0=pb[:, 0:512],
                                                                            in1=g_[:, hf * 512:(hf + 1) * 512], op=ALU.mult), r=[gk], w=['ps%d' % bank, 'tm%d' % b])
        P.add('pool', lambda e, b=b: e.tensor_tensor(out=tm[b], in0=tm[b], in1=xt[b], op=ALU.add), r=['xt%d' % b], w=['tm%d' % b])
        P.dma('pool', self.xres[i * 128:(i + 1) * 128, :], tm[b], r=['tm%d' % b], cw=['xres'])
    P.barrier()


K.phase34 = _phase34
K.phase5 = _phase5


BIG = 8192.0
AWTOP = 50600 - 288
C_EOFF, C_CAPT, C_KCAP, C_TOK = NCONST, NCONST + 288, NCONST + 576, NCONST + 578
NCONST2 = NCONST + 578 + 18


def make_consts2():
    c = np.zeros((128, NCONST2), np.float32)
    c[:, :NCONST] = make_consts()
    eoff = np.zeros((18, 16), np.float32); capt = np.zeros((18, 16), np.float32)
    for i in range(18):
        for e in range(16):
            eoff[i, e] = e * NSLOT + (0 if i < 2 else CAPC)
            capt[i, e] = CAPC if i < 2 else CAPL
    c[:, C_EOFF:C_EOFF + 288] = eoff.reshape(1, 288)
    c[:, C_CAPT:C_CAPT + 288] = capt.reshape(1, 288)
    c[:, C_KCAP] = CAPC; c[:, C_KCAP + 1] = CAPL
    tok = (np.arange(18)[None, :] * 128 + np.arange(128)[:, None]).astype(np.int32)
    c[:, C_TOK:C_TOK + 18] = tok.view(np.float32)
    return c


def _phase56(self, l):
    P, CS = self.P, self.CS
    ident = CS[:, C_ID:C_ID + 128]
    o = [0]

    def cv(n):
        a = self.carve(o[0], n); o[0] += n
        return a
    aff = cv(288).rearrange("p (c e) -> p c e", c=NT)
    YA = cv(8 * T).rearrange("p (k t) -> p k t", k=8)
    wo = [cv(4096).rearrange("p (k n) -> p k n", k=8) for _ in range(2)]
    gt = {'l': cv(1024), 'c': cv(1024)}
    NB5 = 3
    xt = [cv(1024) for _ in range(NB5)]
    tm = [cv(1024) for _ in range(NB5)]
    modt = {}
    for nm, seg, row in (('gs_l', 4, 0), ('sh_l', 3, 0), ('gs_c', 4, 1), ('sh_c', 3, 1)):
        modt[nm] = cv(1024)
        P.dma('sp', modt[nm], self.modrows[l, seg, row, :].partition_broadcast(128), w=[nm])
    wr = cv(128).rearrange("p (k n) -> p k n", k=8)
    P.dma('sp', wr, self.w_router[l, :, :].rearrange("(kc p) n -> p kc n", p=128), w=['wr'])
    zt = cv(1024)
    P.add('pool', lambda e: e.memset(zt, 0.0), w=['zt'])
    for i in range(NT):
        P.dma('pool', self.macc[i * 128:(i + 1) * 128, :], zt, r=['zt'], cw=['macc'])
    xr = [cv(1042) for _ in range(2)]
    xT = [cv(1024).rearrange("p (k t) -> p k t", k=8) for _ in range(2)]
    sq = cv(1024)
    st = [cv(8) for _ in range(2)]
    ex = cv(16)
    for kc in range(8):
        P.dma('sp' if kc % 2 == 0 else 'pool', YA[:, kc, :], self.yT[kc * 128:(kc + 1) * 128, :], w=['YA%d' % kc])
    for hf in range(2):
        P.dma('sp', wo[hf], self.w_out[l, :, hf * 512:(hf + 1) * 512].rearrange("(kc p) n -> p kc n", p=128), w=['wo%d' % hf])
    P.dma('sp', gt['l'], self.modrows[l, 2, 0, :].partition_broadcast(128), w=['gtl'])
    P.dma('sp', gt['c'], self.modrows[l, 2, 1, :].partition_broadcast(128), w=['gtc'])
    nbc = [0]

    abanks = {}

    def stageA_pe(i):
        b = i % NB5
        isc = i < 2
        if l == 0:
            src = self.ctx_in[i * 128:(i + 1) * 128, :] if isc else self.x_in[(i - 2) * 128:(i - 1) * 128, :]
        else:
            src = self.xres[i * 128:(i + 1) * 128, :]
        P.dma('sp', xt[b], src, w=['xt%d' % b])
        abanks[i] = []
        for hf in range(2):
            bank = 4 + nbc[0] % 4; nbc[0] += 1
            abanks[i].append(bank)
            pb = self.ps[bank]
            for kc in range(8):
                P.add('pe', lambda e, i=i, kc=kc, hf=hf, pb=pb: e.matmul(pb[:, 0:512], lhsT=YA[:, kc, i * 128:(i + 1) * 128], rhs=wo[hf][:, kc, :],
                                                                       start=(kc == 0), stop=(kc == 7)), r=['YA%d' % kc, 'wo%d' % hf], w=['ps%d' % bank])

    def stageA_post(i):
        b = i % NB5
        isc = i < 2
        g_ = gt['c' if isc else 'l']; gk = 'gtc' if isc else 'gtl'
        for hf in range(2):
            bank = abanks[i][hf]
            pb = self.ps[bank]
            P.add('dve', lambda e, b=b, hf=hf, pb=pb, g_=g_: e.tensor_tensor(out=tm[b][:, hf * 512:(hf + 1) * 512], in0=pb[:, 0:512],
                                                                            in1=g_[:, hf * 512:(hf + 1) * 512], op=ALU.mult), r=[gk], w=['ps%d' % bank, 'tm%d' % b])
        P.add('pool', lambda e, b=b: e.tensor_tensor(out=tm[b], in0=tm[b], in1=xt[b], op=ALU.add), r=['xt%d' % b], w=['tm%d' % b])
        P.dma('pool', self.xres[i * 128:(i + 1) * 128, :], tm[b], r=['tm%d' % b], cw=['xres'])

    def stageB1(i):
        b = i % 2
        bt = i % NB5
        isc = i < 2
        x_, xr_, st_, xT_ = tm[bt], xr[b], st[b], xT[b]
        xk = 'tm%d' % bt
        P.dma('pool', xr_[:, 1040:1041], self.consts[:, C_TOK + i:C_TOK + i + 1], cw=['xrk%d' % b], allow_slow_non_contiguous=True)
        P.add('act', lambda e, x_=x_, st_=st_: e.activation(out=sq, in_=x_, func=AF.Square, accum_out=st_[:, 0:1]), r=[xk], w=['sq', 'st%d' % b])
        P.add('act', lambda e, st_=st_: e.activation(out=st_[:, 1:2], in_=st_[:, 0:1], func=AF.Sqrt, scale=1.0 / D, bias=CS[:, C_EPS:C_EPS + 1]), w=['st%d' % b])
        P.add('dve', lambda e, st_=st_: e.reciprocal(out=st_[:, 2:3], in_=st_[:, 1:2]), w=['st%d' % b])
        gs = modt['gs_c' if isc else 'gs_l']; sh = modt['sh_c' if isc else 'sh_l']
        gk2 = 'gs_c' if isc else 'gs_l'; sk2 = 'sh_c' if isc else 'sh_l'
        P.add('dve', lambda e, x_=x_, xr_=xr_, st_=st_, gs=gs: e.scalar_tensor_tensor(out=xr_[:, 0:1024], in0=x_, scalar=st_[:, 2:3], in1=gs, op0=ALU.mult, op1=ALU.mult),
              r=[xk, 'st%d' % b, gk2], w=['xr%d' % b])
        P.add('pool', lambda e, xr_=xr_, sh=sh: e.tensor_tensor(out=xr_[:, 0:1024], in0=xr_[:, 0:1024], in1=sh, op=ALU.add), r=[sk2], w=['xr%d' % b])
        for hb in range(2):
            pb = self.ps[hb]
            for k4 in range(4):
                kc = hb * 4 + k4
                P.add('pe', lambda e, pb=pb, k4=k4, kc=kc, xr_=xr_: e.transpose(pb[:, k4 * 128:(k4 + 1) * 128], xr_[:, kc * 128:(kc + 1) * 128], ident),
                      r=['xr%d' % b, 'CS'], w=['ps%d' % hb])
            dst = xT_[:, hb * 4:hb * 4 + 4, :]
            srcp = pb[:, :].rearrange("p (k t) -> p k t", k=4)
            if hb == 0:
                P.add('act', lambda e, dst=dst, srcp=srcp: e.activation(out=dst, in_=srcp, func=AF.Copy), w=['ps0'], cw=['xT%d' % b])
            else:
                P.add('dve', lambda e, dst=dst, srcp=srcp: e.tensor_copy(out=dst, in_=srcp), w=['ps1'], cw=['xT%d' % b])
        p2 = self.ps[2 + b]
        for kc in range(8):
            P.add('pe', lambda e, kc=kc, p2=p2, xT_=xT_: e.matmul(p2[:, 0:16], lhsT=xT_[:, kc, :], rhs=wr[:, kc, :], start=(kc == 0), stop=(kc == 7)),
                  r=['xT%d' % b, 'wr'], w=['ps%d' % (2 + b)])
    def stageB2(i):
        b = i % 2
        xr_, st_ = xr[b], st[b]
        p2 = self.ps[2 + b]
        P.add('dve', lambda e, p2=p2, st_=st_: e.tensor_reduce(out=st_[:, 3:4], in_=p2[:, 0:16], axis=AX.X, op=ALU.max), w=['ps%d' % (2 + b), 'st%d' % b])
        P.add('dve', lambda e, st_=st_: e.tensor_scalar(out=st_[:, 4:5], in0=st_[:, 3:4], scalar1=-1.0, scalar2=None, op0=ALU.mult), w=['st%d' % b])
        P.add('act', lambda e, p2=p2, st_=st_: e.activation(out=ex, in_=p2[:, 0:16], func=AF.Exp, bias=st_[:, 4:5], accum_out=st_[:, 5:6]),
              w=['ps%d' % (2 + b), 'st%d' % b, 'ex'])
        P.add('dve', lambda e, st_=st_: e.reciprocal(out=st_[:, 6:7], in_=st_[:, 5:6]), w=['st%d' % b])
        P.add('dve', lambda e, i=i, st_=st_: e.tensor_scalar(out=aff[:, i, :], in0=ex, scalar1=st_[:, 6:7], scalar2=None, op0=ALU.mult),
              r=['st%d' % b], w=['ex'], cw=['aff'])
        P.add('pool', lambda e, i=i, xr_=xr_: e.tensor_copy(out=xr_[:, 1024:1040], in_=aff[:, i, :]), r=['aff'], cw=['xrk%d' % b])
        P.dma('pool', self.xn2x[i * 128:(i + 1) * 128, :], xr_, r=['xr%d' % b, 'xrk%d' % b], cw=['xn2x'])
    blocks = list(range(NT))
    if l == L - 1:
        blocks = list(range(2, NT))
        P.add('dve', lambda e: e.memset(aff[:, 0:2, :], 0.0), cw=['aff'])
    stageA_pe(blocks[0])
    stageA_post(blocks[0])
    for j, i in enumerate(blocks):
        nxt = blocks[j + 1] if j + 1 < len(blocks) else None
        if nxt is not None:
            stageA_pe(nxt)
        stageB1(i)
        if nxt is not None:
            stageA_post(nxt)
        stageB2(i)
    P.barrier()


def _phase6b(self, l):
    P, CS = self.P, self.CS
    ident = CS[:, C_ID:C_ID + 128]; ones = CS[:, C_ONE:C_ONE + 128]; triS = CS[:, C_TS:C_TS + 128]
    o = [0]

    def cv(n):
        a = self.carve(o[0], n); o[0] += n
        return a
    aff = cv(288).rearrange("p (c e) -> p c e", c=NT)
    affT = cv(T)
    for i in range(NT):
        bank = 4 + (i // 4) % 4
        pb = self.ps[bank]
        P.add('pe', lambda e, i=i, pb=pb: e.transpose(pb[0:16, (i % 4) * 128:(i % 4 + 1) * 128], aff[:, i, :], ident), r=['aff', 'CS'], w=['ps%d' % bank])
        if i % 4 == 3 or i == NT - 1:
            i0 = (i // 4) * 4
            n = (i - i0 + 1) * 128
            P.add('act', lambda e, pb=pb, i0=i0, n=n: e.activation(out=affT[0:16, i0 * 128:i0 * 128 + n], in_=pb[0:16, 0:n], func=AF.Copy),
                  w=['ps%d' % bank], cw=['affT'])
    lo = cv(2); mid = cv(2); cnt = cv(2); ge = cv(2); junk = cv(TL)
    kcap = CS[0:16, C_KCAP:C_KCAP + 2]
    P.add('dve', lambda e: e.memset(lo[0:16, :], 0.0), w=['lo'])
    segs = [(0, TC), (TC, TL)]
    bg_steps = []
    LAG = 3
    for it in range(1, 31):
        wv = float(2.0 ** (-it))
        k_ = it - 1
        if k_ < len(bg_steps):
            bg_steps[k_][0]()
        if 0 <= k_ - LAG < len(bg_steps) and bg_steps[k_ - LAG][1] is not None:
            bg_steps[k_ - LAG][1]()
        P.add('dve', lambda e, wv=wv: e.tensor_scalar(out=mid[0:16, :], in0=lo[0:16, :], scalar1=wv, scalar2=None, op0=ALU.add), r=['lo'], w=['mid'])
        for s, (a0, n) in enumerate(segs):
            P.add('dve', lambda e, s=s, a0=a0, n=n: e.tensor_scalar(out=junk[0:16, 0:n], in0=affT[0:16, a0:a0 + n], scalar1=mid[0:16, s:s + 1], scalar2=None,
                                                                    op0=ALU.is_ge, op1=ALU.add, accum_out=cnt[0:16, s:s + 1]),
                  r=['affT', 'mid'], w=['junk', 'cnt'])
        P.add('dve', lambda e: e.tensor_tensor(out=ge[0:16, :], in0=cnt[0:16, :], in1=kcap, op=ALU.is_ge), r=['cnt', 'CS'], w=['ge'])
        P.add('dve', lambda e, wv=wv: e.scalar_tensor_tensor(out=lo[0:16, :], in0=ge[0:16, :], scalar=wv, in1=lo[0:16, :], op0=ALU.mult, op1=ALU.add),
              r=['ge'], w=['lo'])
    for k_ in range(30 - LAG, len(bg_steps)):
        if k_ >= 0 and bg_steps[k_][1] is not None:
            bg_steps[k_][1]()
    Dg = cv(32); thb = cv(32)
    for s in range(2):
        P.add('dve', lambda e, s=s: e.tensor_scalar(out=Dg[0:16, s * 16:(s + 1) * 16], in0=ident[0:16, 0:16], scalar1=lo[0:16, s:s + 1], scalar2=None, op0=ALU.mult),
              r=['lo', 'CS'], w=['Dg'])
    p0 = self.ps[0]
    P.add('pe', lambda e: e.matmul(p0[:, 0:32], lhsT=ones[0:16, :], rhs=Dg[0:16, :], start=True, stop=True), r=['Dg', 'CS'], w=['ps0'])
    P.add('act', lambda e: e.activation(out=thb, in_=p0[:, 0:32], func=AF.Copy), w=['ps0', 'thb'])
    mask = cv(288).rearrange("p (c e) -> p c e", c=NT)
    for i in range(NT):
        s = 0 if i < 2 else 1
        if i < 2 and l == L - 1:
            P.add('dve', lambda e, i=i: e.memset(mask[:, i, :], 0.0), cw=['mask'])
            continue
        P.add('dve', lambda e, i=i, s=s: e.tensor_tensor(out=mask[:, i, :], in0=aff[:, i, :], in1=thb[:, s * 16:(s + 1) * 16], op=ALU.is_ge),
              r=['aff', 'thb'], cw=['mask'])
    maskf = mask[:, :, :].rearrange("p c e -> p (c e)")
    p1 = self.ps[1]; p2 = self.ps[2]
    P.add('pe', lambda e: e.matmul(p1[:, 0:288], lhsT=triS, rhs=maskf, start=True, stop=True), r=['mask', 'CS'], w=['ps1'])
    P.add('pe', lambda e: e.matmul(p2[:, 0:288], lhsT=ones, rhs=maskf, start=True, stop=True), r=['mask', 'CS'], w=['ps2'])
    tot = cv(288).rearrange("p (c e) -> p c e", c=NT)
    base = cv(288).rearrange("p (c e) -> p c e", c=NT)
    P.add('act', lambda e: e.activation(out=tot[:, :, :].rearrange("p c e -> p (c e)"), in_=p2[:, 0:288], func=AF.Copy), w=['ps2', 'tot'])
    P.add('dve', lambda e: e.memset(base[:, :, :].rearrange("p c e -> p (c e)"), 0.0), w=['base'])
    P.add('dve', lambda e: e.tensor_copy(out=base[:, 1, :], in_=tot[:, 0, :]), r=['tot'], w=['base'])
    for i in range(3, NT):
        P.add('dve', lambda e, i=i: e.tensor_tensor(out=base[:, i, :], in0=base[:, i - 1, :], in1=tot[:, i - 1, :], op=ALU.add), r=['tot'], w=['base'])
    rank = cv(288); val = cv(288)
    idx = self.carve(AWTOP, 288)
    basef = base[:, :, :].rearrange("p c e -> p (c e)")
    P.add('dve', lambda e: e.tensor_tensor(out=rank, in0=p1[:, 0:288], in1=basef, op=ALU.add), r=['base'], w=['ps1', 'rank'])
    P.add('dve', lambda e: e.tensor_tensor(out=val, in0=rank, in1=CS[:, C_CAPT:C_CAPT + 288], op=ALU.is_lt), r=['rank', 'CS'], w=['val'])
    P.add('dve', lambda e: e.tensor_tensor(out=val, in0=val, in1=maskf, op=ALU.mult), r=['mask'], w=['val'])
    P.add('dve', lambda e: e.tensor_tensor(out=rank, in0=rank, in1=CS[:, C_EOFF:C_EOFF + 288], op=ALU.add), r=['CS'], w=['rank'])
    P.add('dve', lambda e: e.tensor_scalar(out=rank, in0=rank, scalar1=-BIG, scalar2=None, op0=ALU.add), w=['rank'])
    P.add('dve', lambda e: e.tensor_tensor(out=rank, in0=rank, in1=val, op=ALU.mult), r=['val'], w=['rank'])
    P.add('dve', lambda e: e.tensor_scalar(out=rank, in0=rank, scalar1=BIG, scalar2=None, op0=ALU.add), w=['rank'])
    idxi = idx.bitcast(I32)
    P.add('dve', lambda e: e.tensor_copy(out=idxi, in_=rank), r=['rank'], w=['idx'])
    if 'idx' in self.tapd and l == 0:
        P.dma('sp', self.tapd['idx'][:, :], rank, r=['rank'], w=['tapi'])
    P.barrier()


K.phase56 = _phase56
K.phase6b = _phase6b


def _phase8(self, l):
    P, CS = self.P, self.CS
    ident = CS[:, C_ID:C_ID + 128]
    o = [0]

    def cv(n):
        a = self.carve(o[0], n); o[0] += n
        return a
    W = 256
    lastl = (l == L - 1)
    S0 = CAPC if lastl else 0
    NS = NSLOT - S0
    T0 = 2 if lastl else 0
    NWB = 6
    wbuf = [cv(8 * W).rearrange("p (k n) -> p k n", k=8) for _ in range(NWB)]
    xgf = [cv(3 * 1042) for _ in range(2)]
    xsT = cv(8 * NSLOT).rearrange("p (k t) -> p k t", k=8)
    hidT = cv(8 * NSLOT).rearrange("p (k t) -> p k t", k=8)
    ysb = [cv(3 * 1024) for _ in range(2)]
    tmpg = cv(NSLOT)
    xrr = [cv(1042) for _ in range(NT)]
    assert o[0] <= AWTOP, o[0]
    idxi = self.carve(AWTOP, 288).bitcast(I32)
    for i in range(NT):
        P.dma('sp' if i % 2 == 0 else 'act', xrr[i], self.xn2x[i * 128:(i + 1) * 128, :], w=['xrr%d' % i])

    def scatter(ex_):
        for i in range(T0, NT):
            P.add('pool', lambda e, i=i, ex_=ex_: e.indirect_dma_start(
                out=self.xs[:, :], out_offset=bass.IndirectOffsetOnAxis(ap=idxi[:, i * 16 + ex_:i * 16 + ex_ + 1], axis=0),
                in_=xrr[i][:, :], in_offset=None, bounds_check=self.bcreg(e), oob_is_err=False),
                r=['xrr%d' % i], cw=['xs%d' % ex_], dma=True)
    AHEAD = 4
    for ex_ in range(AHEAD):
        scatter(ex_)
    CH = [(32, 128), (160, 128)] if lastl else [(0, 128), (128, 128), (256, 32)]
    cnt = {'nw': 0, 'nd': 0, 'nbk': 0}

    def load_xg(ex_):
        b = ex_ % 2
        xg = xgf[b]
        for ch, (r0, rows) in enumerate(CH):
            P.dma('sp', xg[0:rows, ch * 1042:(ch + 1) * 1042], self.xs[ex_ * NSLOT + r0:ex_ * NSLOT + r0 + rows, :], r=['xs%d' % ex_], cw=['xg%d' % b])

    def transp(ex_):
        b = ex_ % 2
        xg = xgf[b]
        for ch, (r0, rows) in enumerate(CH):
            for hb in range(2):
                bank = cnt['nbk'] % 2; cnt['nbk'] += 1
                pb = self.ps[bank]
                for k4 in range(4):
                    kc = hb * 4 + k4
                    P.add('pe', lambda e, pb=pb, k4=k4, kc=kc, xg=xg, ch=ch, rows=rows: e.transpose(
                        pb[:, k4 * 128:k4 * 128 + rows], xg[0:rows, ch * 1042 + kc * 128:ch * 1042 + (kc + 1) * 128], ident[0:rows, 0:rows]),
                        r=['xg%d' % b, 'CS'], w=['ps%d' % bank])
                dst = xsT[:, hb * 4:hb * 4 + 4, r0 - S0:r0 - S0 + rows]
                srcp = pb[:, :].rearrange("p (k t) -> p k t", k=4)[:, :, 0:rows]
                if bank == 0:
                    P.add('act', lambda e, dst=dst, srcp=srcp: e.activation(out=dst, in_=srcp, func=AF.Copy), w=['ps%d' % bank], cw=['xsT'])
                else:
                    P.add('dve', lambda e, dst=dst, srcp=srcp: e.tensor_copy(out=dst, in_=srcp), w=['ps%d' % bank], cw=['xsT'])

    def gate_up(ex_):
        for fq in range(D // W):
            ig = cnt['nw'] % NWB; cnt['nw'] += 1
            iu = cnt['nw'] % NWB; cnt['nw'] += 1
            wgt, wut = wbuf[ig], wbuf[iu]
            gk_, uk_ = 'wbuf%d' % ig, 'wbuf%d' % iu
            P.dma('sp', wgt, self.w_e_gate[l, ex_, :, fq * W:(fq + 1) * W].rearrange("(kc p) n -> p kc n", p=128), w=[gk_])
            P.dma('sp', wut, self.w_e_up[l, ex_, :, fq * W:(fq + 1) * W].rearrange("(kc p) n -> p kc n", p=128), w=[uk_])
            if fq == 1 and ex_ + 1 < NE:
                load_xg(ex_ + 1)
            for fc in range(W // 128):
                fglob = fq * (W // 128) + fc
                bg_, bu_ = 2 + fglob % 2, 4 + fglob % 2
                pg, pu = self.ps[bg_], self.ps[bu_]
                for kc in range(8):
                    P.add('pe', lambda e, pg=pg, kc=kc, fc=fc, wgt=wgt: e.matmul(pg[:, 0:NS], lhsT=wgt[:, kc, fc * 128:(fc + 1) * 128], rhs=xsT[:, kc, 0:NS],
                                                                            start=(kc == 0), stop=(kc == 7)), r=[gk_, 'xsT'], w=['ps%d' % bg_])
                for kc in range(8):
                    P.add('pe', lambda e, pu=pu, kc=kc, fc=fc, wut=wut: e.matmul(pu[:, 0:NS], lhsT=wut[:, kc, fc * 128:(fc + 1) * 128], rhs=xsT[:, kc, 0:NS],
                                                                            start=(kc == 0), stop=(kc == 7)), r=[uk_, 'xsT'], w=['ps%d' % bu_])
                P.add('act', lambda e, pg=pg: e.activation(out=tmpg[:, 0:NS], in_=pg[:, 0:NS], func=AF.Silu), w=['ps%d' % bg_, 'tmpg'])
                P.add('dve', lambda e, pu=pu, fglob=fglob: e.tensor_tensor(out=hidT[:, fglob, 0:NS], in0=pu[:, 0:NS], in1=tmpg[:, 0:NS], op=ALU.mult),
                      r=['tmpg'], w=['ps%d' % bu_], cw=['hidT'])

    def down(ex_):
        b = ex_ % 2
        xg = xgf[b]
        for dq in range(D // W):
            idn = cnt['nw'] % NWB; cnt['nw'] += 1
            wdt = wbuf[idn]; dk_ = 'wbuf%d' % idn
            P.dma('sp', wdt, self.w_e_down[l, ex_, :, dq * W:(dq + 1) * W].rearrange("(kc p) n -> p kc n", p=128), w=[dk_])
            for ch, (r0, rows) in enumerate(CH):
                bank = 6 + (ch + dq) % 2
                pd = self.ps[bank]
                for fc in range(8):
                    P.add('pe', lambda e, pd=pd, fc=fc, wdt=wdt, r0=r0, rows=rows: e.matmul(pd[0:rows, 0:W], lhsT=hidT[:, fc, r0 - S0:r0 - S0 + rows], rhs=wdt[:, fc, :],
                                                                                      start=(fc == 0), stop=(fc == 7)), r=['hidT', dk_], w=['ps%d' % bank])
                gcol = xg[0:rows, ch * 1042 + 1024 + ex_:ch * 1042 + 1025 + ex_]
                dst = ysb[b][0:rows, ch * 1024 + dq * W:ch * 1024 + (dq + 1) * W]
                if (ch + dq) % 2 == 0:
                    P.add('act', lambda e, pd=pd, dst=dst, gcol=gcol, rows=rows: e.activation(out=dst, in_=pd[0:rows, 0:W], func=AF.Copy, scale=gcol),
                          r=['xg%d' % b], w=['ps%d' % bank], cw=['ysb%d' % b])
                else:
                    P.add('dve', lambda e, pd=pd, dst=dst, gcol=gcol, rows=rows: e.tensor_scalar(out=dst, in0=pd[0:rows, 0:W], scalar1=gcol, scalar2=None, op0=ALU.mult),
                          r=['xg%d' % b], w=['ps%d' % bank], cw=['ysb%d' % b])

    def scatter_add(ex_):
        b = ex_ % 2
        xg = xgf[b]
        for ch, (r0, rows) in enumerate(CH):
            icol = xg[0:rows, ch * 1042 + 1040:ch * 1042 + 1041].bitcast(I32)
            src = ysb[b][0:rows, ch * 1024:(ch + 1) * 1024]
            P.add('pool', lambda e, icol=icol, src=src: e.indirect_dma_start(
                out=self.macc[:, :], out_offset=bass.IndirectOffsetOnAxis(ap=icol, axis=0), in_=src, in_offset=None, compute_op=ALU.add),
                r=['ysb%d' % b, 'xg%d' % b], w=['macc'], dma=True)

    load_xg(0)
    transp(0)
    for ex_ in range(NE):
        gate_up(ex_)
        if ex_ + 1 < NE:
            transp(ex_ + 1)
        down(ex_)
        if ex_ + AHEAD < NE:
            scatter(ex_ + AHEAD)
        scatter_add(ex_)
    P.barrier()


def _phase9(self, l):
    P, CS = self.P, self.CS
    last = (l == L - 1)
    o = [0]

    def cv(n):
        a = self.carve(o[0], n); o[0] += n
        return a
    gt = {'l': cv(1024), 'c': cv(1024)}
    gf = cv(1024)
    P.dma('sp', gt['l'], self.modrows[l, 5, 0, :].partition_broadcast(128), w=['gtl'])
    P.dma('sp', gt['c'], self.modrows[l, 5, 1, :].partition_broadcast(128), w=['gtc'])
    P.dma('sp', gf, self.g_final[:].partition_broadcast(128), w=['gf'])
    NB9 = 4
    xt = [cv(1024) for _ in range(NB9)]; mt = [cv(1024) for _ in range(NB9)]; sqs = [cv(1024) for _ in range(NB9)]
    st = [cv(4) for _ in range(NB9)]
    def s1(i):
        b = i % NB9
        isc = i < 2
        sq = sqs[b]
        g_ = gt['c' if isc else 'l']; gk = 'gtc' if isc else 'gtl'
        x_, m_, st_ = xt[b], mt[b], st[b]
        P.dma('sp', x_, self.xres[i * 128:(i + 1) * 128, :], w=['xt%d' % b])
        P.dma('pool', m_, self.macc[i * 128:(i + 1) * 128, :], w=['mt%d' % b])
        P.add('dve', lambda e: e.tensor_tensor(out=m_, in0=m_, in1=g_, op=ALU.mult), r=[gk], w=['mt%d' % b])
        P.add('pool', lambda e: e.tensor_tensor(out=m_, in0=m_, in1=x_, op=ALU.add), r=['xt%d' % b], w=['mt%d' % b])
        if last:
            P.add('act', lambda e: e.activation(out=sq, in_=m_, func=AF.Square, accum_out=st_[:, 0:1]), r=['mt%d' % b], w=['sq%d' % b, 'st%d' % b])
            P.add('act', lambda e: e.activation(out=st_[:, 1:2], in_=st_[:, 0:1], func=AF.Sqrt, scale=1.0 / D, bias=CS[:, C_EPS:C_EPS + 1]), w=['st%d' % b])

    def s2(i):
        b = i % NB9
        m_, st_ = mt[b], st[b]
        if not last:
            P.dma('act', self.xres[i * 128:(i + 1) * 128, :], m_, r=['mt%d' % b], cw=['xres'])
        else:
            P.add('dve', lambda e: e.reciprocal(out=st_[:, 2:3], in_=st_[:, 1:2]), w=['st%d' % b])
            P.add('dve', lambda e: e.scalar_tensor_tensor(out=m_, in0=m_, scalar=st_[:, 2:3], in1=gf, op0=ALU.mult, op1=ALU.mult),
                  r=['st%d' % b, 'gf'], w=['mt%d' % b])
            P.dma('act', self.out[(i - 2) * 128:(i - 1) * 128, :], m_, r=['mt%d' % b], cw=['out'])
    blocks = [i for i in range(NT) if not (last and i < 2)]
    DEP = 2
    for j in range(min(DEP, len(blocks))):
        s1(blocks[j])
    for j, i in enumerate(blocks):
        if j + DEP < len(blocks):
            s1(blocks[j + DEP])
        s2(i)
    P.barrier()


K.phase8 = _phase8
K.phase9 = _phase9


def kernel(**inputs):
    kk = K(stop_after=None)
    nc = kk.build()
    consts = make_consts2()
    dc, ds = make_dft()
    in_maps = []
    for b in range(8):
        m = host_inputs(inputs, b)
        m['consts'] = consts
        m['dftc'] = dc
        m['dfts'] = ds
        in_maps.append(m)
    res = run_bass_kernel_spmd(nc, in_maps, core_ids=list(range(8)))
    return np.stack([np.asarray(res.results[b]['out']) for b in range(8)], 0).astype(np.float32)
```

```python
from concourse.bass_utils import run_bass_kernel_spmd
import numpy as np
import concourse.bass as bass
import concourse.mybir as mybir
from contextlib import ExitStack

F32 = mybir.dt.float32
F32R = mybir.dt.float32r
I32 = mybir.dt.int32
U32 = mybir.dt.uint32
AF = mybir.ActivationFunctionType
ALU = mybir.AluOpType
AX = mybir.AxisListType

ENG_ATTR = {'pe': 'tensor', 'dve': 'vector', 'act': 'scalar', 'pool': 'gpsimd', 'sp': 'sync'}


class _Res:
    __slots__ = ('name', 'writers', 'readers', 'semi')

    def __init__(self, name):
        self.name = name
        self.writers = []
        self.readers = []
        self.semi = {}


class _Op:
    __slots__ = ('eng', 'fn', 'dma_res', 'dma_sem', 'waits_c', 'waits_s', 'signal', 'sig')

    def __init__(self, eng, fn):
        self.eng = eng
        self.fn = fn
        self.dma_res = None
        self.dma_sem = None
        self.waits_c = []
        self.waits_s = []
        self.signal = False
        self.sig = 0


class Prog:
    def __init__(self, nc):
        self.nc = nc
        self.q = {e: [] for e in ENG_ATTR}
        self.res = {}
        self.nops = 0
        self.capturing = None
        self.semcnt = []
        self.semcls = []
        self.free_sems = {'hw': [], 'sw': []}

    def _r(self, key):
        r = self.res.get(key)
        if r is None:
            r = self.res[key] = _Res(key)
        return r

    def mark(self, name):
        if self.capturing is not None:
            self.capturing.append(('mark', name))

    def replay(self, ops):
        for o in ops:
            self.add(*o[0], **o[1])

    def replay_interleaved(self, a, b):
        ia = ib = 0
        na, nb = len(a), len(b)
        while ia < na or ib < nb:
            if ib >= nb or (ia < na and ia * nb <= ib * na):
                self.add(*a[ia][0], **a[ia][1]); ia += 1
            else:
                self.add(*b[ib][0], **b[ib][1]); ib += 1

    def add(self, eng, fn, r=(), w=(), cw=(), dma=False):
        if self.capturing is not None:
            self.capturing.append(((eng, fn), dict(r=r, w=w, cw=cw, dma=dma)))
            return None
        op = _Op(eng, fn)
        deps = []
        for key in r:
            R = self._r(key)
            deps.extend(R.writers)
            R.readers.append(op)
        for key in w:
            R = self._r(key)
            deps.extend(R.writers)
            deps.extend(R.readers)
            R.writers = [op]
            R.readers = []
        for key in cw:
            R = self._r(key)
            if R.readers:
                deps.extend(R.readers)
                R.writers = [op]
                R.readers = []
            else:
                R.writers.append(op)
        for d in deps:
            if d is op:
                continue
            if d.dma_res is not None:
                si = d.dma_sem
                op.waits_s.append((si, self.semcnt[si]))
            else:
                if d.eng == eng and eng == 'pe':
                    continue
                d.signal = True
                op.waits_c.append(d)
        if dma:
            keys = list(w) + list(cw)
            assert len(keys) == 1, keys
            R = self._r(keys[0])
            cls = 'sw' if eng == 'pool' else 'hw'
            si = R.semi.get(cls)
            if si is None:
                if self.free_sems[cls]:
                    si = self.free_sems[cls].pop()
                else:
                    self.semcnt.append(0)
                    self.semcls.append(cls)
                    si = len(self.semcnt) - 1
                R.semi[cls] = si
            self.semcnt[si] += 1
            op.dma_res = R
            op.dma_sem = si
        self.q[eng].append(op)
        self.nops += 1
        return op

    def dma(self, eng, out, in_, r=(), w=(), cw=(), **kw):
        return self.add(eng, lambda e: e.dma_start(out=out, in_=in_, **kw), r=r, w=w, cw=cw, dma=True)

    def barrier(self):
        lasts = []
        for e in ('pe', 'dve', 'act', 'pool'):
            for o in reversed(self.q[e]):
                if o.dma_res is None:
                    lasts.append(o)
                    break
        dres = [(si, self.semcnt[si]) for R in self.res.values() for si in R.semi.values()]
        for e in ENG_ATTR:
            op = _Op(e, lambda en: en.nop())
            for d in lasts:
                if d.dma_res is None:
                    d.signal = True
                    op.waits_c.append(d)
            op.waits_s = dres
            self.q[e].append(op)
        self.res = {}
        self.free_sems = {c: [i for i in range(len(self.semcnt)) if self.semcls[i] == c] for c in ('hw', 'sw')}

    def emit(self):
        nc = self.nc
        with ExitStack() as es:
            csem = {}
            for e in ('pe', 'dve', 'act', 'pool'):
                csem[e] = es.enter_context(nc.semaphore('c_' + e))
            dsem = [es.enter_context(nc.semaphore('d%d' % i)) for i in range(len(self.semcnt))]
            for e, ops in self.q.items():
                n = 0
                for op in ops:
                    if op.signal:
                        n += 1
                        op.sig = n
            q = self.q

            def run(eng_name, e):
                waited = {}
                for op in q[eng_name]:
                    for d in op.waits_c:
                        s = csem[d.eng]
                        if waited.get(d.eng, 0) < d.sig:
                            e.wait_ge(s, d.sig)
                            waited[d.eng] = d.sig
                    for si, cnt in op.waits_s:
                        v = 16 * cnt
                        s = dsem[si]
                        if waited.get(si, 0) < v:
                            e.wait_ge(s, v)
                            waited[si] = v
                    ins = op.fn(e)
                    if op.dma_sem is not None:
                        ins.then_inc(dsem[op.dma_sem], 16)
                    elif op.signal:
                        ins.then_inc(csem[eng_name], 1)

            with nc.Block() as block:
                @block.tensor
                def _(e):
                    run('pe', e)

                @block.vector
                def _(e):
                    run('dve', e)

                @block.scalar
                def _(e):
                    run('act', e)

                @block.gpsimd
                def _(e):
                    run('pool', e)

                @block.sync
                def _(e):
                    run('sp', e)
        return len(self.semcnt)


import os

L = 2
D = 1024
TL = 2048
TC = 256
T = TL + TC
NT = T // 128
INC = 2832
NE = 16
CAPL, CAPC = 256, 32
NSLOT = CAPL + CAPC
EPS = 1e-6
TCH = [(0, 512), (512, 512), (1024, 512), (1536, 512), (2048, 256)]
CHUNKS = ([(i * 128, 128) for i in range(12)] + [(1536, 16)] +
          [(1552 + i * 128, 128) for i in range(4)] + [(2064 + i * 128, 128) for i in range(2)] +
          [(2320 + i * 128, 128) for i in range(2)] + [(2576 + i * 128, 128) for i in range(2)])

C_ID, C_ONE, C_TU, C_TL, C_TS, C_BD, C_DC, C_EPS = 0, 128, 256, 384, 512, 640, 896, 1920
NCONST = 1924
NCONST2 = NCONST + 578 + 18


def make_consts():
    c = np.zeros((128, NCONST), np.float32)
    i = np.arange(128)
    c[:, C_ID:C_ID + 128] = np.eye(128)
    c[:, C_ONE:C_ONE + 128] = 1.0
    c[:, C_TU:C_TU + 128] = (i[:, None] <= i[None, :])
    c[:, C_TL:C_TL + 128] = (i[:, None] >= i[None, :])
    c[:, C_TS:C_TS + 128] = (i[:, None] < i[None, :])
    j = np.arange(64)
    ang = 2 * np.pi * np.outer(j, j) / 64.0
    bc = np.zeros((128, 128)); bs = np.zeros((128, 128))
    for g in range(2):
        bc[g * 64:(g + 1) * 64, g * 64:(g + 1) * 64] = np.cos(ang)
        bs[g * 64:(g + 1) * 64, g * 64:(g + 1) * 64] = np.sin(ang)
    c[:, C_BD:C_BD + 128] = bc
    c[:, C_BD + 128:C_BD + 256] = bs
    t = np.arange(256)
    a2 = 2 * np.pi * np.outer(t, t) / 256.0
    cc = np.cos(a2).reshape(2, 128, 256).transpose(1, 0, 2).reshape(128, 512)
    ss = np.sin(a2).reshape(2, 128, 256).transpose(1, 0, 2).reshape(128, 512)
    c[:, C_DC:C_DC + 512] = cc
    c[:, C_DC + 512:C_DC + 1024] = ss
    c[:, C_EPS] = EPS
    c[:, C_EPS + 1] = 1.0
    c[:, C_EPS + 2] = -0.5 * np.log(128.0)
    return c


def make_dft():
    t = np.arange(TL, dtype=np.float64)
    a = 2 * np.pi * ((np.outer(t, t)) % TL) / TL
    return np.cos(a).astype(np.float32), np.sin(a).astype(np.float32)


class K:
    def __init__(self, stop_after=None, taps=(), ne_decl=NE):
        self.stop_after = stop_after
        self.taps = taps
        nc = self.nc = bass.Bass("TRN2", target_bir_lowering=False)
        self.P = Prog(nc)
        dt = lambda n, s, kind="ExternalInput", d=F32: nc.dram_tensor(n, s, d, kind=kind).ap()
        self.x_in = dt("x", [TL, D]); self.ctx_in = dt("ctx", [TC, D]); self.cT = dt("cT", [128, 16])
        self.w_ada = dt("w_ada", [L, D, 6 * D]); self.b_ada = dt("b_ada", [L, 6 * D])
        self.g_norm1 = dt("g_norm1", [L, D]); self.w_in = dt("w_in", [L, D, INC])
        self.b_gates = dt("b_gates", [L, 16]); self.g_hnorm = dt("g_hnorm", [L, 512])
        self.conv_wT = dt("conv_wT", [L, 256, 31]); self.conv_b = dt("conv_b", [L, 256])
        self.conv_ln_g = dt("conv_ln_g", [L, 256]); self.conv_ln_b = dt("conv_ln_b", [L, 256])
        self.w_out = dt("w_out", [L, D, D]); self.g_norm2 = dt("g_norm2", [L, D])
        self.w_router = dt("w_router", [L, D, NE])
        self.w_e_gate = dt("w_e_gate", [L, ne_decl, D, D]); self.w_e_up = dt("w_e_up", [L, ne_decl, D, D])
        self.w_e_down = dt("w_e_down", [L, ne_decl, D, D]); self.g_final = dt("g_final", [D])
        self.consts = dt("consts", [128, NCONST2]); self.dftc = dt("dftc", [TL, TL]); self.dfts = dt("dfts", [TL, TL])
        self.out = dt("out", [TL, D], kind="ExternalOutput")
        self.modrows = dt("modrows", [L, 6, 2, D], kind="Internal")
        self.uT = dt("uT", [INC, T], kind="Internal")
        self.yT = dt("yT", [D, T], kind="Internal")
        self.xres = dt("xres", [T, D], kind="Internal")
        self.xn2x = dt("xn2x", [T, 1042], kind="Internal")
        self.xs = dt("xs", [NE * NSLOT, 1042], kind="Internal")
        self.macc = dt("macc", [T, D], kind="Internal")
        self.tapd = {}
        for name, shape in taps:
            self.tapd[name] = dt("tap_" + name, shape, kind="ExternalOutput")

    def build(self):
        nc, P = self.nc, self.P
        with ExitStack() as es:
            AW = 50600
            self.A = es.enter_context(nc.sbuf_tensor("arena", [128, AW], F32))
            self.CS = es.enter_context(nc.sbuf_tensor("cs", [128, NCONST2], F32))
            self.ps = [es.enter_context(nc.psum_tensor("ps%d" % i, [128, 512], F32)) for i in range(8)]
            P.dma('sp', self.CS[:, :], self.consts[:, :], w=['CS'])
            P.barrier()
            self.phases()
            P.barrier()
            nsem = P.emit()
            print("ops", P.nops, "dma sems", nsem)
        return nc

    def tap_yT(self):
        if 'yT' in self.tapd:
            self.P.dma('sp', self.tapd['yT'][:, :], self.yT[:, :], w=['tapy'])
            self.P.barrier()

    def tap_xres(self):
        if 'xres' in self.tapd:
            self.P.dma('sp', self.tapd['xres'][:, :], self.xres[:, :], w=['tapxr'])
            self.P.barrier()

    def bcreg(self, e):
        if getattr(self, '_bcr', None) is None:
            self._bcr = e.to_reg(NE * NSLOT - 1)
        return self._bcr

    def carve(self, off, n):
        return self.A[:, off:off + n]

    def phases(self):
        self.phase0(0)
        self.phase0(1)
        if self.stop_after == 0:
            return
        for l in range(L):
            self.phase1(l)
            if self.stop_after == 1:
                return
            caps = []
            for h in range(4):
                self.P.capturing = []
                self.phase2(l, h)
                ops = self.P.capturing
                self.P.capturing = None
                parts = {'pro': [], 'chain': [], 'epi': []}
                cur = 'pro'
                for o_ in ops:
                    if o_[0] == 'mark':
                        cur = o_[1]
                    else:
                        parts[cur].append(o_)
                caps.append(parts)
            self.P.replay(caps[0]['pro'])
            for h in range(4):
                self.P.replay(caps[h]['chain'])
                if h < 3:
                    self.P.replay_interleaved(caps[h]['epi'], caps[h + 1]['pro'])
                else:
                    self.P.replay(caps[h]['epi'])
            self.P.barrier()
            if self.stop_after == 2:
                self.tap_yT()
                return
            self.phase34(l)
            if self.stop_after == 4:
                self.tap_yT()
                return
            self.phase56(l)
            if self.stop_after == 5:
                self.tap_xres()
                return
            self.phase6b(l)
            if self.stop_after in (6, 61):
                return
            self.phase8(l)
            if l == L - 1:
                self.phase9(l)
            if self.stop_after == 9:
                self.tap_xres()
                return

    def phase0_steps(self, l):
        P, CS = self.P, self.CS
        sc = self.carve(20000, 16)
        sc2 = self.carve(20016, 16)
        wb = [self.carve(21024 + i * 4096, 4096).rearrange("p (k n) -> p k n", k=8) for i in range(2)]
        brow = [self.carve(30000 + i * 1024, 1024) for i in range(2)]
        grow = [self.carve(32048 + i * 1024, 1024) for i in range(2)]
        mrow = [self.carve(34096 + i * 1024, 1024) for i in range(2)]
        sc2v = sc2.rearrange("p (k c) -> p k c", c=2)
        steps = []

        def init():
            P.dma('sp', sc, self.cT[:, :], w=['sc'])
            P.add('act', lambda e: e.activation(out=sc2, in_=sc, func=AF.Silu), r=['sc'], w=['sc2'])
        steps.append((init, None))
        for seg in range(6):
            for half in range(2):
                def pe_step(seg=seg, half=half):
                    sb = seg % 2
                    if half == 0:
                        P.dma('sp', brow[sb][0:2, :], self.b_ada[l, seg * D:(seg + 1) * D].partition_broadcast(2), w=['brow%d' % sb])
                        if seg in (1, 4):
                            g = self.g_norm1 if seg == 1 else self.g_norm2
                            P.dma('sp', grow[sb][0:2, :], g[l, :].partition_broadcast(2), w=['grow%d' % sb])
                    n0 = seg * D + half * 512
                    b = half
                    wt = wb[b]
                    P.dma('sp', wt, self.w_ada[l, :, n0:n0 + 512].rearrange("(kc p) n -> p kc n", p=128), w=['wb%d' % b])
                    pb = self.ps[6 + b]
                    for kc in range(8):
                        P.add('pe', lambda e, kc=kc, wt=wt, pb=pb: e.matmul(pb[0:2, :], lhsT=sc2v[:, kc, :], rhs=wt[:, kc, :],
                                                                            start=(kc == 0), stop=(kc == 7)),
                              r=['sc2', 'wb%d' % b], w=['ps%d' % (6 + b)])

                def post_step(seg=seg, half=half):
                    sb = seg % 2
                    b = half
                    pb = self.ps[6 + b]
                    mr = mrow[sb]; mk = 'mrow%d' % sb
                    P.add('dve', lambda e: e.tensor_tensor(out=mr[0:2, half * 512:(half + 1) * 512], in0=pb[0:2, :],
                                                           in1=brow[sb][0:2, half * 512:(half + 1) * 512], op=ALU.add),
                          r=['brow%d' % sb], w=['ps%d' % (6 + b), mk])
                    if half == 1:
                        if seg in (1, 4):
                            P.add('dve', lambda e: e.scalar_tensor_tensor(out=mr[0:2, :], in0=mr[0:2, :], scalar=1.0, in1=grow[sb][0:2, :],
                                                                          op0=ALU.add, op1=ALU.mult), r=['grow%d' % sb], w=[mk])
                        P.dma('pool', self.modrows[l, seg, :, :], mr[0:2, :], r=[mk], cw=['modrows'])
                steps.append((pe_step, post_step))
        return steps

    def phase0(self, l):
        for pe_step, post_step in self.phase0_steps(l):
            pe_step()
            if post_step is not None:
                post_step()
        self.P.barrier()

    def phase1(self, l):
        P, CS = self.P, self.CS
        ident = CS[:, C_ID:C_ID + 128]
        xnT = self.carve(0, 8 * T).rearrange("p (k t) -> p k t", k=8)
        o = 8 * T
        modt = {}
        for nm, seg, row in (('gs_l', 1, 0), ('sh_l', 0, 0), ('gs_c', 1, 1), ('sh_c', 0, 1)):
            modt[nm] = self.carve(o, 1024); o += 1024
            P.dma('sp', modt[nm], self.modrows[l, seg, row, :].partition_broadcast(128), w=[nm])
        NB1 = 3
        xt = [self.carve(o + i * 1024, 1024) for i in range(NB1)]; o += NB1 * 1024
        xn = [self.carve(o + i * 1024, 1024) for i in range(NB1)]; o += NB1 * 1024
        sqs = [self.carve(o + i * 1024, 1024) for i in range(NB1)]; o += NB1 * 1024
        st = [self.carve(o + i * 4, 4) for i in range(NB1)]; o += 4 * NB1
        wbs = [self.carve(o + i * 1024, 1024).rearrange("p (k n) -> p k n", k=8) for i in range(3)]; o += 3072
        stg = [self.carve(o + i * 512, 512) for i in range(4)]; o += 2048
        if l > 0:
            mtl = [self.carve(o + i * 1024, 1024) for i in range(NB1)]; o += NB1 * 1024
            gt2 = {'l': self.carve(o, 1024), 'c': self.carve(o + 1024, 1024)}; o += 2048
            P.dma('sp', gt2['l'], self.modrows[l - 1, 5, 0, :].partition_broadcast(128), w=['gt2l'])
            P.dma('sp', gt2['c'], self.modrows[l - 1, 5, 1, :].partition_broadcast(128), w=['gt2c'])
        NPRE = int(os.environ.get('K_NPRE', '0'))
        nbc = [0]

        def loadw(ci):
            c0, w = CHUNKS[ci]
            wt = wbs[ci % 3]
            P.dma('sp', wt[:, :, 0:w], self.w_in[l, :, c0:c0 + w].rearrange("(kc p) n -> p kc n", p=128), w=['wbs%d' % (ci % 3)])

        def proj(ci, tci):
            c0, w = CHUNKS[ci]
            t0, n = TCH[tci]
            if l == L - 1 and c0 >= 1552 and tci == 0:
                t0, n = TC, 512 - TC
            wt = wbs[ci % 3]; wk = 'wbs%d' % (ci % 3)
            bank = 2 + nbc[0] % 6; sb = nbc[0] % 4; nbc[0] += 1
            pb = self.ps[bank]
            rk = ['xnT%d' % i for i in range(t0 // 128, (t0 + n) // 128)] + [wk]
            for kc in range(8):
                lh, rh = wt[:, kc, 0:w], xnT[:, kc, t0:t0 + n]
                P.add('pe', lambda e, pb=pb, lh=lh, rh=rh, kc=kc, w=w, n=n: e.matmul(pb[0:w, 0:n], lhsT=lh, rhs=rh, start=(kc == 0), stop=(kc == 7)),
                      r=rk, w=['ps%d' % bank])
            sg = stg[sb]
            if nbc[0] % 2 == 0:
                P.add('act', lambda e, sg=sg, pb=pb, w=w, n=n: e.activation(out=sg[0:w, 0:n], in_=pb[0:w, 0:n], func=AF.Copy),
                      w=['ps%d' % bank, 'stg%d' % sb])
            else:
                P.add('dve', lambda e, sg=sg, pb=pb, w=w, n=n: e.tensor_copy(out=sg[0:w, 0:n], in_=pb[0:w, 0:n]),
                      w=['ps%d' % bank, 'stg%d' % sb])
            P.dma('pool', self.uT[c0:c0 + w, t0:t0 + n], sg[0:w, 0:n], r=['stg%d' % sb], cw=['uT'])
        for ci in range(NPRE):
            loadw(ci)
        ready = {3: 0, 7: 1, 11: 2, 15: 3, 17: 4}
        def normN(i):
            b = i % NB1
            isc = i < 2
            sq = sqs[b]
            if l == 0:
                src = self.ctx_in[i * 128:(i + 1) * 128, :] if isc else self.x_in[(i - 2) * 128:(i - 1) * 128, :]
            else:
                src = self.xres[i * 128:(i + 1) * 128, :]
            x_, xn_, st_ = xt[b], xn[b], st[b]
            P.dma('sp', x_, src, w=['xt%d' % b])
            if l > 0:
                m_ = mtl[b]
                g2 = gt2['c' if isc else 'l']; g2k = 'gt2c' if isc else 'gt2l'
                P.dma('sp', m_, self.macc[i * 128:(i + 1) * 128, :], w=['mtl%d' % b])
                P.add('dve', lambda e, m_=m_, g2=g2: e.tensor_tensor(out=m_, in0=m_, in1=g2, op=ALU.mult), r=[g2k], w=['mtl%d' % b])
                P.add('pool', lambda e, m_=m_, x_=x_: e.tensor_tensor(out=x_, in0=m_, in1=x_, op=ALU.add), r=['mtl%d' % b], w=['xt%d' % b])
                P.dma('pool', self.xres[i * 128:(i + 1) * 128, :], x_, r=['xt%d' % b], cw=['xres'])
            P.add('act', lambda e, x_=x_, st_=st_, sq=sq: e.activation(out=sq, in_=x_, func=AF.Square, accum_out=st_[:, 0:1]),
                  r=['xt%d' % b], w=['sq%d' % b, 'st%d' % b])
            P.add('act', lambda e, st_=st_: e.activation(out=st_[:, 1:2], in_=st_[:, 0:1], func=AF.Sqrt, scale=1.0 / D,
                                                         bias=CS[:, C_EPS:C_EPS + 1]), w=['st%d' % b])
            P.add('dve', lambda e, st_=st_: e.reciprocal(out=st_[:, 2:3], in_=st_[:, 1:2]), w=['st%d' % b])
            gs = modt['gs_c' if isc else 'gs_l']; sh = modt['sh_c' if isc else 'sh_l']
            gk = 'gs_c' if isc else 'gs_l'; sk = 'sh_c' if isc else 'sh_l'
            P.add('dve', lambda e, x_=x_, xn_=xn_, st_=st_, gs=gs: e.scalar_tensor_tensor(out=xn_, in0=x_, scalar=st_[:, 2:3], in1=gs,
                                                                                           op0=ALU.mult, op1=ALU.mult),
                  r=['xt%d' % b, 'st%d' % b, gk], w=['xn%d' % b])
            P.add('pool', lambda e, xn_=xn_, sh=sh: e.tensor_tensor(out=xn_, in0=xn_, in1=sh, op=ALU.add), r=[sk], w=['xn%d' % b])

        def transX(i):
            b = i % NB1
            xn_ = xn[b]
            for hb in range(2):
                pb = self.ps[hb]
                for k4 in range(4):
                    kc = hb * 4 + k4
                    P.add('pe', lambda e, pb=pb, k4=k4, kc=kc, xn_=xn_: e.transpose(pb[:, k4 * 128:(k4 + 1) * 128], xn_[:, kc * 128:(kc + 1) * 128], ident),
                          r=['xn%d' % b, 'CS'], w=['ps%d' % hb])
                dst = xnT[:, hb * 4:hb * 4 + 4, i * 128:(i + 1) * 128]
                srcp = pb[:, :].rearrange("p (k t) -> p k t", k=4)
                if hb == 0:
                    P.add('act', lambda e, dst=dst, srcp=srcp: e.activation(out=dst, in_=srcp, func=AF.Copy), w=['ps0'], cw=['xnT%d' % i])
                else:
                    P.add('dve', lambda e, dst=dst, srcp=srcp: e.tensor_copy(out=dst, in_=srcp), w=['ps1'], cw=['xnT%d' % i])
            if i in ready:
                for ci in range(NPRE):
                    proj(ci, ready[i])
        normN(0)
        for i in range(NT):
            if i + 1 < NT:
                normN(i + 1)
            transX(i)
        if 'xnT' in self.tapd and l == 0:
            P.dma('sp', self.tapd['xnT'].rearrange("(k p) t -> p k t", p=128), xnT, r=['xnT%d' % i for i in range(NT)], w=['tapx'])
        for ci in range(NPRE, len(CHUNKS)):
            loadw(ci)
            for tci in range(len(TCH)):
                proj(ci, tci)
        P.barrier()
        if 'uT' in self.tapd and l == 0:
            P.dma('sp', self.tapd['uT'][:, :], self.uT[:, :], w=['tapu'])
            P.barrier()


def host_inputs(inp, b):
    cT = np.stack([np.asarray(inp['c'][b]).reshape(8, 128).T, np.asarray(inp['c_ctx']).reshape(8, 128).T], axis=-1)
    m = {
        'x': np.ascontiguousarray(inp['x'][b]), 'ctx': np.ascontiguousarray(inp['ctx'][b]),
        'cT': np.ascontiguousarray(cT.reshape(128, 16)).astype(np.float32),
        'b_gates': np.asarray(inp['b_gates']).reshape(L, 16),
    }
    m['conv_wT'] = np.ascontiguousarray(np.asarray(inp['conv_w']).transpose(0, 2, 1))
    for k in ('w_ada', 'b_ada', 'g_norm1', 'w_in', 'g_hnorm', 'conv_b', 'conv_ln_g', 'conv_ln_b', 'w_out', 'g_norm2',
              'w_router', 'w_e_gate', 'w_e_up', 'w_e_down', 'g_final'):
        m[k] = np.asarray(inp[k])
    return m


def _phase2(self, l, h):
    P, CS = self.P, self.CS
    ident = CS[:, C_ID:C_ID + 128]; ones = CS[:, C_ONE:C_ONE + 128]
    triU = CS[:, C_TU:C_TU + 128]; triL = CS[:, C_TL:C_TL + 128]
    o = [0]

    def cv(n):
        a = self.carve(o[0], n); o[0] += n
        return a
    rawbuf = [[cv(T) for _ in range(3)] for _ in range(2)]
    raw = rawbuf[h % 2]
    rk = ['raw%d_%d' % (h % 2, j) for j in range(3)]
    colmaj = h >= 2
    scanbuf = [cv(T) for _ in range(3)]
    scan = scanbuf if colmaj else raw
    Q, Kt, Vt = scan
    g4 = cv(T); g4s_ = cv(T); g4s = g4s_ if colmaj else g4
    ktm = cv(NT * 128).rearrange("p (c d) -> p c d", c=NT)
    vp = [cv(NT * 130).rearrange("p (c d) -> p c d", c=NT) for _ in range(2)]
    H = [cv(NT * 128).rearrange("p (c d) -> p c d", c=NT) for _ in range(2)]
    oTb = [cv(T) for _ in range(2)]; oT = oTb[h % 2]; oTk = 'oT%d' % (h % 2)
    YT = cv(T)
    bg = cv(4); ghnb = [cv(1) for _ in range(2)]; ghn = ghnb[h % 2]; ghk = 'ghn%d' % (h % 2)
    G = [cv(NT) for _ in range(4)]
    nlf = [cv(NT) for _ in range(2)]
    totS = [cv(NT) for _ in range(2)]
    dd = [cv(NT) for _ in range(2)]
    flo = [cv(NT) for _ in range(2)]
    wk = [cv(NT) for _ in range(2)]
    dec = [cv(NT) for _ in range(2)]
    Cst = [cv(130) for _ in range(2)]
    Cd2 = [[cv(130) for _ in range(2)] for _ in range(2)]
    STm2 = [[cv(128) for _ in range(2)] for _ in range(2)]
    dn2 = [[cv(2) for _ in range(2)] for _ in range(2)]
    ssq = cv(NT); rst = cv(NT); tmp = cv(NT * 128)
    for j, base in enumerate((0, 512, 1024)):
        P.dma('sp', raw[j], self.uT[base + h * 128: base + (h + 1) * 128, :], w=[rk[j]])
    P.dma('sp', g4[0:4, :], self.uT[1536 + 4 * h:1536 + 4 * h + 4, :], w=['g4'])
    P.dma('sp', oT, self.uT[1552 + h * 128:1552 + (h + 1) * 128, :], w=[oTk])
    P.dma('sp', bg, self.b_gates[l, 4 * h:4 * h + 4].partition_broadcast(128), w=['bg'])
    P.dma('sp', ghn, self.g_hnorm[l, h * 128:(h + 1) * 128].rearrange("(p o) -> p o", o=1), w=[ghk])
    if colmaj:
        for j in range(3):
            eng = ('pool', 'dve', 'act')[j]
            s_, d_ = raw[j], scan[j]
            if eng == 'act':
                P.add(eng, lambda e, s_=s_, d_=d_: e.activation(out=d_[:, 0:TC], in_=s_[:, 0:TC], func=AF.Copy), r=[rk[j]], w=['scanA%d' % j])
                P.add(eng, lambda e, s_=s_, d_=d_: e.activation(out=d_[:, TC:].rearrange("p (c r) -> p c r", r=32),
                                                                in_=s_[:, TC:].rearrange("p (r c) -> p c r", c=64), func=AF.Copy),
                      r=[rk[j]], w=['scan%d' % j])
            else:
                P.add(eng, lambda e, s_=s_, d_=d_: e.tensor_copy(out=d_[:, 0:TC], in_=s_[:, 0:TC]), r=[rk[j]], w=['scanA%d' % j])
                P.add(eng, lambda e, s_=s_, d_=d_: e.tensor_copy(out=d_[:, TC:].rearrange("p (c r) -> p c r", r=32),
                                                                 in_=s_[:, TC:].rearrange("p (r c) -> p c r", c=64)),
                      r=[rk[j]], w=['scan%d' % j])
        P.add('pool', lambda e: e.tensor_copy(out=g4s[0:4, 0:TC], in_=g4[0:4, 0:TC]), r=['g4'], w=['g4sA'])
        P.add('pool', lambda e: e.tensor_copy(out=g4s[0:4, TC:].rearrange("p (c r) -> p c r", r=32),
                                              in_=g4[0:4, TC:].rearrange("p (r c) -> p c r", c=64)), r=['g4'], w=['g4s'])
        sk = [['scan%d' % j, 'scanA%d' % j] for j in range(3)]
        gk = ['g4s', 'g4sA']
    else:
        sk = [[rk[j]] for j in range(3)]
        gk = ['g4']
    pb = self.ps[0]
    for c in range(NT):
        P.add('pe', lambda e, c=c: e.transpose(pb[:, c * 4:(c + 1) * 4], g4s[0:4, c * 128:(c + 1) * 128], ident[0:4, 0:4]), r=gk + ['CS'], w=['ps0'])
    pv = pb[:, 0:NT * 4].rearrange("p (c g) -> p c g", g=4)
    for g in range(4):
        P.add('dve', lambda e, g=g: e.tensor_scalar(out=G[g], in0=pv[:, :, g], scalar1=bg[:, g:g + 1], scalar2=None, op0=ALU.add),
              r=['bg'], w=['ps0', 'G%d' % g])
    p1 = self.ps[1]
    for d in range(2):
        Fg = G[1 + 2 * d]; Ig = G[2 * d]
        P.add('act', lambda e, d=d, Fg=Fg: e.activation(out=nlf[d], in_=Fg, func=AF.Exp, scale=-1.0), r=['G%d' % (1 + 2 * d)], w=['nlf%d' % d])
        P.add('act', lambda e, d=d: e.activation(out=nlf[d], in_=nlf[d], func=AF.Ln, bias=CS[:, C_EPS + 1:C_EPS + 2]), w=['nlf%d' % d])
        tri = triU if d == 0 else triL
        P.add('pe', lambda e, d=d, tri=tri: e.matmul(p1[:, d * 64:d * 64 + NT], lhsT=tri, rhs=nlf[d], start=True, stop=True), r=['nlf%d' % d, 'CS'], w=['ps1'])
        P.add('pe', lambda e, d=d: e.matmul(p1[:, d * 64 + 32:d * 64 + 32 + NT], lhsT=ones, rhs=nlf[d], start=True, stop=True), r=['nlf%d' % d, 'CS'], w=['ps1'])
        P.add('act', lambda e, d=d: e.activation(out=totS[d], in_=p1[:, d * 64 + 32:d * 64 + 32 + NT], func=AF.Copy), w=['ps1', 'tot%d' % d])
        P.add('dve', lambda e, d=d: e.tensor_tensor(out=dd[d], in0=p1[:, d * 64:d * 64 + NT], in1=totS[d], op=ALU.subtract), r=['tot%d' % d], w=['ps1', 'dd%d' % d])
        P.add('act', lambda e, d=d: e.activation(out=flo[d], in_=dd[d], func=AF.Exp), r=['dd%d' % d], w=['flo%d' % d])
        P.add('dve', lambda e, d=d, Ig=Ig: e.tensor_tensor(out=wk[d], in0=dd[d], in1=Ig, op=ALU.add), r=['dd%d' % d, 'G%d' % (2 * d)], w=['wk%d' % d])
        P.add('act', lambda e, d=d: e.activation(out=wk[d], in_=wk[d], func=AF.Exp, bias=CS[:, C_EPS + 2:C_EPS + 3]), w=['wk%d' % d])
        P.add('act', lambda e, d=d: e.activation(out=dec[d], in_=totS[d], func=AF.Exp, scale=-1.0), r=['tot%d' % d], w=['dec%d' % d])
        P.add('pool', lambda e, d=d: e.memset(vp[d][:, :, 128:130], 0.0), w=['vpx%d' % d])
        P.add('pool', lambda e, d=d: e.memset(Cst[d], 0.0), w=['C%d' % d])
    for d in range(2):
        P.add('dve', lambda e, d=d: e.tensor_copy(out=vp[d][:, :, 128], in_=wk[d]), r=['wk%d' % d], w=['vpx%d' % d])
    nb = 0
    for c in range(NT):
        bank = 2 + nb % 6; nb += 1
        pk = self.ps[bank]
        P.add('pe', lambda e, c=c, pk=pk: e.transpose(pk[:, 0:128], Kt[:, c * 128:(c + 1) * 128], ident), r=sk[1] + ['CS'], w=['ps%d' % bank])
        P.add('pe', lambda e, c=c, pk=pk: e.transpose(pk[:, 128:256], Vt[:, c * 128:(c + 1) * 128], ident), r=sk[2] + ['CS'], w=['ps%d' % bank])
        P.add('act', lambda e, c=c, pk=pk: e.activation(out=ktm[:, c, :], in_=pk[:, 0:128], func=AF.Copy), w=['ps%d' % bank], cw=['ktm'])
        P.add('dve', lambda e, c=c, pk=pk: e.tensor_scalar(out=vp[0][:, c, 0:128], in0=pk[:, 128:256], scalar1=wk[0][:, c:c + 1], scalar2=None, op0=ALU.mult),
              r=['wk0'], w=['ps%d' % bank], cw=['vp0'])
        P.add('act', lambda e, c=c, pk=pk: e.activation(out=vp[1][:, c, 0:128], in_=pk[:, 128:256], func=AF.Copy, scale=wk[1][:, c:c + 1]),
              r=['wk1'], w=['ps%d' % bank], cw=['vp1'])
    P.mark('chain')
    order = [list(range(NT)), [1, 0] + list(range(NT - 1, 1, -1))]

    def front(step, d):
        c = order[d][step]
        cs = slice(c * 128, (c + 1) * 128)
        bST, bO, bC = self.ps[4 * d], self.ps[4 * d + 1 + step % 2], self.ps[4 * d + 3]
        kST, kO, kC = 'ps%d' % (4 * d), 'ps%d' % (4 * d + 1 + step % 2), 'ps%d' % (4 * d + 3)
        sp_ = step % 2
        stm = STm2[d][sp_]; stk = 'STm%d_%d' % (d, sp_)
        msk = triU if d == 0 else triL
        cdt = Cd2[d][sp_]; cdk = 'Cd%d_%d' % (d, sp_)
        P.add('pe', lambda e: e.matmul(bST[:, 0:128], lhsT=Kt[:, cs], rhs=Q[:, cs], start=True, stop=True), r=sk[0] + sk[1], w=[kST])
        P.add('pe', lambda e: e.matmul(bC[:, 0:130], lhsT=ktm[:, c, :], rhs=vp[d][:, c, :], start=True, stop=True),
              r=['ktm', 'vp%d' % d, 'vpx%d' % d], w=[kC])
        P.add('dve', lambda e: e.tensor_scalar(out=cdt, in0=Cst[d], scalar1=dec[d][:, c:c + 1], scalar2=None, op0=ALU.mult),
              r=['C%d' % d, 'dec%d' % d], w=[cdk])
        P.add('dve', lambda e: e.tensor_tensor(out=stm, in0=bST[:, 0:128], in1=msk, op=ALU.mult), r=['CS'], w=[kST, stk])
        P.add('dve', lambda e: e.tensor_tensor(out=Cst[d], in0=bC[:, 0:130], in1=cdt, op=ALU.add), r=[cdk], w=[kC, 'C%d' % d])
        P.add('pe', lambda e: e.matmul(bO[:, 0:130], lhsT=stm, rhs=vp[d][:, c, :], start=True, stop=False),
              r=[stk, 'vp%d' % d, 'vpx%d' % d], w=[kO])
        P.add('pe', lambda e: e.matmul(bO[:, 0:130], lhsT=Q[:, cs], rhs=cdt, start=False, stop=True), r=sk[0] + [cdk], w=[kO])

    def back(step, d):
        c = order[d][step]
        bO = self.ps[4 * d + 1 + step % 2]; kO = 'ps%d' % (4 * d + 1 + step % 2)
        sp_ = step % 2
        dn_ = dn2[d][sp_]; dnk = 'dn%d_%d' % (d, sp_)
        P.add('act', lambda e: e.activation(out=dn_[:, 0:1], in_=bO[:, 128:129], func=AF.Abs), w=[kO, dnk])
        P.add('dve', lambda e: e.tensor_tensor(out=dn_[:, 0:1], in0=dn_[:, 0:1], in1=flo[d][:, c:c + 1], op=ALU.max), r=['flo%d' % d], w=[dnk])
        P.add('dve', lambda e: e.reciprocal(out=dn_[:, 1:2], in_=dn_[:, 0:1]), w=[dnk])
        P.add('act', lambda e: e.activation(out=H[d][:, c, :], in_=bO[:, 0:128], func=AF.Copy, scale=dn_[:, 1:2]), r=[dnk], w=[kO], cw=['H%d' % d])

    for step in range(NT):
        for d in range(2):
            front(step, d)
        if step > 0:
            for d in range(2):
                back(step - 1, d)
    for d in range(2):
        back(NT - 1, d)
    P.mark('epi')
    Hf = H[0][:, :, :].rearrange("p c d -> p (c d)"); Hb = H[1][:, :, :].rearrange("p c d -> p (c d)")
    P.add('pool', lambda e: e.tensor_tensor(out=Hf, in0=Hf, in1=Hb, op=ALU.add), r=['H1'], w=['H0'])
    P.add('dve', lambda e: e.tensor_tensor(out=tmp, in0=Hf, in1=Hf, op=ALU.mult), r=['H0'], w=['tmp'])
    P.add('dve', lambda e: e.tensor_reduce(out=ssq, in_=tmp.rearrange("p (c d) -> p c d", c=NT), axis=AX.X, op=ALU.add), r=['tmp'], w=['ssq'])
    P.add('act', lambda e: e.activation(out=rst, in_=ssq, func=AF.Sqrt, scale=1.0 / 128, bias=CS[:, C_EPS:C_EPS + 1]), r=['ssq'], w=['rst'])
    P.add('dve', lambda e: e.reciprocal(out=rst, in_=rst), w=['rst'])
    P.add('act', lambda e: e.activation(out=oT, in_=oT, func=AF.Sigmoid), w=[oTk])
    for c in range(NT):
        P.add('act', lambda e, c=c: e.activation(out=H[1][:, c, :], in_=H[0][:, c, :], func=AF.Copy, scale=rst[:, c:c + 1]), r=['H0', 'rst'], cw=['H1'])
    for c in range(NT):
        bank = c % 8
        pk = self.ps[bank]
        P.add('pe', lambda e, c=c, pk=pk: e.transpose(pk[:, 0:128], H[1][:, c, :], ident), r=['H1', 'CS'], w=['ps%d' % bank])
        if colmaj and c >= 2:
            cl = c - 2
            dst = YT[:, TC:].rearrange("p (r c) -> p c r", c=64)[:, 4 * cl:4 * cl + 4, :]
            srcp = pk[:, 0:128].rearrange("p (c r) -> p c r", r=32)
        else:
            dst = YT[:, c * 128:(c + 1) * 128]
            srcp = pk[:, 0:128]
        P.add('dve', lambda e, dst=dst, srcp=srcp: e.tensor_scalar(out=dst, in0=srcp, scalar1=ghn[:, 0:1], scalar2=None, op0=ALU.mult),
              r=[ghk], w=['ps%d' % bank], cw=['YT'])
    P.add('pool', lambda e: e.tensor_tensor(out=YT, in0=YT, in1=oT, op=ALU.add if False else ALU.mult), r=[oTk], w=['YT'])
    P.dma('sp', self.yT[h * 128:(h + 1) * 128, :], YT, r=['YT'], cw=['yTd'])


K.phase2 = _phase2


def _phase34(self, l):
    P, CS = self.P, self.CS
    ones = CS[:, C_ONE:C_ONE + 128]
    o = [0]

    def cv(n):
        a = self.carve(o[0], n); o[0] += n
        return a
    WA = 2334
    cacg = [cv(2 * T) for _ in range(2)]
    ca = [cacg[j][:, 0:T] for j in range(2)]; cg = [cacg[j][:, T:2 * T] for j in range(2)]
    ypad = [cv(2364) for _ in range(2)]; acc = [cv(WA) for _ in range(2)]
    sqt = [cacg[j][:, 0:WA] for j in range(2)]
    ptmp = cv(WA)
    cw = [cv(31) for _ in range(2)]; cb = [cv(1) for _ in range(2)]; lg = [cv(1) for _ in range(2)]; lb = [cv(1) for _ in range(2)]
    mt = [cv(512) for _ in range(2)]; vt = [cv(512) for _ in range(2)]
    col = lambda v, j: v[l, j * 128:(j + 1) * 128].rearrange("(p o) -> p o", o=1)
    for j in range(2):
        P.dma('sp', ca[j], self.uT[2064 + j * 128:2064 + (j + 1) * 128, :], w=['ca%d' % j])
        P.dma('sp', cg[j], self.uT[2320 + j * 128:2320 + (j + 1) * 128, :], w=['cg%d' % j])
        P.dma('sp', cw[j], self.conv_wT[l, j * 128:(j + 1) * 128, :], w=['cw%d' % j])
        P.dma('sp', cb[j], col(self.conv_b, j), w=['cb%d' % j])
        P.dma('sp', lg[j], col(self.conv_ln_g, j), w=['lg%d' % j])
        P.dma('sp', lb[j], col(self.conv_ln_b, j), w=['lb%d' % j])
        P.add('pool', lambda e, j=j: e.memset(ypad[j], 0.0), w=['yp%d' % j])
        P.add('act', lambda e, j=j: e.activation(out=cg[j], in_=cg[j], func=AF.Sigmoid), w=['cg%d' % j])
        P.add('dve', lambda e, j=j: e.tensor_tensor(out=ypad[j][:, 15:15 + TC], in0=ca[j][:, 0:TC], in1=cg[j][:, 0:TC], op=ALU.mult),
              r=['ca%d' % j, 'cg%d' % j], w=['yp%d' % j])
        P.add('dve', lambda e, j=j: e.tensor_tensor(out=ypad[j][:, 301:301 + TL], in0=ca[j][:, TC:], in1=cg[j][:, TC:], op=ALU.mult),
              r=['ca%d' % j, 'cg%d' % j], w=['yp%d' % j])
    BD = CS[:, C_BD:C_BD + 256]
    DCc = CS[:, C_DC:C_DC + 512].rearrange("p (k n) -> p k n", k=2)
    DSc = CS[:, C_DC + 512:C_DC + 1024].rearrange("p (k n) -> p k n", k=2)
    fr = [cv(T) for _ in range(2)]
    Z = [cv(NT * 256).rearrange("p (c n) -> p c n", c=NT) for _ in range(2)]
    YF = fr
    DW = 128
    dcb = [cv(16 * DW).rearrange("p (k n) -> p k n", k=16) for _ in range(2)]
    dsb = [cv(16 * DW).rearrange("p (k n) -> p k n", k=16) for _ in range(2)]
    sc_c = 1.0 / 128.0
    sc_l = float(1.0 / np.sqrt(TL * 64.0))
    nb = 0
    for j in range(2):
        P.dma('sp', fr[j], self.uT[2576 + j * 128:2576 + (j + 1) * 128, :], w=['fr%d' % j])
        for i in range(NT):
            bank = 2 + nb % 6; nb += 1
            pb = self.ps[bank]
            s = sc_c if i < 2 else sc_l
            P.add('pe', lambda e, j=j, i=i, pb=pb: e.matmul(pb[:, 0:256], lhsT=fr[j][:, i * 128:(i + 1) * 128], rhs=BD, start=True, stop=True),
                  r=['fr%d' % j, 'CS'], w=['ps%d' % bank])
            P.add('act', lambda e, j=j, i=i, pb=pb, s=s: e.activation(out=Z[j][:, i, 0:128], in_=pb[:, 0:128], func=AF.Copy, scale=s),
                  w=['ps%d' % bank], cw=['Z%d' % j])
            P.add('act', lambda e, j=j, i=i, pb=pb, s=s: e.activation(out=Z[j][:, i, 128:256], in_=pb[:, 128:256], func=AF.Copy, scale=-s),
                  w=['ps%d' % bank], cw=['Z%d' % j])
    for j in range(2):
        bank = 2 + nb % 6; nb += 1
        pb = self.ps[bank]
        n = 0
        for i in range(2):
            for part, M in ((0, DCc), (1, DSc)):
                P.add('pe', lambda e, j=j, i=i, part=part, M=M, pb=pb, n=n: e.matmul(pb[:, 0:256], lhsT=Z[j][:, i, part * 128:(part + 1) * 128], rhs=M[:, i, :],
                                                                                 start=(n == 0), stop=(n == 3)), r=['Z%d' % j, 'CS'], w=['ps%d' % bank])
                n += 1
        P.add('act', lambda e, j=j, pb=pb: e.activation(out=YF[j][:, 0:TC], in_=pb[:, 0:256], func=AF.Copy), w=['ps%d' % bank], cw=['fr%d' % j])
    for tc in range(TL // DW):
        b = tc % 2
        cols = slice(tc * DW, (tc + 1) * DW)
        P.dma('sp', dcb[b], self.dftc[:, cols].rearrange("(k p) n -> p k n", p=128), w=['dcb%d' % b])
        P.dma('sp', dsb[b], self.dfts[:, cols].rearrange("(k p) n -> p k n", p=128), w=['dsb%d' % b])
        for j in range(2):
            bank = 2 + nb % 6; nb += 1
            pb = self.ps[bank]
            n = 0
            for i in range(16):
                for part, M, mk in ((0, dcb[b], 'dcb%d' % b), (1, dsb[b], 'dsb%d' % b)):
                    P.add('pe', lambda e, j=j, i=i, part=part, M=M, pb=pb, n=n: e.matmul(pb[:, 0:DW], lhsT=Z[j][:, i + 2, part * 128:(part + 1) * 128], rhs=M[:, i, :],
                                                                                     start=(n == 0), stop=(n == 31)), r=['Z%d' % j, mk], w=['ps%d' % bank])
                    n += 1
            P.add('act', lambda e, j=j, pb=pb, tc=tc: e.activation(out=YF[j][:, TC + tc * DW:TC + (tc + 1) * DW], in_=pb[:, 0:DW], func=AF.Copy),
                  w=['ps%d' % bank], cw=['fr%d' % j])
    for j in range(2):
        P.dma('sp', self.yT[768 + j * 128:768 + (j + 1) * 128, :], YF[j], r=['fr%d' % j], cw=['yTd'])


    for k in range(31):
        j = 0
        if k == 0:
            P.add('dve', lambda e, j=j: e.tensor_scalar(out=acc[j], in0=ypad[j][:, 0:WA], scalar1=cw[j][:, 0:1], scalar2=cb[j][:, 0:1], op0=ALU.mult, op1=ALU.add),
                  r=['yp0', 'cw0', 'cb0'], w=['acc0'])
        else:
            P.add('dve', lambda e, j=j, k=k: e.scalar_tensor_tensor(out=acc[j], in0=ypad[j][:, k:k + WA], scalar=cw[j][:, k:k + 1], in1=acc[j],
                                                                    op0=ALU.mult, op1=ALU.add), r=['yp0', 'cw0'], w=['acc0'])
        j = 1
        if k == 0:
            P.add('pool', lambda e, j=j: e.tensor_scalar(out=acc[j], in0=ypad[j][:, 0:WA], scalar1=cw[j][:, 0:1], scalar2=cb[j][:, 0:1], op0=ALU.mult, op1=ALU.add),
                  r=['yp1', 'cw1', 'cb1'], w=['acc1'])
        else:
            P.add('pool', lambda e, j=j, k=k: e.tensor_scalar(out=ptmp, in0=ypad[j][:, k:k + WA], scalar1=cw[j][:, k:k + 1], scalar2=0.0, op0=ALU.mult, op1=ALU.add),
                  r=['yp1', 'cw1'], w=['ptmp'])
            P.add('pool', lambda e, j=j: e.tensor_tensor(out=acc[j], in0=acc[j], in1=ptmp, op=ALU.add), w=['acc1', 'ptmp'])
    for j in range(2):
        P.add('act', lambda e, j=j: e.activation(out=sqt[j], in_=acc[j], func=AF.Square), r=['acc%d' % j], w=['sq%d' % j, 'ca%d' % j, 'cg%d' % j])
    chunks = [(0, 512), (512, 512), (1024, 512), (1536, 512), (2048, 286)]
    def lnS(ci):
        a0, n = chunks[ci]
        b1, b2 = self.ps[0], self.ps[1]
        k1, k2 = 'ps0', 'ps1'
        m_, v_ = mt[ci % 2], vt[ci % 2]
        mk, vk = 'mt%d' % (ci % 2), 'vt%d' % (ci % 2)
        for j in range(2):
            P.add('pe', lambda e, j=j, b1=b1, a0=a0, n=n: e.matmul(b1[:, 0:n], lhsT=ones, rhs=acc[j][:, a0:a0 + n], start=(j == 0), stop=(j == 1)),
                  r=['acc%d' % j, 'CS'], w=[k1])
        for j in range(2):
            P.add('pe', lambda e, j=j, b2=b2, a0=a0, n=n: e.matmul(b2[:, 0:n], lhsT=ones, rhs=sqt[j][:, a0:a0 + n], start=(j == 0), stop=(j == 1)),
                  r=['sq%d' % j, 'CS'], w=[k2])
        P.add('act', lambda e, b1=b1, m_=m_, n=n: e.activation(out=m_[:, 0:n], in_=b1[:, 0:n], func=AF.Copy, scale=1.0 / 256), w=[k1, mk])
        P.add('dve', lambda e, m_=m_, v_=v_, n=n: e.tensor_tensor(out=v_[:, 0:n], in0=m_[:, 0:n], in1=m_[:, 0:n], op=ALU.mult), r=[mk], w=[vk])
        P.add('dve', lambda e, b2=b2, v_=v_, n=n: e.scalar_tensor_tensor(out=v_[:, 0:n], in0=b2[:, 0:n], scalar=1.0 / 256, in1=v_[:, 0:n],
                                                                         op0=ALU.mult, op1=ALU.subtract), w=[k2, vk])
        P.add('act', lambda e, v_=v_, n=n: e.activation(out=v_[:, 0:n], in_=v_[:, 0:n], func=AF.Sqrt, bias=CS[:, C_EPS:C_EPS + 1]), w=[vk])
        P.add('dve', lambda e, v_=v_, n=n: e.reciprocal(out=v_[:, 0:n], in_=v_[:, 0:n]), w=[vk])

    def lnA(ci):
        a0, n = chunks[ci]
        b1, b2 = self.ps[0], self.ps[1]
        k1, k2 = 'ps0', 'ps1'
        m_, v_ = mt[ci % 2], vt[ci % 2]
        mk, vk = 'mt%d' % (ci % 2), 'vt%d' % (ci % 2)
        for j in range(2):
            eng = 'dve' if j == 0 else 'pool'
            P.add(eng, lambda e, j=j, m_=m_, a0=a0, n=n: e.tensor_tensor(out=acc[j][:, a0:a0 + n], in0=acc[j][:, a0:a0 + n], in1=m_[:, 0:n], op=ALU.subtract),
                  r=[mk], w=['acc%d' % j])
            P.add(eng, lambda e, j=j, v_=v_, a0=a0, n=n: e.tensor_tensor(out=acc[j][:, a0:a0 + n], in0=acc[j][:, a0:a0 + n], in1=v_[:, 0:n], op=ALU.mult),
                  r=[vk], w=['acc%d' % j])
            P.add('act', lambda e, j=j, a0=a0, n=n: e.activation(out=acc[j][:, a0:a0 + n], in_=acc[j][:, a0:a0 + n], func=AF.Silu,
                                                                 scale=lg[j][:, 0:1], bias=lb[j][:, 0:1]), r=['lg%d' % j, 'lb%d' % j], w=['acc%d' % j])
    lnS(0)
    for ci in range(len(chunks)):
        if ci + 1 < len(chunks):
            lnS(ci + 1)
        lnA(ci)
    for j in range(2):
        P.dma('sp', self.yT[512 + j * 128:512 + (j + 1) * 128, 0:TC], acc[j][:, 0:TC], r=['acc%d' % j], cw=['yTd'])
        P.dma('sp', self.yT[512 + j * 128:512 + (j + 1) * 128, TC:T], acc[j][:, 286:286 + TL], r=['acc%d' % j], cw=['yTd'])
    P.barrier()


def _phase5(self, l):
    P, CS = self.P, self.CS
    o = [0]

    def cv(n):
        a = self.carve(o[0], n); o[0] += n
        return a
    YA = cv(8 * T).rearrange("p (k t) -> p k t", k=8)
    wo = [cv(4096).rearrange("p (k n) -> p k n", k=8) for _ in range(2)]
    gt = {'l': cv(1024), 'c': cv(1024)}
    xt = [cv(1024) for _ in range(2)]
    tm = [cv(1024) for _ in range(2)]
    for kc in range(8):
        P.dma('sp' if kc % 2 == 0 else 'pool', YA[:, kc, :], self.yT[kc * 128:(kc + 1) * 128, :], w=['YA%d' % kc])
    for hf in range(2):
        P.dma('sp', wo[hf], self.w_out[l, :, hf * 512:(hf + 1) * 512].rearrange("(kc p) n -> p kc n", p=128), w=['wo%d' % hf])
    P.dma('sp', gt['l'], self.modrows[l, 2, 0, :].partition_broadcast(128), w=['gtl'])
    P.dma('sp', gt['c'], self.modrows[l, 2, 1, :].partition_broadcast(128), w=['gtc'])
    nb = 0
    for i in range(NT):
        b = i % 2
        isc = i < 2
        if l == 0:
            src = self.ctx_in[i * 128:(i + 1) * 128, :] if isc else self.x_in[(i - 2) * 128:(i - 1) * 128, :]
        else:
            src = self.xres[i * 128:(i + 1) * 128, :]
        P.dma('sp', xt[b], src, w=['xt%d' % b])
        g_ = gt['c' if isc else 'l']; gk = 'gtc' if isc else 'gtl'
        for hf in range(2):
            bank = nb % 8; nb += 1
            pb = self.ps[bank]
            for kc in range(8):
                P.add('pe', lambda e, i=i, kc=kc, hf=hf, pb=pb: e.matmul(pb[:, 0:512], lhsT=YA[:, kc, i * 128:(i + 1) * 128], rhs=wo[hf][:, kc, :],
                                                                       start=(kc == 0), stop=(kc == 7)), r=['YA%d' % kc, 'wo%d' % hf], w=['ps%d' % bank])
            P.add('dve', lambda e, b=b, hf=hf, pb=pb, g_=g_: e.tensor_tensor(out=tm[b][:, hf * 512:(hf + 1) * 512], in0=pb[:, 0:512],
                                                                            in1=g_[:, hf * 512:(hf + 1) * 512], op=ALU.mult), r=[gk], w=['ps%d' % bank, 'tm%d' % b])
        P.add('pool', lambda e, b=b: e.tensor_tensor(out=tm[b], in0=tm[b], in1=xt[b], op=ALU.add), r=['xt%d' % b], w=['tm%d' % b])
        P.dma('pool', self.xres[i * 128:(i + 1) * 128, :], tm[b], r=['tm%d' % b], cw=['xres'])
    P.barrier()


K.phase34 = _phase34
K.phase5 = _phase5


BIG = 8192.0
AWTOP = 50600 - 288
C_EOFF, C_CAPT, C_KCAP, C_TOK = NCONST, NCONST + 288, NCONST + 576, NCONST + 578
NCONST2 = NCONST + 578 + 18


def make_consts2():
    c = np.zeros((128, NCONST2), np.float32)
    c[:, :NCONST] = make_consts()
    eoff = np.zeros((18, 16), np.float32); capt = np.zeros((18, 16), np.float32)
    for i in range(18):
        for e in range(16):
            eoff[i, e] = e * NSLOT + (0 if i < 2 else CAPC)
            capt[i, e] = CAPC if i < 2 else CAPL
    c[:, C_EOFF:C_EOFF + 288] = eoff.reshape(1, 288)
    c[:, C_CAPT:C_CAPT + 288] = capt.reshape(1, 288)
    c[:, C_KCAP] = CAPC; c[:, C_KCAP + 1] = CAPL
    tok = (np.arange(18)[None, :] * 128 + np.arange(128)[:, None]).astype(np.int32)
    c[:, C_TOK:C_TOK + 18] = tok.view(np.float32)
    return c


def _phase56(self, l):
    P, CS = self.P, self.CS
    ident = CS[:, C_ID:C_ID + 128]
    o = [0]

    def cv(n):
        a = self.carve(o[0], n); o[0] += n
        return a
    aff = cv(288).rearrange("p (c e) -> p c e", c=NT)
    YA = cv(8 * T).rearrange("p (k t) -> p k t", k=8)
    wo = [cv(4096).rearrange("p (k n) -> p k n", k=8) for _ in range(2)]
    gt = {'l': cv(1024), 'c': cv(1024)}
    NB5 = 3
    xt = [cv(1024) for _ in range(NB5)]
    tm = [cv(1024) for _ in range(NB5)]
    modt = {}
    for nm, seg, row in (('gs_l', 4, 0), ('sh_l', 3, 0), ('gs_c', 4, 1), ('sh_c', 3, 1)):
        modt[nm] = cv(1024)
        P.dma('sp', modt[nm], self.modrows[l, seg, row, :].partition_broadcast(128), w=[nm])
    wr = cv(128).rearrange("p (k n) -> p k n", k=8)
    P.dma('sp', wr, self.w_router[l, :, :].rearrange("(kc p) n -> p kc n", p=128), w=['wr'])
    zt = cv(1024)
    P.add('pool', lambda e: e.memset(zt, 0.0), w=['zt'])
    for i in range(NT):
        P.dma('pool', self.macc[i * 128:(i + 1) * 128, :], zt, r=['zt'], cw=['macc'])
    xr = [cv(1042) for _ in range(2)]
    xT = [cv(1024).rearrange("p (k t) -> p k t", k=8) for _ in range(2)]
    sq = cv(1024)
    st = [cv(8) for _ in range(2)]
    ex = cv(16)
    for kc in range(8):
        P.dma('sp' if kc % 2 == 0 else 'pool', YA[:, kc, :], self.yT[kc * 128:(kc + 1) * 128, :], w=['YA%d' % kc])
    for hf in range(2):
        P.dma('sp', wo[hf], self.w_out[l, :, hf * 512:(hf + 1) * 512].rearrange("(kc p) n -> p kc n", p=128), w=['wo%d' % hf])
    P.dma('sp', gt['l'], self.modrows[l, 2, 0, :].partition_broadcast(128), w=['gtl'])
    P.dma('sp', gt['c'], self.modrows[l, 2, 1, :].partition_broadcast(128), w=['gtc'])
    nbc = [0]

    abanks = {}

    def stageA_pe(i):
        b = i % NB5
        isc = i < 2
        if l == 0:
            src = self.ctx_in[i * 128:(i + 1) * 128, :] if isc else self.x_in[(i - 2) * 128:(i - 1) * 128, :]
        else:
            src = self.xres[i * 128:(i + 1) * 128, :]
        P.dma('sp', xt[b], src, w=['xt%d' % b])
        abanks[i] = []
        for hf in range(2):
            bank = 4 + nbc[0] % 4; nbc[0] += 1
            abanks[i].append(bank)
            pb = self.ps[bank]
            for kc in range(8):
                P.add('pe', lambda e, i=i, kc=kc, hf=hf, pb=pb: e.matmul(pb[:, 0:512], lhsT=YA[:, kc, i * 128:(i + 1) * 128], rhs=wo[hf][:, kc, :],
                                                                       start=(kc == 0), stop=(kc == 7)), r=['YA%d' % kc, 'wo%d' % hf], w=['ps%d' % bank])

    def stageA_post(i):
        b = i % NB5
        isc = i < 2
        g_ = gt['c' if isc else 'l']; gk = 'gtc' if isc else 'gtl'
        for hf in range(2):
            bank = abanks[i][hf]
            pb = self.ps[bank]
            P.add('dve', lambda e, b=b, hf=hf, pb=pb, g_=g_: e.tensor_tensor(out=tm[b][:, hf * 512:(hf + 1) * 512], in0=pb[:, 0:512],
                                                                            in1=g_[:, hf * 512:(hf + 1) * 512], op=ALU.mult), r=[gk], w=['ps%d' % bank, 'tm%d' % b])
        P.add('pool', lambda e, b=b: e.tensor_tensor(out=tm[b], in0=tm[b], in1=xt[b], op=ALU.add), r=['xt%d' % b], w=['tm%d' % b])
        P.dma('pool', self.xres[i * 128:(i + 1) * 128, :], tm[b], r=['tm%d' % b], cw=['xres'])

    def stageB1(i):
        b = i % 2
        bt = i % NB5
        isc = i < 2
        x_, xr_, st_, xT_ = tm[bt], xr[b], st[b], xT[b]
        xk = 'tm%d' % bt
        P.dma('pool', xr_[:, 1040:1041], self.consts[:, C_TOK + i:C_TOK + i + 1], cw=['xrk%d' % b], allow_slow_non_contiguous=True)
        P.add('act', lambda e, x_=x_, st_=st_: e.activation(out=sq, in_=x_, func=AF.Square, accum_out=st_[:, 0:1]), r=[xk], w=['sq', 'st%d' % b])
        P.add('act', lambda e, st_=st_: e.activation(out=st_[:, 1:2], in_=st_[:, 0:1], func=AF.Sqrt, scale=1.0 / D, bias=CS[:, C_EPS:C_EPS + 1]), w=['st%d' % b])
        P.add('dve', lambda e, st_=st_: e.reciprocal(out=st_[:, 2:3], in_=st_[:, 1:2]), w=['st%d' % b])
        gs = modt['gs_c' if isc else 'gs_l']; sh = modt['sh_c' if isc else 'sh_l']
        gk2 = 'gs_c' if isc else 'gs_l'; sk2 = 'sh_c' if isc else 'sh_l'
        P.add('dve', lambda e, x_=x_, xr_=xr_, st_=st_, gs=gs: e.scalar_tensor_tensor(out=xr_[:, 0:1024], in0=x_, scalar=st_[:, 2:3], in1=gs, op0=ALU.mult, op1=ALU.mult),
              r=[xk, 'st%d' % b, gk2], w=['xr%d' % b])
        P.add('pool', lambda e, xr_=xr_, sh=sh: e.tensor_tensor(out=xr_[:, 0:1024], in0=xr_[:, 0:1024], in1=sh, op=ALU.add), r=[sk2], w=['xr%d' % b])
        for hb in range(2):
            pb = self.ps[hb]
            for k4 in range(4):
                kc = hb * 4 + k4
                P.add('pe', lambda e, pb=pb, k4=k4, kc=kc, xr_=xr_: e.transpose(pb[:, k4 * 128:(k4 + 1) * 128], xr_[:, kc * 128:(kc + 1) * 128], ident),
                      r=['xr%d' % b, 'CS'], w=['ps%d' % hb])
            dst = xT_[:, hb * 4:hb * 4 + 4, :]
            srcp = pb[:, :].rearrange("p (k t) -> p k t", k=4)
            if hb == 0:
                P.add('act', lambda e, dst=dst, srcp=srcp: e.activation(out=dst, in_=srcp, func=AF.Copy), w=['ps0'], cw=['xT%d' % b])
            else:
                P.add('dve', lambda e, dst=dst, srcp=srcp: e.tensor_copy(out=dst, in_=srcp), w=['ps1'], cw=['xT%d' % b])
        p2 = self.ps[2 + b]
        for kc in range(8):
            P.add('pe', lambda e, kc=kc, p2=p2, xT_=xT_: e.matmul(p2[:, 0:16], lhsT=xT_[:, kc, :], rhs=wr[:, kc, :], start=(kc == 0), stop=(kc == 7)),
                  r=['xT%d' % b, 'wr'], w=['ps%d' % (2 + b)])
    def stageB2(i):
        b = i % 2
        xr_, st_ = xr[b], st[b]
        p2 = self.ps[2 + b]
        P.add('dve', lambda e, p2=p2, st_=st_: e.tensor_reduce(out=st_[:, 3:4], in_=p2[:, 0:16], axis=AX.X, op=ALU.max), w=['ps%d' % (2 + b), 'st%d' % b])
        P.add('dve', lambda e, st_=st_: e.tensor_scalar(out=st_[:, 4:5], in0=st_[:, 3:4], scalar1=-1.0, scalar2=None, op0=ALU.mult), w=['st%d' % b])
        P.add('act', lambda e, p2=p2, st_=st_: e.activation(out=ex, in_=p2[:, 0:16], func=AF.Exp, bias=st_[:, 4:5], accum_out=st_[:, 5:6]),
              w=['ps%d' % (2 + b), 'st%d' % b, 'ex'])
        P.add('dve', lambda e, st_=st_: e.reciprocal(out=st_[:, 6:7], in_=st_[:, 5:6]), w=['st%d' % b])
        P.add('dve', lambda e, i=i, st_=st_: e.tensor_scalar(out=aff[:, i, :], in0=ex, scalar1=st_[:, 6:7], scalar2=None, op0=ALU.mult),
              r=['st%d' % b], w=['ex'], cw=['aff'])
        P.add('pool', lambda e, i=i, xr_=xr_: e.tensor_copy(out=xr_[:, 1024:1040], in_=aff[:, i, :]), r=['aff'], cw=['xrk%d' % b])
        P.dma('pool', self.xn2x[i * 128:(i + 1) * 128, :], xr_, r=['xr%d' % b, 'xrk%d' % b], cw=['xn2x'])
    blocks = list(range(NT))
    if l == L - 1:
        blocks = list(range(2, NT))
        P.add('dve', lambda e: e.memset(aff[:, 0:2, :], 0.0), cw=['aff'])
    stageA_pe(blocks[0])
    stageA_post(blocks[0])
    for j, i in enumerate(blocks):
        nxt = blocks[j + 1] if j + 1 < len(blocks) else None
        if nxt is not None:
            stageA_pe(nxt)
        stageB1(i)
        if nxt is not None:
            stageA_post(nxt)
        stageB2(i)
    P.barrier()


def _phase6b(self, l):
    P, CS = self.P, self.CS
    ident = CS[:, C_ID:C_ID + 128]; ones = CS[:, C_ONE:C_ONE + 128]; triS = CS[:, C_TS:C_TS + 128]
    o = [0]

    def cv(n):
        a = self.carve(o[0], n); o[0] += n
        return a
    aff = cv(288).rearrange("p (c e) -> p c e", c=NT)
    affT = cv(T)
    for i in range(NT):
        bank = 4 + (i // 4) % 4
        pb = self.ps[bank]
        P.add('pe', lambda e, i=i, pb=pb: e.transpose(pb[0:16, (i % 4) * 128:(i % 4 + 1) * 128], aff[:, i, :], ident), r=['aff', 'CS'], w=['ps%d' % bank])
        if i % 4 == 3 or i == NT - 1:
            i0 = (i // 4) * 4
            n = (i - i0 + 1) * 128
            P.add('act', lambda e, pb=pb, i0=i0, n=n: e.activation(out=affT[0:16, i0 * 128:i0 * 128 + n], in_=pb[0:16, 0:n], func=AF.Copy),
                  w=['ps%d' % bank], cw=['affT'])
    lo = cv(2); mid = cv(2); cnt = cv(2); ge = cv(2); junk = cv(TL)
    kcap = CS[0:16, C_KCAP:C_KCAP + 2]
    P.add('dve', lambda e: e.memset(lo[0:16, :], 0.0), w=['lo'])
    segs = [(0, TC), (TC, TL)]
    bg_steps = []
    LAG = 3
    for it in range(1, 31):
        wv = float(2.0 ** (-it))
        k_ = it - 1
        if k_ < len(bg_steps):
            bg_steps[k_][0]()
        if 0 <= k_ - LAG < len(bg_steps) and bg_steps[k_ - LAG][1] is not None:
            bg_steps[k_ - LAG][1]()
        P.add('dve', lambda e, wv=wv: e.tensor_scalar(out=mid[0:16, :], in0=lo[0:16, :], scalar1=wv, scalar2=None, op0=ALU.add), r=['lo'], w=['mid'])
        for s, (a0, n) in enumerate(segs):
            P.add('dve', lambda e, s=s, a0=a0, n=n: e.tensor_scalar(out=junk[0:16, 0:n], in0=affT[0:16, a0:a0 + n], scalar1=mid[0:16, s:s + 1], scalar2=None,
                                                                    op0=ALU.is_ge, op1=ALU.add, accum_out=cnt[0:16, s:s + 1]),
                  r=['affT', 'mid'], w=['junk', 'cnt'])
        P.add('dve', lambda e: e.tensor_tensor(out=ge[0:16, :], in0=cnt[0:16, :], in1=kcap, op=ALU.is_ge), r=['cnt', 'CS'], w=['ge'])
        P.add('dve', lambda e, wv=wv: e.scalar_tensor_tensor(out=lo[0:16, :], in0=ge[0:16, :], scalar=wv, in1=lo[0:16, :], op0=ALU.mult, op1=ALU.add),
              r=['ge'], w=['lo'])
    for k_ in range(30 - LAG, len(bg_steps)):
        if k_ >= 0 and bg_steps[k_][1] is not None:
            bg_steps[k_][1]()
    Dg = cv(32); thb = cv(32)
    for s in range(2):
        P.add('dve', lambda e, s=s: e.tensor_scalar(out=Dg[0:16, s * 16:(s + 1) * 16], in0=ident[0:16, 0:16], scalar1=lo[0:16, s:s + 1], scalar2=None, op0=ALU.mult),
              r=['lo', 'CS'], w=['Dg'])
    p0 = self.ps[0]
    P.add('pe', lambda e: e.matmul(p0[:, 0:32], lhsT=ones[0:16, :], rhs=Dg[0:16, :], start=True, stop=True), r=['Dg', 'CS'], w=['ps0'])
    P.add('act', lambda e: e.activation(out=thb, in_=p0[:, 0:32], func=AF.Copy), w=['ps0', 'thb'])
    mask = cv(288).rearrange("p (c e) -> p c e", c=NT)
    for i in range(NT):
        s = 0 if i < 2 else 1
        if i < 2 and l == L - 1:
            P.add('dve', lambda e, i=i: e.memset(mask[:, i, :], 0.0), cw=['mask'])
            continue
        P.add('dve', lambda e, i=i, s=s: e.tensor_tensor(out=mask[:, i, :], in0=aff[:, i, :], in1=thb[:, s * 16:(s + 1) * 16], op=ALU.is_ge),
              r=['aff', 'thb'], cw=['mask'])
    maskf = mask[:, :, :].rearrange("p c e -> p (c e)")
    p1 = self.ps[1]; p2 = self.ps[2]
    P.add('pe', lambda e: e.matmul(p1[:, 0:288], lhsT=triS, rhs=maskf, start=True, stop=True), r=['mask', 'CS'], w=['ps1'])
    P.add('pe', lambda e: e.matmul(p2[:, 0:288], lhsT=ones, rhs=maskf, start=True, stop=True), r=['mask', 'CS'], w=['ps2'])
    tot = cv(288).rearrange("p (c e) -> p c e", c=NT)
    base = cv(288).rearrange("p (c e) -> p c e", c=NT)
    P.add('act', lambda e: e.activation(out=tot[:, :, :].rearrange("p c e -> p (c e)"), in_=p2[:, 0:288], func=AF.Copy), w=['ps2', 'tot'])
    P.add('dve', lambda e: e.memset(base[:, :, :].rearrange("p c e -> p (c e)"), 0.0), w=['base'])
    P.add('dve', lambda e: e.tensor_copy(out=base[:, 1, :], in_=tot[:, 0, :]), r=['tot'], w=['base'])
    for i in range(3, NT):
        P.add('dve', lambda e, i=i: e.tensor_tensor(out=base[:, i, :], in0=base[:, i - 1, :], in1=tot[:, i - 1, :], op=ALU.add), r=['tot'], w=['base'])
    rank = cv(288); val = cv(288)
    idx = self.carve(AWTOP, 288)
    basef = base[:, :, :].rearrange("p c e -> p (c e)")
    P.add('dve', lambda e: e.tensor_tensor(out=rank, in0=p1[:, 0:288], in1=basef, op=ALU.add), r=['base'], w=['ps1', 'rank'])
    P.add('dve', lambda e: e.tensor_tensor(out=val, in0=rank, in1=CS[:, C_CAPT:C_CAPT + 288], op=ALU.is_lt), r=['rank', 'CS'], w=['val'])
    P.add('dve', lambda e: e.tensor_tensor(out=val, in0=val, in1=maskf, op=ALU.mult), r=['mask'], w=['val'])
    P.add('dve', lambda e: e.tensor_tensor(out=rank, in0=rank, in1=CS[:, C_EOFF:C_EOFF + 288], op=ALU.add), r=['CS'], w=['rank'])
    P.add('dve', lambda e: e.tensor_scalar(out=rank, in0=rank, scalar1=-BIG, scalar2=None, op0=ALU.add), w=['rank'])
    P.add('dve', lambda e: e.tensor_tensor(out=rank, in0=rank, in1=val, op=ALU.mult), r=['val'], w=['rank'])
    P.add('dve', lambda e: e.tensor_scalar(out=rank, in0=rank, scalar1=BIG, scalar2=None, op0=ALU.add), w=['rank'])
    idxi = idx.bitcast(I32)
    P.add('dve', lambda e: e.tensor_copy(out=idxi, in_=rank), r=['rank'], w=['idx'])
    if 'idx' in self.tapd and l == 0:
        P.dma('sp', self.tapd['idx'][:, :], rank, r=['rank'], w=['tapi'])
    P.barrier()


K.phase56 = _phase56
K.phase6b = _phase6b


def _phase8(self, l):
    P, CS = self.P, self.CS
    ident = CS[:, C_ID:C_ID + 128]
    o = [0]

    def cv(n):
        a = self.carve(o[0], n); o[0] += n
        return a
    W = 256
    lastl = (l == L - 1)
    S0 = CAPC if lastl else 0
    NS = NSLOT - S0
    T0 = 2 if lastl else 0
    NWB = 6
    wbuf = [cv(8 * W).rearrange("p (k n) -> p k n", k=8) for _ in range(NWB)]
    xgf = [cv(3 * 1042) for _ in range(2)]
    xsT = cv(8 * NSLOT).rearrange("p (k t) -> p k t", k=8)
    hidT = cv(8 * NSLOT).rearrange("p (k t) -> p k t", k=8)
    ysb = [cv(3 * 1024) for _ in range(2)]
    tmpg = cv(NSLOT)
    xrr = [cv(1042) for _ in range(NT)]
    assert o[0] <= AWTOP, o[0]
    idxi = self.carve(AWTOP, 288).bitcast(I32)
    for i in range(NT):
        P.dma('sp' if i % 2 == 0 else 'act', xrr[i], self.xn2x[i * 128:(i + 1) * 128, :], w=['xrr%d' % i])

    def scatter(ex_):
        for i in range(T0, NT):
            P.add('pool', lambda e, i=i, ex_=ex_: e.indirect_dma_start(
                out=self.xs[:, :], out_offset=bass.IndirectOffsetOnAxis(ap=idxi[:, i * 16 + ex_:i * 16 + ex_ + 1], axis=0),
                in_=xrr[i][:, :], in_offset=None, bounds_check=self.bcreg(e), oob_is_err=False),
                r=['xrr%d' % i], cw=['xs%d' % ex_], dma=True)
    AHEAD = 4
    for ex_ in range(AHEAD):
        scatter(ex_)
    CH = [(32, 128), (160, 128)] if lastl else [(0, 128), (128, 128), (256, 32)]
    cnt = {'nw': 0, 'nd': 0, 'nbk': 0}

    def load_xg(ex_):
        b = ex_ % 2
        xg = xgf[b]
        for ch, (r0, rows) in enumerate(CH):
            P.dma('sp', xg[0:rows, ch * 1042:(ch + 1) * 1042], self.xs[ex_ * NSLOT + r0:ex_ * NSLOT + r0 + rows, :], r=['xs%d' % ex_], cw=['xg%d' % b])

    def transp(ex_):
        b = ex_ % 2
        xg = xgf[b]
        for ch, (r0, rows) in enumerate(CH):
            for hb in range(2):
                bank = cnt['nbk'] % 2; cnt['nbk'] += 1
                pb = self.ps[bank]
                for k4 in range(4):
                    kc = hb * 4 + k4
                    P.add('pe', lambda e, pb=pb, k4=k4, kc=kc, xg=xg, ch=ch, rows=rows: e.transpose(
                        pb[:, k4 * 128:k4 * 128 + rows], xg[0:rows, ch * 1042 + kc * 128:ch * 1042 + (kc + 1) * 128], ident[0:rows, 0:rows]),
                        r=['xg%d' % b, 'CS'], w=['ps%d' % bank])
                dst = xsT[:, hb * 4:hb * 4 + 4, r0 - S0:r0 - S0 + rows]
                srcp = pb[:, :].rearrange("p (k t) -> p k t", k=4)[:, :, 0:rows]
                if bank == 0:
                    P.add('act', lambda e, dst=dst, srcp=srcp: e.activation(out=dst, in_=srcp, func=AF.Copy), w=['ps%d' % bank], cw=['xsT'])
                else:
                    P.add('dve', lambda e, dst=dst, srcp=srcp: e.tensor_copy(out=dst, in_=srcp), w=['ps%d' % bank], cw=['xsT'])

    def gate_up(ex_):
        for fq in range(D // W):
            ig = cnt['nw'] % NWB; cnt['nw'] += 1
            iu = cnt['nw'] % NWB; cnt['nw'] += 1
            wgt, wut = wbuf[ig], wbuf[iu]
            gk_, uk_ = 'wbuf%d' % ig, 'wbuf%d' % iu
            P.dma('sp', wgt, self.w_e_gate[l, ex_, :, fq * W:(fq + 1) * W].rearrange("(kc p) n -> p kc n", p=128), w=[gk_])
            P.dma('sp', wut, self.w_e_up[l, ex_, :, fq * W:(fq + 1) * W].rearrange("(kc p) n -> p kc n", p=128), w=[uk_])
            if fq == 1 and ex_ + 1 < NE:
                load_xg(ex_ + 1)
            for fc in range(W // 128):
                fglob = fq * (W // 128) + fc
                bg_, bu_ = 2 + fglob % 2, 4 + fglob % 2
                pg, pu = self.ps[bg_], self.ps[bu_]
                for kc in range(8):
                    P.add('pe', lambda e, pg=pg, kc=kc, fc=fc, wgt=wgt: e.matmul(pg[:, 0:NS], lhsT=wgt[:, kc, fc * 128:(fc + 1) * 128], rhs=xsT[:, kc, 0:NS],
                                                                            start=(kc == 0), stop=(kc == 7)), r=[gk_, 'xsT'], w=['ps%d' % bg_])
                for kc in range(8):
                    P.add('pe', lambda e, pu=pu, kc=kc, fc=fc, wut=wut: e.matmul(pu[:, 0:NS], lhsT=wut[:, kc, fc * 128:(fc + 1) * 128], rhs=xsT[:, kc, 0:NS],
                                                                            start=(kc == 0), stop=(kc == 7)), r=[uk_, 'xsT'], w=['ps%d' % bu_])
                P.add('act', lambda e, pg=pg: e.activation(out=tmpg[:, 0:NS], in_=pg[:, 0:NS], func=AF.Silu), w=['ps%d' % bg_, 'tmpg'])
                P.add('dve', lambda e, pu=pu, fglob=fglob: e.tensor_tensor(out=hidT[:, fglob, 0:NS], in0=pu[:, 0:NS], in1=tmpg[:, 0:NS], op=ALU.mult),
                      r=['tmpg'], w=['ps%d' % bu_], cw=['hidT'])

    def down(ex_):
        b = ex_ % 2
        xg = xgf[b]
        for dq in range(D // W):
            idn = cnt['nw'] % NWB; cnt['nw'] += 1
            wdt = wbuf[idn]; dk_ = 'wbuf%d' % idn
            P.dma('sp', wdt, self.w_e_down[l, ex_, :, dq * W:(dq + 1) * W].rearrange("(kc p) n -> p kc n", p=128), w=[dk_])
            for ch, (r0, rows) in enumerate(CH):
                bank = 6 + (ch + dq) % 2
                pd = self.ps[bank]
                for fc in range(8):
                    P.add('pe', lambda e, pd=pd, fc=fc, wdt=wdt, r0=r0, rows=rows: e.matmul(pd[0:rows, 0:W], lhsT=hidT[:, fc, r0 - S0:r0 - S0 + rows], rhs=wdt[:, fc, :],
                                                                                      start=(fc == 0), stop=(fc == 7)), r=['hidT', dk_], w=['ps%d' % bank])
                gcol = xg[0:rows, ch * 1042 + 1024 + ex_:ch * 1042 + 1025 + ex_]
                dst = ysb[b][0:rows, ch * 1024 + dq * W:ch * 1024 + (dq + 1) * W]
                if (ch + dq) % 2 == 0:
                    P.add('act', lambda e, pd=pd, dst=dst, gcol=gcol, rows=rows: e.activation(out=dst, in_=pd[0:rows, 0:W], func=AF.Copy, scale=gcol),
                          r=['xg%d' % b], w=['ps%d' % bank], cw=['ysb%d' % b])
                else:
                    P.add('dve', lambda e, pd=pd, dst=dst, gcol=gcol, rows=rows: e.tensor_scalar(out=dst, in0=pd[0:rows, 0:W], scalar1=gcol, scalar2=None, op0=ALU.mult),
                          r=['xg%d' % b], w=['ps%d' % bank], cw=['ysb%d' % b])

    def scatter_add(ex_):
        b = ex_ % 2
        xg = xgf[b]
        for ch, (r0, rows) in enumerate(CH):
            icol = xg[0:rows, ch * 1042 + 1040:ch * 1042 + 1041].bitcast(I32)
            src = ysb[b][0:rows, ch * 1024:(ch + 1) * 1024]
            P.add('pool', lambda e, icol=icol, src=src: e.indirect_dma_start(
                out=self.macc[:, :], out_offset=bass.IndirectOffsetOnAxis(ap=icol, axis=0), in_=src, in_offset=None, compute_op=ALU.add),
                r=['ysb%d' % b, 'xg%d' % b], w=['macc'], dma=True)

    load_xg(0)
    transp(0)
    for ex_ in range(NE):
        gate_up(ex_)
        if ex_ + 1 < NE:
            transp(ex_ + 1)
        down(ex_)
        if ex_ + AHEAD < NE:
            scatter(ex_ + AHEAD)
        scatter_add(ex_)
    P.barrier()


def _phase9(self, l):
    P, CS = self.P, self.CS
    last = (l == L - 1)
    o = [0]

    def cv(n):
        a = self.carve(o[0], n); o[0] += n
        return a
    gt = {'l': cv(1024), 'c': cv(1024)}
    gf = cv(1024)
    P.dma('sp', gt['l'], self.modrows[l, 5, 0, :].partition_broadcast(128), w=['gtl'])
    P.dma('sp', gt['c'], self.modrows[l, 5, 1, :].partition_broadcast(128), w=['gtc'])
    P.dma('sp', gf, self.g_final[:].partition_broadcast(128), w=['gf'])
    NB9 = 4
    xt = [cv(1024) for _ in range(NB9)]; mt = [cv(1024) for _ in range(NB9)]; sqs = [cv(1024) for _ in range(NB9)]
    st = [cv(4) for _ in range(NB9)]
    def s1(i):
        b = i % NB9
        isc = i < 2
        sq = sqs[b]
        g_ = gt['c' if isc else 'l']; gk = 'gtc' if isc else 'gtl'
        x_, m_, st_ = xt[b], mt[b], st[b]
        P.dma('sp', x_, self.xres[i * 128:(i + 1) * 128, :], w=['xt%d' % b])
        P.dma('pool', m_, self.macc[i * 128:(i + 1) * 128, :], w=['mt%d' % b])
        P.add('dve', lambda e: e.tensor_tensor(out=m_, in0=m_, in1=g_, op=ALU.mult), r=[gk], w=['mt%d' % b])
        P.add('pool', lambda e: e.tensor_tensor(out=m_, in0=m_, in1=x_, op=ALU.add), r=['xt%d' % b], w=['mt%d' % b])
        if last:
            P.add('act', lambda e: e.activation(out=sq, in_=m_, func=AF.Square, accum_out=st_[:, 0:1]), r=['mt%d' % b], w=['sq%d' % b, 'st%d' % b])
            P.add('act', lambda e: e.activation(out=st_[:, 1:2], in_=st_[:, 0:1], func=AF.Sqrt, scale=1.0 / D, bias=CS[:, C_EPS:C_EPS + 1]), w=['st%d' % b])

    def s2(i):
        b = i % NB9
        m_, st_ = mt[b], st[b]
        if not last:
            P.dma('act', self.xres[i * 128:(i + 1) * 128, :], m_, r=['mt%d' % b], cw=['xres'])
        else:
            P.add('dve', lambda e: e.reciprocal(out=st_[:, 2:3], in_=st_[:, 1:2]), w=['st%d' % b])
            P.add('dve', lambda e: e.scalar_tensor_tensor(out=m_, in0=m_, scalar=st_[:, 2:3], in1=gf, op0=ALU.mult, op1=ALU.mult),
                  r=['st%d' % b, 'gf'], w=['mt%d' % b])
            P.dma('act', self.out[(i - 2) * 128:(i - 1) * 128, :], m_, r=['mt%d' % b], cw=['out'])
    blocks = [i for i in range(NT) if not (last and i < 2)]
    DEP = 2
    for j in range(min(DEP, len(blocks))):
        s1(blocks[j])
    for j, i in enumerate(blocks):
        if j + DEP < len(blocks):
            s1(blocks[j + DEP])
        s2(i)
    P.barrier()


K.phase8 = _phase8
K.phase9 = _phase9


def kernel(**inputs):
    kk = K(stop_after=None)
    nc = kk.build()
    consts = make_consts2()
    dc, ds = make_dft()
    in_maps = []
    for b in range(8):
        m = host_inputs(inputs, b)
        m['consts'] = consts
        m['dftc'] = dc
        m['dfts'] = ds
        in_maps.append(m)
    res = run_bass_kernel_spmd(nc, in_maps, core_ids=list(range(8)))
    return np.stack([np.asarray(res.results[b]['out']) for b in range(8)], 0).astype(np.float32)
```
